# Optimizing a Trainium2 kernel written in Bass

```python
import jax
import jax.numpy as jnp
from jax import lax
import numpy as np


D_MODEL = 2048
BATCH = 8
SEQ = 2048
DEPTH = 2

CTX_LEN = 256
GRID_W = 64
CHUNK = 64
Q_BLOCK = 128
EPS = 1e-6
N_BRANCH = 4
BR_W = 512

ML_HEADS = 4
ML_DH = 128
ML_W = ML_HEADS * ML_DH
HG_HEADS = 4
HG_DK = 128
HG_DV = 128
HG_W = HG_HEADS * HG_DV
MLA_HEADS = 4
MLA_Q_RANK = 512
MLA_KV_RANK = 256
MLA_NOPE = 128
MLA_ROPE = 64
MLA_DV = 128
MLA_SCALE = (MLA_NOPE + MLA_ROPE) ** -0.5
ROPE_BASE = 10000.0
SSD_HEADS = 8
SSD_P = 64
SSD_W = SSD_HEADS * SSD_P
SSD_GROUPS = 2
SSD_N = 128
SSD_GN = SSD_GROUPS * SSD_N
SSD_XBC = SSD_W + 2 * SSD_GN
SSD_CONV = 3
N_GROUPS = 4
EXP_PER_GROUP = 8
N_EXPERTS = N_GROUPS * EXP_PER_GROUP
TOP_K = 2
D_EXPERT = 512

IN_SIZES = (ML_W, ML_W, ML_W, ML_W, 4 * ML_HEADS,
            HG_W, HG_W, HG_W, 2 * HG_W,
            MLA_Q_RANK, MLA_KV_RANK, MLA_ROPE,
            SSD_W, SSD_XBC, 2 * SSD_HEADS)
D_IN = sum(IN_SIZES)

F32 = jnp.float32

kernel_name = 'hybrid_mlstm_hgrn2_mla_ssd_hmoe_dit'


def rmsnorm(x, g):
    x32 = x.astype(F32)
    y = x32 * lax.rsqrt(jnp.mean(x32 * x32, axis=-1, keepdims=True) + EPS)
    return (y * g.astype(F32)).astype(x.dtype)


def modulate(x, shift, scale):
    return x * (1.0 + scale) + shift


def split_cols(u, sizes):
    idx, acc = [], 0
    for s in sizes[:-1]:
        acc += s
        idx.append(acc)
    return jnp.split(u, idx, axis=-1)


def split_heads(t, n_heads):
    b, l, _ = t.shape
    return t.reshape(b, l, n_heads, -1).transpose(0, 2, 1, 3)


def merge_heads(t):
    b, n, l, d = t.shape
    return t.transpose(0, 2, 1, 3).reshape(b, l, n * d)


def chunk_tril():
    return jnp.tril(jnp.ones((CHUNK, CHUNK), dtype=bool))


def to_chunks(t):
    b, h, l = t.shape[:3]
    t = t.reshape((b, h, l // CHUNK, CHUNK) + t.shape[3:])
    return jnp.moveaxis(t, 2, 0)


def from_chunks(t):
    t = jnp.moveaxis(t, 0, 2)
    return t.reshape(t.shape[:2] + (t.shape[2] * t.shape[3],) + t.shape[4:])


def chunk_scan(body, inputs, state):
    xs = tuple(to_chunks(t) for t in inputs)
    state, ys = lax.scan(body, state, xs)
    return from_chunks(ys), state


def run_direction(body, ctx_in, lat_in, init, reverse):
    def flip(t):
        return jnp.flip(t, axis=2) if reverse else t
    y_ctx, state = chunk_scan(body, tuple(flip(t) for t in ctx_in), init)
    y_lat, _ = chunk_scan(body, tuple(flip(t) for t in lat_in), state)
    return flip(y_ctx), flip(y_lat)


def mlstm_chunk(carry, inp):
    c_mat, n_vec, m_sc = carry
    q, k, v, i_log, f_log = inp
    tril = chunk_tril()
    f_cum = jnp.cumsum(f_log, axis=-1)
    d_mat = jnp.where(tril, f_cum[..., :, None] - f_cum[..., None, :] + i_log[..., None, :], -jnp.inf)
    from_state = f_cum + m_sc[..., None]
    m_t = jnp.maximum(from_state, jnp.max(d_mat, axis=-1))
    s = jnp.einsum('bhtd,bhsd->bhts', q, k) * jnp.exp(d_mat - m_t[..., None])
    w_state = jnp.exp(from_state - m_t)
    num = jnp.einsum('bhts,bhsv->bhtv', s, v) + w_state[..., None] * jnp.einsum('bhtk,bhkv->bhtv', q, c_mat)
    den = jnp.sum(s, axis=-1) + w_state * jnp.einsum('bhtk,bhk->bht', q, n_vec)
    h = num / jnp.maximum(jnp.abs(den), jnp.exp(-m_t))[..., None]
    f_tot = f_cum[..., -1]
    w_log = f_tot[..., None] - f_cum + i_log
    m_new = jnp.maximum(f_tot + m_sc, jnp.max(w_log, axis=-1))
    decay = jnp.exp(f_tot + m_sc - m_new)
    w_in = jnp.exp(w_log - m_new[..., None])
    c_new = decay[..., None, None] * c_mat + jnp.einsum('bhs,bhsk,bhsv->bhkv', w_in, k, v)
    n_new = decay[..., None] * n_vec + jnp.einsum('bhs,bhsk->bhk', w_in, k)
    return (c_new, n_new, m_new), h


def mlstm_branch(parts_c, parts_l, f_bias):
    def prep(q, k, v, o, gates):
        b, l, _ = q.shape
        qh = split_heads(q.astype(F32), ML_HEADS) * (ML_DH ** -0.5)
        kh = split_heads(k.astype(F32), ML_HEADS)
        vh = split_heads(v.astype(F32), ML_HEADS)
        g = gates.astype(F32).reshape(b, l, 2, 2, ML_HEADS).transpose(2, 3, 0, 4, 1)
        f_log = jax.nn.log_sigmoid(g[:, 1] + f_bias.astype(F32)[:, None, :, None])
        return (qh, kh, vh, g[0, 0], f_log[0]), (qh, kh, vh, g[1, 0], f_log[1]), o
    fc, bc, oc = prep(*parts_c)
    fl, bl, ol = prep(*parts_l)
    b = parts_l[0].shape[0]
    init = (jnp.zeros((b, ML_HEADS, ML_DH, ML_DH), F32), jnp.zeros((b, ML_HEADS, ML_DH), F32),
            jnp.full((b, ML_HEADS), -jnp.inf, F32))
    yc_f, yl_f = run_direction(mlstm_chunk, fc, fl, init, False)
    yc_b, yl_b = run_direction(mlstm_chunk, bc, bl, init, True)

    def finish(y_f, y_b, o):
        return (merge_heads(y_f + y_b) * jax.nn.sigmoid(o.astype(F32))).astype(o.dtype)
    return finish(yc_f, yc_b, oc), finish(yl_f, yl_b, ol)


def hgrn2_chunk(s_state, inp):
    q, k, v, lf = inp
    tril = chunk_tril()
    g_cum = jnp.cumsum(lf, axis=2)
    rel = jnp.where(tril[:, :, None], g_cum[:, :, :, None, :] - g_cum[:, :, None, :, :], -jnp.inf)
    a = jnp.einsum('bhtd,bhsd,bhtsd->bhts', q, k, jnp.exp(rel))
    o = jnp.einsum('bhts,bhsv->bhtv', a, v) + jnp.einsum('bhtd,bhdv->bhtv', q * jnp.exp(g_cum), s_state)
    g_tot = g_cum[:, :, -1]
    k_dec = k * jnp.exp(g_tot[:, :, None, :] - g_cum)
    s_new = jnp.exp(g_tot)[..., None] * s_state + jnp.einsum('bhsd,bhsv->bhdv', k_dec, v)
    return s_new, o


def hgrn2_branch(parts_c, parts_l, lb, norm_g):
    def prep(q, i, g, f_raw):
        b, l, _ = q.shape
        qh = split_heads(jax.nn.silu(q.astype(F32)), HG_HEADS)
        vh = split_heads(i.astype(F32), HG_HEADS)
        f = lb + (1.0 - lb) * jax.nn.sigmoid(f_raw.astype(F32).reshape(b, l, 2, HG_W))
        dirs = []
        for d in range(2):
            fd = split_heads(f[:, :, d], HG_HEADS)
            dirs.append((qh, 1.0 - fd, vh, jnp.log(fd)))
        return dirs[0], dirs[1], g
    fc, bc, gc = prep(*parts_c)
    fl, bl, gl = prep(*parts_l)
    b = parts_l[0].shape[0]
    init = jnp.zeros((b, HG_HEADS, HG_DK, HG_DV), F32)
    yc_f, yl_f = run_direction(hgrn2_chunk, fc, fl, init, False)
    yc_b, yl_b = run_direction(hgrn2_chunk, bc, bl, init, True)

    def finish(y_f, y_b, g):
        bb, h, l, dv = y_f.shape
        o = (y_f + y_b).transpose(0, 2, 1, 3)
        o = rmsnorm(o, norm_g.reshape(HG_HEADS, HG_DV)).reshape(bb, l, HG_W)
        return (o * jax.nn.sigmoid(g.astype(F32))).astype(g.dtype)
    return finish(yc_f, yc_b, gc), finish(yl_f, yl_b, gl)


def axial_rope(x, pos_row, pos_col):
    half = MLA_ROPE // 2
    quarter = half // 2
    inv_freq = ROPE_BASE ** (-jnp.arange(quarter, dtype=F32) / quarter)

    def rot(xa, pos):
        ang = pos.astype(F32)[:, None] * inv_freq
        shape = (ang.shape[0],) + (1,) * (xa.ndim - 3) + (quarter,)
        cos, sin = jnp.cos(ang).reshape(shape), jnp.sin(ang).reshape(shape)
        x1, x2 = xa[..., :quarter], xa[..., quarter:]
        return jnp.concatenate([x1 * cos - x2 * sin, x1 * sin + x2 * cos], axis=-1)
    x32 = x.astype(F32)
    return jnp.concatenate([rot(x32[..., :half], pos_row), rot(x32[..., half:], pos_col)], axis=-1).astype(x.dtype)


def softmax_attend(q, k, v):
    s = jnp.einsum('bhqd,bhkd->bhqk', q, k).astype(F32) * MLA_SCALE
    p = jax.nn.softmax(s, axis=-1)
    return jnp.einsum('bhqk,bhkv->bhqv', p.astype(v.dtype), v)


def mla_branch(parts_c, parts_l, pos, g_q, g_kv, w_uq, w_ukv, emit_ctx):
    def project(cq, ckv, k_rope, rope_pos):
        b, l, _ = cq.shape
        q = (rmsnorm(cq, g_q) @ w_uq).reshape(b, l, MLA_HEADS, MLA_NOPE + MLA_ROPE)
        kv = (rmsnorm(ckv, g_kv) @ w_ukv).reshape(b, l, MLA_HEADS, MLA_NOPE + MLA_DV)
        q_nope, q_rope = q[..., :MLA_NOPE], q[..., MLA_NOPE:]
        k_nope, v = kv[..., :MLA_NOPE], kv[..., MLA_NOPE:]
        if rope_pos is not None:
            q_rope = axial_rope(q_rope, *rope_pos)
            k_rope = axial_rope(k_rope, *rope_pos)
        k_rope = jnp.broadcast_to(k_rope[:, :, None, :], (b, l, MLA_HEADS, MLA_ROPE))
        q = jnp.concatenate([q_nope, q_rope], axis=-1).transpose(0, 2, 1, 3)
        k = jnp.concatenate([k_nope, k_rope], axis=-1).transpose(0, 2, 1, 3)
        return q, k, v.transpose(0, 2, 1, 3)
    qc, kc, vc = project(*parts_c, None)
    ql, kl, vl = project(*parts_l, pos)
    k_all = jnp.concatenate([kc, kl], axis=2)
    v_all = jnp.concatenate([vc, vl], axis=2)
    b, h, s, dh = ql.shape
    n_blk = s // Q_BLOCK
    q_blocks = ql.reshape(b, h, n_blk, Q_BLOCK, dh).transpose(2, 0, 1, 3, 4)

    def attend(q_blk):
        return softmax_attend(q_blk, k_all, v_all)
    ol = lax.map(attend, q_blocks)
    ol = ol.transpose(1, 0, 3, 2, 4).reshape(b, s, h * MLA_DV)
    oc = merge_heads(softmax_attend(qc, kc, vc)) if emit_ctx else None
    return oc, ol


def centred_dwconv(x, w, b):
    pad = (SSD_CONV - 1) // 2
    y = lax.conv_general_dilated(x, w[:, None, :].astype(x.dtype), window_strides=(1,),
                                 padding=[(pad, pad)], dimension_numbers=('NWC', 'WIO', 'NWC'),
                                 feature_group_count=x.shape[-1])
    return y + b.astype(x.dtype)


def ssd_chunk(h_state, inp):
    x, bm, cm, da, dt = inp
    tril = chunk_tril()
    a_cum = jnp.cumsum(da, axis=-1)
    decay = jnp.exp(jnp.where(tril, a_cum[..., :, None] - a_cum[..., None, :], -jnp.inf))
    xdt = x * dt[..., None]
    s = jnp.einsum('bhtn,bhsn->bhts', cm, bm) * decay
    y = jnp.einsum('bhts,bhsp->bhtp', s, xdt) + jnp.exp(a_cum)[..., None] * jnp.einsum('bhtn,bhpn->bhtp', cm, h_state)
    a_tot = a_cum[..., -1]
    w = jnp.exp(a_tot[..., None] - a_cum)
    h_new = jnp.exp(a_tot)[..., None, None] * h_state + jnp.einsum('bhs,bhsp,bhsn->bhpn', w, xdt, bm)
    return h_new, y


def ssd_branch(parts_c, parts_l, conv_w, conv_b, a_log, dt_bias, d_skip, norm_g):
    a_neg = -jnp.exp(a_log.astype(F32))

    def prep(z, xbc, dt_raw):
        b, l, _ = xbc.shape
        xbc = jax.nn.silu(centred_dwconv(xbc, conv_w, conv_b)).astype(F32)
        xs, bm, cm = xbc[..., :SSD_W], xbc[..., SSD_W:SSD_W + SSD_GN], xbc[..., SSD_W + SSD_GN:]
        xh = split_heads(xs, SSD_HEADS)

        def group_to_heads(t):
            t = t.reshape(b, l, SSD_GROUPS, SSD_N)
            return jnp.repeat(t, SSD_HEADS // SSD_GROUPS, axis=2).transpose(0, 2, 1, 3)
        bh, ch = group_to_heads(bm), group_to_heads(cm)
        dt = jax.nn.softplus(dt_raw.astype(F32).reshape(b, l, 2, SSD_HEADS) + dt_bias.astype(F32))
        dt = dt.transpose(2, 0, 3, 1)
        da = dt * a_neg[:, None, :, None]
        return (xh, bh, ch, da[0], dt[0]), (xh, bh, ch, da[1], dt[1]), xh, z
    fc, bc, xc, zc = prep(*parts_c)
    fl, bl, xl, zl = prep(*parts_l)
    b = parts_l[0].shape[0]
    init = jnp.zeros((b, SSD_HEADS, SSD_P, SSD_N), F32)
    yc_f, yl_f = run_direction(ssd_chunk, fc, fl, init, False)
    yc_b, yl_b = run_direction(ssd_chunk, bc, bl, init, True)

    def finish(y_f, y_b, xh, z):
        y = y_f + y_b + d_skip.astype(F32)[None, :, None, None] * xh
        y = merge_heads(y) * jax.nn.silu(z.astype(F32))
        return rmsnorm(y, norm_g).astype(z.dtype)
    return finish(yc_f, yc_b, xc, zc), finish(yl_f, yl_b, xl, zl)


def merge_branches(h, branches, w_gate, w_br, w_out):
    acc = None
    for k in range(N_BRANCH):
        gate = jax.nn.sigmoid((h @ w_gate[k]).astype(F32))
        term = gate * (branches[k] @ w_br[k]).astype(F32)
        acc = term if acc is None else acc + term
    return acc.astype(h.dtype) @ w_out


def mixer_sublayer(hc, hl, pos, lb, w_in, ml_f_bias, hg_norm_g, mla_g_q, mla_g_kv, mla_w_uq, mla_w_ukv,
                   ssd_conv_w, ssd_conv_b, ssd_a_log, ssd_dt_bias, ssd_d, ssd_norm_g,
                   w_gate, w_br, w_out, emit_ctx):
    n_ctx = hc.shape[1]
    h = jnp.concatenate([hc, hl], axis=1)
    parts = split_cols(h @ w_in, IN_SIZES)
    pc = [t[:, :n_ctx] for t in parts]
    pl = [t[:, n_ctx:] for t in parts]
    ml = mlstm_branch(pc[0:5], pl[0:5], ml_f_bias)
    hg = hgrn2_branch(pc[5:9], pl[5:9], lb, hg_norm_g)
    mla = mla_branch(pc[9:12], pl[9:12], pos, mla_g_q, mla_g_kv, mla_w_uq, mla_w_ukv, emit_ctx)
    ssd = ssd_branch(pc[12:15], pl[12:15], ssd_conv_w, ssd_conv_b, ssd_a_log, ssd_dt_bias, ssd_d, ssd_norm_g)
    outs = (ml, hg, mla, ssd)
    if emit_ctx:
        y = merge_branches(h, [jnp.concatenate([o[0], o[1]], axis=1) for o in outs], w_gate, w_br, w_out)
        return y[:, :n_ctx], y[:, n_ctx:]
    return None, merge_branches(hl, [o[1] for o in outs], w_gate, w_br, w_out)


def moe_ffn(h, w_grp, w_exp, w1, w3, w2):
    b, l, _ = h.shape
    grp_prob = jax.nn.softmax((h @ w_grp).astype(F32), axis=-1)
    grp_p, grp_idx = lax.top_k(grp_prob, 1)
    exp_logits = (h @ w_exp).astype(F32).reshape(b, l, N_GROUPS, EXP_PER_GROUP)
    sel = jnp.take_along_axis(exp_logits, grp_idx[..., None], axis=2)[..., 0, :]
    top_v, top_i = lax.top_k(sel, TOP_K)
    w_top = jax.nn.softmax(top_v, axis=-1) * grp_p
    expert_id = grp_idx * EXP_PER_GROUP + top_i
    combine = jnp.sum(jax.nn.one_hot(expert_id, N_EXPERTS, dtype=F32) * w_top[..., None], axis=-2)
    out = None
    for e in range(N_EXPERTS):
        y = (jax.nn.silu(h @ w1[e]) * (h @ w3[e])) @ w2[e]
        term = combine[..., e:e + 1] * y.astype(F32)
        out = term if out is None else out + term
    return out.astype(h.dtype)


def setup_inputs(seed: int = 0) -> dict:
    key = jax.random.key(seed)
    ks = jax.random.split(key, 29)

    def nrm(k, shape, scale):
        return scale * jax.random.normal(k, shape, F32)
    d = D_MODEL
    dt0 = jnp.exp(jax.random.uniform(ks[18], (DEPTH, 2, SSD_HEADS), F32, np.log(1e-3), np.log(1e-1)))
    return {
        'x': nrm(ks[0], (BATCH, SEQ, d), 1.0),
        'c': nrm(ks[1], (BATCH, d), 1.0),
        'ctx': nrm(ks[2], (BATCH, CTX_LEN, d), 1.0),
        'c_ctx': nrm(ks[3], (d,), 1.0),
        'w_ada': nrm(ks[4], (DEPTH, d, 6 * d), 0.2 * d ** -0.5),
        'b_ada': nrm(ks[5], (DEPTH, 6 * d), 0.01),
        'norm_g': 1.0 + nrm(ks[6], (DEPTH, 4, d), 0.05),
        'w_in': nrm(ks[7], (DEPTH, d, D_IN), d ** -0.5),
        'ml_f_bias': jax.random.uniform(ks[8], (DEPTH, 2, ML_HEADS), F32, 3.0, 6.0),
        'hg_lb_logits': nrm(ks[9], (DEPTH + 1, HG_W), 0.5),
        'hg_norm_g': 1.0 + nrm(ks[10], (DEPTH, HG_W), 0.05),
        'mla_g_q': 1.0 + nrm(ks[11], (DEPTH, MLA_Q_RANK), 0.05),
        'mla_g_kv': 1.0 + nrm(ks[12], (DEPTH, MLA_KV_RANK), 0.05),
        'mla_w_uq': nrm(ks[13], (DEPTH, MLA_Q_RANK, MLA_HEADS * (MLA_NOPE + MLA_ROPE)), MLA_Q_RANK ** -0.5),
        'mla_w_ukv': nrm(ks[14], (DEPTH, MLA_KV_RANK, MLA_HEADS * (MLA_NOPE + MLA_DV)), MLA_KV_RANK ** -0.5),
        'ssd_conv_w': nrm(ks[15], (DEPTH, SSD_CONV, SSD_XBC), SSD_CONV ** -0.5),
        'ssd_conv_b': nrm(ks[16], (DEPTH, SSD_XBC), 0.01),
        'ssd_a_log': jnp.log(jax.random.uniform(ks[17], (DEPTH, 2, SSD_HEADS), F32, 1.0, 16.0)),
        'ssd_dt_bias': dt0 + jnp.log(-jnp.expm1(-dt0)),
        'ssd_d': 1.0 + nrm(ks[19], (DEPTH, SSD_HEADS), 0.1),
        'ssd_norm_g': 1.0 + nrm(ks[20], (DEPTH, SSD_W), 0.05),
        'w_gate': nrm(ks[21], (DEPTH, N_BRANCH, d, d), d ** -0.5),
        'w_br': nrm(ks[22], (DEPTH, N_BRANCH, BR_W, d), BR_W ** -0.5),
        'w_out': nrm(ks[23], (DEPTH, d, d), d ** -0.5),
        'moe_w_grp': nrm(ks[24], (DEPTH, d, N_GROUPS), d ** -0.5),
        'moe_w_exp': nrm(ks[25], (DEPTH, d, N_EXPERTS), d ** -0.5),
        'moe_w1': nrm(ks[26], (DEPTH, N_EXPERTS, d, D_EXPERT), d ** -0.5),
        'moe_w3': nrm(ks[27], (DEPTH, N_EXPERTS, d, D_EXPERT), d ** -0.5),
        'moe_w2': nrm(ks[28], (DEPTH, N_EXPERTS, D_EXPERT, d), D_EXPERT ** -0.5),
    }


def reference(x, c, ctx, c_ctx, w_ada, b_ada, norm_g, w_in, ml_f_bias, hg_lb_logits, hg_norm_g,
              mla_g_q, mla_g_kv, mla_w_uq, mla_w_ukv, ssd_conv_w, ssd_conv_b, ssd_a_log, ssd_dt_bias,
              ssd_d, ssd_norm_g, w_gate, w_br, w_out, moe_w_grp, moe_w_exp, moe_w1, moe_w3, moe_w2):
    n_ctx = ctx.shape[1]
    rows = x.shape[1] // GRID_W
    tok = jnp.arange(rows * GRID_W)
    pos = (tok // GRID_W, tok % GRID_W)
    lb_all = jnp.cumsum(jax.nn.softmax(hg_lb_logits.astype(F32), axis=0), axis=0)
    s_c = jax.nn.silu(c)
    s_ctx = jax.nn.silu(c_ctx)
    xl, xc = x, ctx
    for l in range(DEPTH):
        emit_ctx = l < DEPTH - 1
        mod_l = [t[:, None, :] for t in jnp.split(s_c @ w_ada[l] + b_ada[l], 6, axis=-1)]
        mod_c = jnp.split(s_ctx @ w_ada[l] + b_ada[l], 6, axis=-1)
        hl = modulate(rmsnorm(xl, norm_g[l, 0]), mod_l[0], mod_l[1])
        hc = modulate(rmsnorm(xc, norm_g[l, 0]), mod_c[0], mod_c[1])
        yc, yl = mixer_sublayer(hc, hl, pos, lb_all[l], w_in[l], ml_f_bias[l], hg_norm_g[l],
                                mla_g_q[l], mla_g_kv[l], mla_w_uq[l], mla_w_ukv[l],
                                ssd_conv_w[l], ssd_conv_b[l], ssd_a_log[l], ssd_dt_bias[l], ssd_d[l],
                                ssd_norm_g[l], w_gate[l], w_br[l], w_out[l], emit_ctx)
        xl = xl + mod_l[2] * rmsnorm(yl, norm_g[l, 1])
        hl2 = modulate(rmsnorm(xl, norm_g[l, 2]), mod_l[3], mod_l[4])
        if emit_ctx:
            xc = xc + mod_c[2] * rmsnorm(yc, norm_g[l, 1])
            hc2 = modulate(rmsnorm(xc, norm_g[l, 2]), mod_c[3], mod_c[4])
            y2 = moe_ffn(jnp.concatenate([hc2, hl2], axis=1), moe_w_grp[l], moe_w_exp[l], moe_w1[l], moe_w3[l], moe_w2[l])
            xc = xc + mod_c[5] * rmsnorm(y2[:, :n_ctx], norm_g[l, 3])
            yl2 = y2[:, n_ctx:]
        else:
            yl2 = moe_ffn(hl2, moe_w_grp[l], moe_w_exp[l], moe_w1[l], moe_w3[l], moe_w2[l])
        xl = xl + mod_l[5] * rmsnorm(yl2, norm_g[l, 3])
    return xl
```

```python
import os
import numpy as np
from contextlib import ExitStack
import concourse.bass as bass
import concourse.mybir as mybir
from concourse.bass_utils import run_bass_kernel_spmd

F32 = mybir.dt.float32
BF16 = mybir.dt.bfloat16
AF = mybir.ActivationFunctionType
ALU = mybir.AluOpType
AX = mybir.AxisListType

D = 2048
KC = 16
DIN = 7008
EPS = 1e-6
NEG = -30000.0
EPOCH = 20000
N_DMA_SEMS = 24

O_MLQ, O_MLK, O_MLV, O_MLO, O_MLG = 0, 512, 1024, 1536, 2048
O_HGQ, O_HGI, O_HGG, O_HGF = 2064, 2576, 3088, 3600
O_CQ, O_CKV, O_KR = 4624, 5136, 5392
O_SZ, O_XBC, O_DT = 5456, 5968, 6992


class Buf:
    __slots__ = ("w", "r", "excl")

    def __init__(self):
        self.w = None
        self.r = {}
        self.excl = False


class Tl:
    def __init__(self, t):
        self.t = t
        self.b = Buf()

    def __getitem__(self, idx):
        return self.t.ap()[idx]

    @property
    def ap(self):
        return self.t.ap()


class Prog:
    def __init__(self, nc):
        self.nc = nc
        self.engs = {"pe": nc.tensor, "act": nc.scalar, "dve": nc.vector,
                     "pool": nc.gpsimd, "sp": nc.sync}
        self.sem = {}
        self.cnt = {}
        self.nsem = 0
        self.last = {}
        for e in self.engs:
            self._new_epoch(e)
        self.waited = {e: {} for e in self.engs}
        self.dma_sems = []
        for i in range(N_DMA_SEMS):
            s = nc.alloc_semaphore(f"dsem{i}")
            self.dma_sems.append([s, 0, ("d", i)])
        self.dma_rr = 0
        self.ninst = 0

    def _new_epoch(self, e):
        self.nsem += 1
        s = self.nc.alloc_semaphore(f"s_{e}_{self.nsem}")
        self.sem[e] = (s, ("e", e, self.nsem))
        self.cnt[e] = 0

    def _wait(self, eng, evs):
        best = {}
        for ev in evs:
            if ev is None:
                continue
            sem, val, key, src = ev
            if eng == "pe" and src == "pe":
                continue
            if self.waited[eng].get(key, 0) >= val:
                continue
            if key not in best or best[key][1] < val:
                best[key] = ev
        for key, (sem, val, _, _) in best.items():
            self.engs[eng].wait_ge(sem, val)
            self.waited[eng][key] = val

    def _tick(self, eng, inst):
        if self.cnt[eng] >= EPOCH:
            self._new_epoch(eng)
        sem, key = self.sem[eng]
        self.cnt[eng] += 1
        inst.then_inc(sem, 1)
        self.ninst += 1
        ev = (sem, self.cnt[eng], key, eng)
        self.last[key] = ev
        return ev

    @staticmethod
    def _deps(r, w):
        evs = []
        for b in r:
            evs.append(b.w)
            if b.excl:
                evs.extend(b.r.values())
        for b in w:
            evs.append(b.w)
            evs.extend(b.r.values())
        return evs

    @staticmethod
    def _commit(ev, r, w):
        for b in r:
            old = b.r.get(ev[2])
            if old is None or old[1] < ev[1]:
                b.r[ev[2]] = ev
        for b in w:
            b.w = ev
            b.r = {}

    def op(self, eng, fn, r=(), w=()):
        r = [x.b if isinstance(x, Tl) else x for x in r]
        w = [x.b if isinstance(x, Tl) else x for x in w]
        self._wait(eng, self._deps(r, w))
        inst = fn(self.engs[eng])
        ev = self._tick(eng, inst)
        self._commit(ev, r, w)
        return ev

    def dma(self, q, out, in_, r=(), w=(), **kw):
        r = [x.b if isinstance(x, Tl) else x for x in r]
        w = [x.b if isinstance(x, Tl) else x for x in w]
        ent = self.dma_sems[self.dma_rr]
        self.dma_rr = (self.dma_rr + 1) % len(self.dma_sems)
        sem, val, key = ent
        evs = self._deps(r, w)
        if val > 0:
            evs.append((sem, val, key, "dma"))
        self._wait(q, evs)
        inst = self.engs[q].dma_start(out=out, in_=in_, **kw)
        ent[1] = val + 16
        inst.then_inc(sem, 16)
        ev = (sem, val + 16, key, "dma")
        self._commit(ev, r, w)
        self.last[key] = ev
        self.ninst += 1
        return ev

    def barrier(self):
        evs = list(self.last.values())
        for e in self.engs:
            self._wait(e, evs)


C_ID, C_TRF, C_TRB, C_MNF, C_MNB, C_ONE, C_LCF, C_LCB, C_HC, C_PERM = range(10)
NCST = 10


def make_consts(n_ctx, n_lat):
    s = np.arange(128)[:, None]
    t = np.arange(128)[None, :]
    c = np.zeros((NCST, 128, 128), np.float32)
    c[C_ID] = np.eye(128)
    c[C_TRF] = (s <= t)
    c[C_TRB] = (s >= t)
    c[C_MNF] = np.where(s <= t, 0.0, NEG)
    c[C_MNB] = np.where(s >= t, 0.0, NEG)
    c[C_ONE] = 1.0
    c[C_LCF] = (s <= t).astype(np.float32) - (s <= 63)
    c[C_LCB] = (s >= t).astype(np.float32) - (s >= 64)
    sv = np.arange(128)
    c[C_HC][:, 0] = (sv <= 63)
    c[C_HC][:, 1] = 1.0
    c[C_HC][:, 2] = 1.0 - (sv <= 63)
    c[C_HC][:, 3] = (sv >= 64)
    c[C_HC][:, 4] = 1.0
    c[C_HC][:, 5] = 1.0 - (sv >= 64)
    Pm = np.zeros((64, 64), np.float32)
    for base in (0, 32):
        for i in range(16):
            Pm[base + i, base + 16 + i] = -1.0
            Pm[base + 16 + i, base + i] = 1.0
    c[C_PERM][:64, :64] = Pm.T
    consts = np.ascontiguousarray(c.transpose(1, 0, 2).reshape(128, NCST * 128))
    T = n_ctx + n_lat
    tok = np.arange(n_lat)
    pr, pc = tok // 64, tok % 64
    inv = (10000.0 ** (-np.arange(16, dtype=np.float32) / 16)).astype(np.float32)
    cos = np.ones((64, T), np.float32)
    sin = np.zeros((64, T), np.float32)
    for base, pos in ((0, pr), (32, pc)):
        ang = pos.astype(np.float32)[None, :] * inv[:, None]
        cos[base:base + 16, n_ctx:] = np.cos(ang)
        cos[base + 16:base + 32, n_ctx:] = np.cos(ang)
        sin[base:base + 16, n_ctx:] = np.sin(ang)
        sin[base + 16:base + 32, n_ctx:] = np.sin(ang)
    return consts, np.concatenate([cos, sin], axis=0)


class MK:
    def __init__(self, n_ctx, n_lat, depth=2, dbg=False, upto=None, tiny_moe=False):
        self.n_ctx, self.n_lat, self.depth, self.dbg, self.upto = n_ctx, n_lat, depth, dbg, upto
        self.tiny_moe = tiny_moe
        self.T = n_ctx + n_lat
        self.NT = self.T // 128
        self.NCT = n_ctx // 128
        nc = self.nc = bass.Bass("TRN2", target_bir_lowering=False)
        self.P = Prog(nc)
        self.ins = {}
        self.uid = 0
        self.dumped = set()

    def din(self, name, shape):
        a = self.nc.dram_tensor(name, list(shape), F32, kind="ExternalInput").ap()
        self.ins[name] = a
        return a

    def dscr(self, name, shape, dt=F32):
        kind = "ExternalOutput" if self.dbg else "Internal"
        return self.nc.dram_tensor(name, list(shape), dt, kind=kind).ap()

    def sb(self, st, shape, dt=F32, name=None):
        self.uid += 1
        return Tl(st.enter_context(self.nc.sbuf_tensor(f"{name or 't'}{self.uid}", list(shape), dt)))

    def dump(self, name, tl, ap=None, dt=F32):
        if not self.dbg or name in self.dumped:
            return
        self.dumped.add(name)
        ap = tl.ap if ap is None else ap
        dst = self.nc.dram_tensor("dbg_" + name, list(ap.shape), dt, kind="ExternalOutput").ap()
        self.P.dma("sp", dst, ap, r=[tl])

    def take_banks(self, k):
        self.free_banks = list(range(8 - k))
        self.bank_rr = 0
        return [self.ps[8 - k + i] for i in range(k)]

    def release_banks(self):
        self.free_banks = list(range(8))
        self.bank_rr = 0

    def bank(self):
        self.bank_rr = (self.bank_rr + 1) % len(self.free_banks)
        return self.ps[self.free_banks[self.bank_rr]]

    def tok_blocks(self, t0, t1, bs=512):
        out = []
        while t0 < t1:
            n = min(bs, t1 - t0)
            out.append((t0, n))
            t0 += n
        return out

    def build(self):
        nc, P = self.nc, self.P
        T, NT = self.T, self.NT
        L = self.depth
        I = self.din
        self.x_in = I("x", [self.n_lat, D])
        self.ctx_in = I("ctx", [self.n_ctx, D])
        self.c_in = I("c", [128, KC])
        self.cctx_in = I("c_ctx", [128, KC])
        self.w_ada = I("w_ada", [L, D, 6 * D])
        self.b_ada = I("b_ada", [L, 6 * D])
        self.norm_g = I("norm_g", [L, 4, D])
        self.w_in = I("w_in", [L, D, DIN])
        self.ml_f_bias = I("ml_f_bias", [L, 8])
        self.hg_lb = I("hg_lb_logits", [L + 1, 512])
        self.hg_norm_g = I("hg_norm_g", [L, 512])
        self.hg_lbT = I("hg_lbT", [128, L + 1, 4])
        self.mla_g_q = I("mla_g_q", [L, 512])
        self.mla_g_kv = I("mla_g_kv", [L, 256])
        self.mla_w_uq = I("mla_w_uq", [L, 512, 768])
        self.mla_w_ukv = I("mla_w_ukv", [L, 256, 1024])
        self.ssd_conv_w = I("ssd_conv_w", [L, 128, 8, 3])
        self.ssd_conv_b = I("ssd_conv_b", [L, 128, 8])
        self.ssd_a_log = I("ssd_a_log", [L, 16])
        self.ssd_dt_bias = I("ssd_dt_bias", [L, 16])
        self.ssd_d = I("ssd_d", [L, 8])
        self.ssd_norm_g = I("ssd_norm_g", [L, 512])
        self.w_gate = I("w_gate", [L, 4, D, D])
        self.w_br = I("w_br", [L, 4, 512, D])
        self.w_out = I("w_out", [L, D, D])
        self.w_grp = I("moe_w_grp", [L, D, 4])
        self.w_exp = I("moe_w_exp", [L, D, 32])
        ne = 1 if self.tiny_moe else 32
        self.w1 = I("moe_w1", [L, ne, D, 512])
        self.w3 = I("moe_w3", [L, ne, D, 512])
        self.w2 = I("moe_w2", [L, ne, 512, D])
        self.consts_in = I("consts", [128, NCST * 128])
        self.rope_in = I("rope", [128, T])
        self.out = nc.dram_tensor("out", [self.n_lat, D], F32, kind="ExternalOutput").ap()

        S = self.dscr
        self.xres = S("xres", [T, D])
        self.modv = S("modv", [L, 2, 6, D])
        self.lbd = S("lbd", [L, 512])
        self.UT = S("UT", [T, DIN])
        self.UF = S("UF", [DIN, T])
        self.BR = S("BR", [4, T, 512])
        self.ACC = S("ACC", [D, T], BF16)
        self.Y = S("Y", [T, D])
        self.HTd = S("HTd", [128, KC, T], BF16)

        with ExitStack() as top:
            self.ps = [Tl(top.enter_context(nc.psum_tensor(f"ps{i}", [128, 512], F32))) for i in range(8)]
            for p_ in self.ps:
                p_.b.excl = True
            self.free_banks = list(range(8))
            self.bank_rr = 0
            self.cst = self.sb(top, [128, NCST, 128], F32, "cst")
            self.idb = self.sb(top, [128, 128], BF16, "idb")
            self.lbB = self.sb(top, [128, L, 512], F32, "lbB")
            P.dma("sp", self.cst.ap, self.consts_in.rearrange("p (c n) -> p c n", c=NCST), w=[self.cst])
            P.op("dve", lambda e: e.tensor_copy(out=self.idb.ap, in_=self.cst[:, C_ID, :]), r=[self.cst], w=[self.idb])
            P.dma("sp", self.xres[0:self.n_ctx, :], self.ctx_in)
            P.dma("sp", self.xres[self.n_ctx:T, :], self.x_in)
            steps = [("lb", lambda: self.setup_lb())]
            for l in range(L):
                steps.append((f"mod{l}", lambda l=l: self.phase_mod(l)))
            stages = ["norm1", "win", "ml", "hg", "mla", "ssd", "merge", "wout", "moe", "fin"]
            for l in range(L):
                for st_ in stages:
                    steps.append(((l, st_), lambda l=l, st_=st_: getattr(self, "phase_" + st_)(l)))
            P.barrier()
            if self.upto != "init":
                for name, fn in steps:
                    fn()
                    P.barrier()
                    if self.upto == name:
                        break
            P.dma("sp", self.out, self.xres[self.n_ctx:T, :])
            P.barrier()
        return nc

    def setup_lb(self):
        P, L = self.P, self.depth
        with ExitStack() as st:
            lg = self.sb(st, [128, L + 1, 512])
            ex = self.sb(st, [128, L + 1, 512])
            sm = self.sb(st, [128, 512])
            rc = self.sb(st, [128, 512])
            cum = self.sb(st, [128, 512])
            for j in range(L + 1):
                P.dma("sp", lg[:, j, :], self.hg_lb[j:j + 1, :].broadcast_to([128, 512]), w=[lg])
            P.op("act", lambda e: e.activation(out=ex.ap, in_=lg.ap, func=AF.Exp), r=[lg], w=[ex])
            P.op("dve", lambda e: e.tensor_tensor(out=sm.ap, in0=ex[:, 0, :], in1=ex[:, 1, :], op=ALU.add), r=[ex], w=[sm])
            for j in range(2, L + 1):
                P.op("dve", lambda e: e.tensor_tensor(out=sm.ap, in0=sm.ap, in1=ex[:, j, :], op=ALU.add), r=[ex, sm], w=[sm])
            P.op("dve", lambda e: e.reciprocal(out=rc.ap, in_=sm.ap), r=[sm], w=[rc])
            for l in range(L):
                if l == 0:
                    P.op("dve", lambda e: e.tensor_copy(out=cum.ap, in_=ex[:, 0, :]), r=[ex], w=[cum])
                else:
                    P.op("dve", lambda e: e.tensor_tensor(out=cum.ap, in0=cum.ap, in1=ex[:, l, :], op=ALU.add), r=[ex, cum], w=[cum])
                P.op("dve", lambda e: e.tensor_tensor(out=self.lbB[:, l, :], in0=cum.ap, in1=rc.ap, op=ALU.mult), r=[cum, rc], w=[self.lbB])
                P.dma("sp", self.lbd[l:l + 1, :], self.lbB[0:1, l, :], r=[self.lbB])
            P.barrier()

    def phase_mod(self, l):
        P, nc = self.P, self.nc
        with ExitStack() as st:
            cv = self.sb(st, [128, 2, KC])
            cs = self.sb(st, [128, 2, KC])
            SB = self.sb(st, [128, 2, KC, 128])
            bb = [self.sb(st, [1, 512]) for _ in range(2)]
            mo = [self.sb(st, [1, 2, 512]) for _ in range(2)]
            wst = [self.sb(st, [128, KC, 512]) for _ in range(2)]
            P.dma("sp", cv[:, 0, :], self.c_in, w=[cv])
            P.dma("sp", cv[:, 1, :], self.cctx_in, w=[cv])
            P.op("act", lambda e: e.activation(out=cs.ap, in_=cv.ap, func=AF.Silu), r=[cv], w=[cs])
            for j in range(2):
                for kc in range(KC):
                    P.op("dve", lambda e: e.tensor_copy(out=SB[:, j, kc, :], in_=cs[:, j, kc:kc + 1].broadcast_to([128, 128])), r=[cs], w=[SB])
            for ch in range(24):
                w_, b_, m_ = wst[ch % 2], bb[ch % 2], mo[ch % 2]
                P.dma("sp", w_.ap, self.w_ada[l, :, ch * 512:(ch + 1) * 512].rearrange("(k p) n -> p k n", p=128), w=[w_])
                P.dma("sp", b_.ap, self.b_ada[l:l + 1, ch * 512:(ch + 1) * 512], w=[b_])
                s_, c0 = divmod(ch * 512, D)
                for j in range(2):
                    pb = self.bank()
                    for kc in range(KC):
                        P.op("pe", lambda e: e.matmul(pb[:, :], lhsT=SB[:, j, kc, :], rhs=w_[:, kc, :], start=(kc == 0), stop=(kc == KC - 1)),
                             r=[SB, w_], w=[pb])
                    P.op("dve", lambda e: e.tensor_tensor(out=m_[:, j, :], in0=pb[0:1, :], in1=b_.ap, op=ALU.add), r=[pb, b_], w=[m_])
                P.dma("sp", self.modv[l:l + 1, :, s_, c0:c0 + 512], m_.ap, r=[m_])
            P.barrier()

    def load_modvec(self, st, l, kind):
        P = self.P
        mi, gi, add1 = {"A1": (1, 0, True), "B1": (0, None, False), "G2": (2, 1, False),
                        "A2": (4, 2, True), "B2": (3, None, False), "G4": (5, 3, False)}[kind]
        t = self.sb(st, [128, 2, D])
        for j in range(2):
            P.dma("sp", t[:, j, :], self.modv[l, j:j + 1, mi, :].broadcast_to([128, D]), w=[t])
        if gi is not None:
            with ExitStack() as s2:
                g = self.sb(s2, [128, D])
                P.dma("sp", g.ap, self.norm_g[l, gi:gi + 1, :].broadcast_to([128, D]), w=[g])
                for j in range(2):
                    if add1:
                        P.op("dve", lambda e: e.scalar_tensor_tensor(out=t[:, j, :], in0=t[:, j, :], scalar=1.0, in1=g.ap,
                                                                     op0=ALU.add, op1=ALU.mult), r=[t, g], w=[t])
                    else:
                        P.op("dve", lambda e: e.tensor_tensor(out=t[:, j, :], in0=t[:, j, :], in1=g.ap, op=ALU.mult), r=[t, g], w=[t])
                P.barrier()
        return t

    def rstd_of(self, src_ap, src_dep, n, junk, ss):
        P = self.P
        P.op("act", lambda e: e.activation(out=junk, in_=src_ap, func=AF.Square, accum_out=ss[:, 0:1]), r=src_dep, w=[ss])
        P.op("dve", lambda e: e.tensor_scalar(out=ss[:, 1:2], in0=ss[:, 0:1], scalar1=1.0 / n, scalar2=EPS, op0=ALU.mult, op1=ALU.add), r=[ss], w=[ss])
        P.op("act", lambda e: e.activation(out=ss[:, 2:3], in_=ss[:, 1:2], func=AF.Sqrt), r=[ss], w=[ss])
        P.op("dve", lambda e: e.reciprocal(out=ss[:, 3:4], in_=ss[:, 2:3]), r=[ss], w=[ss])

    def transpose_to(self, src, ncol, dst_fn, r_extra=()):
        P = self.P
        nblk = ncol // 128
        for q in range(0, nblk, 4):
            nb = min(4, nblk - q)
            pb = self.bank()
            for j in range(nb):
                P.op("pe", lambda e: e.matmul(pb[:, j * 128:(j + 1) * 128], lhsT=src[:, (q + j) * 128:(q + j + 1) * 128], rhs=self.idb.ap,
                                              start=True, stop=True), r=[src, self.idb], w=[pb])
            dst_fn(q, nb, pb)

    def norm_mod_to_hT(self, l, ia, ib):
        P = self.P
        with ExitStack() as st:
            A = self.load_modvec(st, l, ia)
            B = self.load_modvec(st, l, ib)
            xt = [self.sb(st, [128, D]) for _ in range(2)]
            tmp = self.sb(st, [128, D])
            hb = [self.sb(st, [128, D], BF16) for _ in range(2)]
            junk = self.sb(st, [128, D], BF16)
            ss = [self.sb(st, [128, 4]) for _ in range(2)]
            self.hT = self.sb(st, [128, KC, self.T], BF16, "hT")
            for i in range(self.NT):
                j = 1 if i < self.NCT else 0
                x_, h_, s_ = xt[i % 2], hb[i % 2], ss[i % 2]
                P.dma("sp", x_.ap, self.xres[i * 128:(i + 1) * 128, :], w=[x_])
                self.rstd_of(x_.ap, [x_], D, junk.ap, s_)
                P.op("dve", lambda e: e.scalar_tensor_tensor(out=tmp.ap, in0=x_.ap, scalar=s_[:, 3:4], in1=A[:, j, :], op0=ALU.mult, op1=ALU.mult),
                     r=[x_, s_, A], w=[tmp])
                P.op("pool", lambda e: e.tensor_tensor(out=h_.ap, in0=tmp.ap, in1=B[:, j, :], op=ALU.add), r=[tmp, B], w=[h_])

                def dst(q, nb, pb, i=i):
                    P.op("act", lambda e: e.copy(out=self.hT[:, q:q + nb, i * 128:(i + 1) * 128],
                                                 in_=pb[:, 0:nb * 128].rearrange("p (a t) -> p a t", a=nb)), r=[pb], w=[self.hT])
                self.transpose_to(h_, D, dst)
            P.dma("sp", self.HTd, self.hT.ap, r=[self.hT])
            P.barrier()

    def load_hT(self, st):
        self.hT = self.sb(st, [128, KC, self.T], BF16, "hT")
        self.P.dma("sp", self.hT.ap, self.HTd, w=[self.hT])

    def phase_norm1(self, l):
        self.norm_mod_to_hT(l, "A1", "B1")

    def wpipe(self, st, kc, ncol, nbuf=2):
        return {"stg": [self.sb(st, [128, kc, ncol]) for _ in range(nbuf)],
                "wb": [self.sb(st, [128, kc, ncol], BF16) for _ in range(nbuf)], "i": 0, "n": nbuf}

    def wload(self, wp, dram_ap, kc, ncol):
        P = self.P
        i = wp["i"] % wp["n"]
        wp["i"] += 1
        s_, b_ = wp["stg"][i], wp["wb"][i]
        P.dma("sp", s_[:, 0:kc, 0:ncol], dram_ap.rearrange("(k p) n -> p k n", p=128), w=[s_])
        P.op("pool", lambda e: e.tensor_copy(out=b_[:, 0:kc, 0:ncol], in_=s_[:, 0:kc, 0:ncol]), r=[s_], w=[b_])
        return b_

    def phase_win(self, l):
        P = self.P
        T = self.T
        groups = [(O_MLQ, 512, "F"), (O_MLK, 512, "FT"), (O_MLV, 512, "T"), (O_MLO, 512, "T"), (O_MLG, 16, "T"),
                  (O_HGQ, 512, "F"), (O_HGI, 512, "T"), (O_HGG, 512, "T"), (O_HGF, 1024, "FT"),
                  (O_CQ, 512, "T"), (O_CKV, 256, "T"), (O_KR, 64, "F"),
                  (O_SZ, 512, "T"), (O_XBC, 1024, "F"), (O_DT, 16, "T")]
        with ExitStack() as st:
            self.load_hT(st)
            wp = self.wpipe(st, KC, 256)
            ev = [self.sb(st, [128, 512]) for _ in range(4)]
            ei = 0
            for (c0, n, mode) in groups:
                for cc in range(c0, c0 + n, 256):
                    nn = min(256, c0 + n - cc)
                    wb = self.wload(wp, self.w_in[l, :, cc:cc + nn], KC, nn)
                    if "F" in mode:
                        for m0 in range(0, nn, 128):
                            mm = min(128, nn - m0)
                            for (t0, nt) in self.tok_blocks(0, T):
                                pb = self.bank()
                                for kc in range(KC):
                                    P.op("pe", lambda e: e.matmul(pb[0:mm, 0:nt], lhsT=wb[:, kc, m0:m0 + mm], rhs=self.hT[:, kc, t0:t0 + nt],
                                                                  start=(kc == 0), stop=(kc == KC - 1)), r=[wb, self.hT], w=[pb])
                                e_ = ev[ei % 4]
                                eng = "act" if ei % 2 == 0 else "dve"
                                ei += 1
                                if eng == "act":
                                    P.op("act", lambda e: e.copy(out=e_[0:mm, 0:nt], in_=pb[0:mm, 0:nt]), r=[pb], w=[e_])
                                else:
                                    P.op("dve", lambda e: e.tensor_copy(out=e_[0:mm, 0:nt], in_=pb[0:mm, 0:nt]), r=[pb], w=[e_])
                                P.dma("sp", self.UF[cc + m0:cc + m0 + mm, t0:t0 + nt], e_[0:mm, 0:nt], r=[e_])
                    if "T" in mode:
                        for i in range(self.NT):
                            pb = self.bank()
                            for kc in range(KC):
                                P.op("pe", lambda e: e.matmul(pb[:, 0:nn], lhsT=self.hT[:, kc, i * 128:(i + 1) * 128], rhs=wb[:, kc, 0:nn],
                                                              start=(kc == 0), stop=(kc == KC - 1)), r=[wb, self.hT], w=[pb])
                            e_ = ev[ei % 4]
                            eng = "act" if ei % 2 == 0 else "dve"
                            ei += 1
                            if eng == "act":
                                P.op("act", lambda e: e.copy(out=e_[:, 0:nn], in_=pb[:, 0:nn]), r=[pb], w=[e_])
                            else:
                                P.op("dve", lambda e: e.tensor_copy(out=e_[:, 0:nn], in_=pb[:, 0:nn]), r=[pb], w=[e_])
                            P.dma("sp", self.UT[i * 128:(i + 1) * 128, cc:cc + nn], e_[:, 0:nn], r=[e_])

    def order(self, d):
        c = list(range(self.NCT))
        la = list(range(self.NCT, self.NT))
        return c + la if d == 0 else c[::-1] + la[::-1]

    def decay_scalars(self, sc, lf_ap, lf_dep, n, d):
        P = self.P
        tri = C_TRF if d == 0 else C_TRB
        pb = self.bank()
        P.op("pe", lambda e: e.matmul(pb[:, 0:n], lhsT=self.cst[:, tri, :], rhs=lf_ap, start=True, stop=True), r=[self.cst] + lf_dep, w=[pb])
        P.op("pe", lambda e: e.matmul(pb[:, n:2 * n], lhsT=self.cst[:, C_ONE, :], rhs=lf_ap, start=True, stop=True), r=[self.cst] + lf_dep, w=[pb])
        P.op("dve", lambda e: e.tensor_copy(out=sc[:, 0:2 * n], in_=pb[:, 0:2 * n]), r=[pb], w=[sc])
        P.op("dve", lambda e: e.tensor_scalar(out=sc[:, 2 * n:3 * n], in0=sc[:, 0:n], scalar1=-1.0, scalar2=None, op0=ALU.mult), r=[sc], w=[sc])
        P.op("dve", lambda e: e.tensor_tensor(out=sc[:, 3 * n:4 * n], in0=sc[:, n:2 * n], in1=sc[:, 0:n], op=ALU.subtract), r=[sc], w=[sc])
        P.op("act", lambda e: e.activation(out=sc[:, 3 * n:4 * n], in_=sc[:, 3 * n:4 * n], func=AF.Exp), r=[sc], w=[sc])
        P.op("act", lambda e: e.activation(out=sc[:, 4 * n:6 * n], in_=sc[:, 0:2 * n], func=AF.Exp), r=[sc], w=[sc])

    def decay_matrix(self, lf_col, lf_dep, bias_col, bias_dep, d, lfB, Dm):
        P = self.P
        tri = C_TRF if d == 0 else C_TRB
        mn = C_MNF if d == 0 else C_MNB
        P.op("dve", lambda e: e.tensor_copy(out=lfB.ap, in_=lf_col.broadcast_to([128, 128])), r=lf_dep, w=[lfB])
        pb = self.bank()
        P.op("pe", lambda e: e.matmul(pb[:, 0:128], lhsT=lfB.ap, rhs=self.cst[:, tri, :], start=True, stop=False), r=[lfB, self.cst], w=[pb])
        P.op("pe", lambda e: e.matmul(pb[:, 0:128], lhsT=self.cst[:, C_ID, :], rhs=self.cst[:, mn, :], start=False, stop=True), r=[self.cst], w=[pb])
        P.op("act", lambda e: e.activation(out=Dm.ap, in_=pb[:, 0:128], func=AF.Exp, bias=bias_col, scale=1.0), r=[pb] + bias_dep, w=[Dm])

    def phase_ml(self, l):
        P = self.P
        with ExitStack() as st:
            bias = self.sb(st, [128, 8])
            yf = self.sb(st, [128, self.NT, 512])
            Cs = self.sb(st, [128, 4, 132])
            Cb = self.sb(st, [128, 4, 132], BF16)
            qT = self.sb(st, [128, 4, 128]); kT = self.sb(st, [128, 4, 128])
            qTb = self.sb(st, [128, 4, 128], BF16); kTb = self.sb(st, [128, 4, 128], BF16)
            kt = self.sb(st, [128, 512]); vt = self.sb(st, [128, 512]); ot = self.sb(st, [128, 512])
            vaug = self.sb(st, [128, 4, 132], BF16)
            gt = self.sb(st, [128, 16]); xg = self.sb(st, [128, 4]); lf = self.sb(st, [128, 4]); ab = self.sb(st, [128, 8])
            sc = self.sb(st, [128, 24])
            lfB = self.sb(st, [128, 128]); Dm = self.sb(st, [128, 128]); PT = self.sb(st, [128, 128], BF16)
            tmpI = self.sb(st, [128, 132]); tot = self.sb(st, [128, 132]); dn = self.sb(st, [128, 2])
            kw = self.sb(st, [128, 128], BF16)
            ys = self.sb(st, [128, 512]); sg = self.sb(st, [128, 512])
            P.dma("sp", bias.ap, self.ml_f_bias[l:l + 1, :].broadcast_to([128, 8]), w=[bias])
            P.op("pool", lambda e: e.memset(vaug.ap, 1.0), w=[vaug])
            for d in range(2):
                P.op("dve", lambda e: e.memset(Cs.ap, 0.0), w=[Cs])
                P.op("pool", lambda e: e.memset(Cb.ap, 0.0), w=[Cb])
                for i in self.order(d):
                    ts = slice(i * 128, (i + 1) * 128)
                    P.dma("sp", qT.ap, self.UF[O_MLQ:O_MLQ + 512, ts].rearrange("(h p) t -> p h t", p=128), w=[qT])
                    P.dma("sp", kT.ap, self.UF[O_MLK:O_MLK + 512, ts].rearrange("(h p) t -> p h t", p=128), w=[kT])
                    P.dma("sp", kt.ap, self.UT[ts, O_MLK:O_MLK + 512], w=[kt])
                    P.dma("sp", vt.ap, self.UT[ts, O_MLV:O_MLV + 512], w=[vt])
                    P.dma("sp", gt.ap, self.UT[ts, O_MLG:O_MLG + 16], w=[gt])
                    if d == 1:
                        P.dma("sp", ot.ap, self.UT[ts, O_MLO:O_MLO + 512], w=[ot])
                    P.op("act", lambda e: e.mul(out=qTb.ap, in_=qT.ap, mul=128.0 ** -0.5), r=[qT], w=[qTb])
                    P.op("pool", lambda e: e.tensor_copy(out=kTb.ap, in_=kT.ap), r=[kT], w=[kTb])
                    P.op("pool", lambda e: e.tensor_copy(out=vaug[:, :, 0:128], in_=vt.ap.rearrange("p (h d) -> p h d", h=4)), r=[vt], w=[vaug])
                    P.op("dve", lambda e: e.tensor_tensor(out=xg.ap, in0=gt[:, d * 8 + 4:d * 8 + 8], in1=bias[:, d * 4:d * 4 + 4], op=ALU.add), r=[gt, bias], w=[xg])
                    P.op("act", lambda e: e.activation(out=xg.ap, in_=xg.ap, func=AF.Exp, scale=-1.0), r=[xg], w=[xg])
                    P.op("act", lambda e: e.activation(out=xg.ap, in_=xg.ap, func=AF.Ln, bias=1.0), r=[xg], w=[xg])
                    P.op("dve", lambda e: e.tensor_scalar(out=lf.ap, in0=xg.ap, scalar1=-1.0, scalar2=None, op0=ALU.mult), r=[xg], w=[lf])
                    self.decay_scalars(sc, lf.ap, [lf], 4, d)
                    P.op("dve", lambda e: e.tensor_tensor(out=ab[:, 0:4], in0=gt[:, d * 8:d * 8 + 4], in1=sc[:, 0:4], op=ALU.subtract), r=[gt, sc], w=[ab])
                    P.op("dve", lambda e: e.tensor_tensor(out=ab[:, 4:8], in0=ab[:, 0:4], in1=sc[:, 4:8], op=ALU.add), r=[ab, sc], w=[ab])
                    P.op("act", lambda e: e.activation(out=ab[:, 4:8], in_=ab[:, 4:8], func=AF.Exp), r=[ab], w=[ab])
                    for h in range(4):
                        hs = slice(h * 128, (h + 1) * 128)
                        pS = self.bank()
                        P.op("pe", lambda e: e.matmul(pS[:, 0:128], lhsT=kTb[:, h, :], rhs=qTb[:, h, :], start=True, stop=True), r=[kTb, qTb], w=[pS])
                        self.decay_matrix(lf[:, h:h + 1], [lf], ab[:, h:h + 1], [ab], d, lfB, Dm)
                        P.op("dve", lambda e: e.tensor_tensor(out=PT.ap, in0=pS[:, 0:128], in1=Dm.ap, op=ALU.mult), r=[pS, Dm], w=[PT])
                        pO = self.bank()
                        P.op("pe", lambda e: e.matmul(pO[:, 0:129], lhsT=PT.ap, rhs=vaug[:, h, 0:129], start=True, stop=True), r=[PT, vaug], w=[pO])
                        pI = self.bank()
                        P.op("pe", lambda e: e.matmul(pI[:, 0:129], lhsT=qTb[:, h, :], rhs=Cb[:, h, 0:129], start=True, stop=True), r=[qTb, Cb], w=[pI])
                        P.op("dve", lambda e: e.tensor_scalar(out=tmpI[:, 0:129], in0=pI[:, 0:129], scalar1=sc[:, 16 + h:17 + h], scalar2=None, op0=ALU.mult), r=[pI, sc], w=[tmpI])
                        P.op("dve", lambda e: e.tensor_tensor(out=tot[:, 0:129], in0=pO[:, 0:129], in1=tmpI[:, 0:129], op=ALU.add), r=[pO, tmpI], w=[tot])
                        P.op("act", lambda e: e.activation(out=dn[:, 0:1], in_=tot[:, 128:129], func=AF.Abs), r=[tot], w=[dn])
                        P.op("dve", lambda e: e.tensor_scalar(out=dn[:, 0:1], in0=dn[:, 0:1], scalar1=1.0, scalar2=None, op0=ALU.max), r=[dn], w=[dn])
                        P.op("dve", lambda e: e.reciprocal(out=dn[:, 1:2], in_=dn[:, 0:1]), r=[dn], w=[dn])
                        if d == 0:
                            P.op("dve", lambda e: e.tensor_scalar(out=yf[:, i, hs], in0=tot[:, 0:128], scalar1=dn[:, 1:2], scalar2=None, op0=ALU.mult), r=[tot, dn], w=[yf])
                        else:
                            P.op("dve", lambda e: e.scalar_tensor_tensor(out=ys[:, hs], in0=tot[:, 0:128], scalar=dn[:, 1:2], in1=yf[:, i, hs], op0=ALU.mult, op1=ALU.add),
                                 r=[tot, dn, yf], w=[ys])
                        P.op("pool", lambda e: e.tensor_scalar(out=kw.ap, in0=kt[:, hs], scalar1=ab[:, 4 + h:5 + h], scalar2=None, op0=ALU.mult), r=[kt, ab], w=[kw])
                        pU = self.bank()
                        P.op("pe", lambda e: e.matmul(pU[:, 0:129], lhsT=kw.ap, rhs=vaug[:, h, 0:129], start=True, stop=True), r=[kw, vaug], w=[pU])
                        P.op("dve", lambda e: e.scalar_tensor_tensor(out=Cs[:, h, 0:129], in0=Cs[:, h, 0:129], scalar=sc[:, 20 + h:21 + h], in1=pU[:, 0:129], op0=ALU.mult, op1=ALU.add),
                             r=[Cs, sc, pU], w=[Cs])
                        P.op("act", lambda e: e.copy(out=Cb[:, h, 0:129], in_=Cs[:, h, 0:129]), r=[Cs], w=[Cb])
                    if d == 1:
                        P.op("act", lambda e: e.activation(out=sg.ap, in_=ot.ap, func=AF.Sigmoid), r=[ot], w=[sg])
                        P.op("dve", lambda e: e.tensor_tensor(out=sg.ap, in0=sg.ap, in1=ys.ap, op=ALU.mult), r=[sg, ys], w=[sg])
                        P.dma("sp", self.BR[0, ts, :], sg.ap, r=[sg])

    def phase_ssd(self, l):
        P = self.P
        T, NCX = self.T, self.n_ctx
        with ExitStack() as st:
            xbcT = self.sb(st, [128, 8, T], BF16)
            cw = self.sb(st, [128, 8, 3]); cb = self.sb(st, [128, 8])
            dtb = self.sb(st, [128, 16]); aneg = self.sb(st, [128, 16]); dsk = self.sb(st, [128, 8]); ng = self.sb(st, [128, 512])
            P.dma("sp", cw.ap, self.ssd_conv_w[l], w=[cw])
            P.dma("sp", cb.ap, self.ssd_conv_b[l], w=[cb])
            P.dma("sp", dtb.ap, self.ssd_dt_bias[l:l + 1, :].broadcast_to([128, 16]), w=[dtb])
            P.dma("sp", aneg.ap, self.ssd_a_log[l:l + 1, :].broadcast_to([128, 16]), w=[aneg])
            P.dma("sp", dsk.ap, self.ssd_d[l:l + 1, :].broadcast_to([128, 8]), w=[dsk])
            P.dma("sp", ng.ap, self.ssd_norm_g[l:l + 1, :].broadcast_to([128, 512]), w=[ng])
            P.op("act", lambda e: e.activation(out=aneg.ap, in_=aneg.ap, func=AF.Exp), r=[aneg], w=[aneg])
            P.op("dve", lambda e: e.tensor_scalar(out=aneg.ap, in0=aneg.ap, scalar1=-1.0, scalar2=None, op0=ALU.mult), r=[aneg], w=[aneg])
            with ExitStack() as s2:
                xin = [self.sb(s2, [128, T]) for _ in range(2)]
                acc = self.sb(s2, [128, T])
                for g in range(8):
                    x_ = xin[g % 2]
                    P.dma("sp", x_.ap, self.UF[O_XBC + g * 128:O_XBC + (g + 1) * 128, :], w=[x_])
                    P.op("dve", lambda e: e.tensor_scalar(out=acc.ap, in0=x_.ap, scalar1=cw[:, g, 1:2], scalar2=None, op0=ALU.mult), r=[x_, cw], w=[acc])
                    for (s0, s1) in ((0, NCX), (NCX, T)):
                        P.op("dve", lambda e: e.scalar_tensor_tensor(out=acc[:, s0 + 1:s1], in0=x_[:, s0:s1 - 1], scalar=cw[:, g, 0:1], in1=acc[:, s0 + 1:s1],
                                                                     op0=ALU.mult, op1=ALU.add), r=[x_, cw, acc], w=[acc])
                        P.op("dve", lambda e: e.scalar_tensor_tensor(out=acc[:, s0:s1 - 1], in0=x_[:, s0 + 1:s1], scalar=cw[:, g, 2:3], in1=acc[:, s0:s1 - 1],
                                                                     op0=ALU.mult, op1=ALU.add), r=[x_, cw, acc], w=[acc])
                    P.op("act", lambda e: e.activation(out=xbcT[:, g, :], in_=acc.ap, func=AF.Silu, bias=cb[:, g:g + 1], scale=1.0), r=[acc, cb], w=[xbcT])
                P.barrier()
            yf = self.sb(st, [128, self.NT, 512])
            Hs = self.sb(st, [128, 2, 256]); Hb = self.sb(st, [128, 2, 256], BF16)
            xtok = self.sb(st, [128, 512]); Btok = self.sb(st, [128, 256], BF16)
            xdt = self.sb(st, [128, 8, 64], BF16); xdtw = self.sb(st, [128, 8, 64], BF16)
            gt = self.sb(st, [128, 8]); dt = self.sb(st, [128, 8]); da = self.sb(st, [128, 8])
            sc = self.sb(st, [128, 48])
            lfB = self.sb(st, [128, 128]); Dm = self.sb(st, [128, 128]); PT = self.sb(st, [128, 128], BF16)
            tmpI = self.sb(st, [128, 512]); yd = self.sb(st, [128, 512]); zt = self.sb(st, [128, 512])
            junk = self.sb(st, [128, 512], BF16); ss = self.sb(st, [128, 4])
            pS, pI, pO = self.take_banks(3)
            for d in range(2):
                P.op("dve", lambda e: e.memset(Hs.ap, 0.0), w=[Hs])
                P.op("pool", lambda e: e.memset(Hb.ap, 0.0), w=[Hb])
                for i in self.order(d):
                    ts = slice(i * 128, (i + 1) * 128)
                    pb = self.bank()
                    for g in range(4):
                        P.op("pe", lambda e: e.matmul(pb[:, g * 128:(g + 1) * 128], lhsT=xbcT[:, g, ts], rhs=self.idb.ap, start=True, stop=True), r=[xbcT, self.idb], w=[pb])
                    P.op("act", lambda e: e.copy(out=xtok.ap, in_=pb[:, :]), r=[pb], w=[xtok])
                    pb = self.bank()
                    for g in range(2):
                        P.op("pe", lambda e: e.matmul(pb[:, g * 128:(g + 1) * 128], lhsT=xbcT[:, 4 + g, ts], rhs=self.idb.ap, start=True, stop=True), r=[xbcT, self.idb], w=[pb])
                    P.op("act", lambda e: e.copy(out=Btok.ap, in_=pb[:, 0:256]), r=[pb], w=[Btok])
                    P.dma("sp", gt.ap, self.UT[ts, O_DT + d * 8:O_DT + d * 8 + 8], w=[gt])
                    P.op("dve", lambda e: e.tensor_tensor(out=dt.ap, in0=gt.ap, in1=dtb[:, d * 8:d * 8 + 8], op=ALU.add), r=[gt, dtb], w=[dt])
                    P.op("act", lambda e: e.activation(out=dt.ap, in_=dt.ap, func=AF.Exp), r=[dt], w=[dt])
                    P.op("act", lambda e: e.activation(out=dt.ap, in_=dt.ap, func=AF.Ln, bias=1.0), r=[dt], w=[dt])
                    P.op("dve", lambda e: e.tensor_tensor(out=da.ap, in0=dt.ap, in1=aneg[:, d * 8:d * 8 + 8], op=ALU.mult), r=[dt, aneg], w=[da])
                    self.decay_scalars(sc, da.ap, [da], 8, d)
                    P.op("dve", lambda e: e.tensor_tensor(out=xdt.ap, in0=xtok.ap.rearrange("p (h c) -> p h c", h=8), in1=dt.ap.unsqueeze(2).broadcast_to([128, 8, 64]), op=ALU.mult),
                         r=[xtok, dt], w=[xdt])
                    P.op("pool", lambda e: e.tensor_tensor(out=xdtw.ap, in0=xdt.ap, in1=sc[:, 24:32].unsqueeze(2).broadcast_to([128, 8, 64]), op=ALU.mult), r=[xdt, sc], w=[xdtw])
                    for g in range(2):
                        P.op("pe", lambda e: e.matmul(pS[:, g * 128:(g + 1) * 128], lhsT=xbcT[:, 4 + g, ts], rhs=xbcT[:, 6 + g, ts], start=True, stop=True), r=[xbcT], w=[pS])
                        P.op("pe", lambda e: e.matmul(pI[:, g * 256:(g + 1) * 256], lhsT=xbcT[:, 6 + g, ts], rhs=Hb[:, g, :], start=True, stop=True), r=[xbcT, Hb], w=[pI])
                    P.op("dve", lambda e: e.tensor_tensor(out=tmpI.ap.rearrange("p (h c) -> p h c", h=8), in0=pI[:, :].rearrange("p (h c) -> p h c", h=8),
                                                          in1=sc[:, 32:40].unsqueeze(2).broadcast_to([128, 8, 64]), op=ALU.mult), r=[pI, sc], w=[tmpI])
                    for j in range(8):
                        g = j // 4
                        self.decay_matrix(da[:, j:j + 1], [da], sc[:, 16 + j:17 + j], [sc], d, lfB, Dm)
                        P.op("dve", lambda e: e.tensor_tensor(out=PT.ap, in0=pS[:, g * 128:(g + 1) * 128], in1=Dm.ap, op=ALU.mult), r=[pS, Dm], w=[PT])
                        P.op("pe", lambda e: e.matmul(pO[:, j * 64:(j + 1) * 64], lhsT=PT.ap, rhs=xdt[:, j, :], start=True, stop=True), r=[PT, xdt], w=[pO])
                    if d == 0:
                        P.op("dve", lambda e: e.tensor_tensor(out=yf[:, i, :], in0=pO[:, :], in1=tmpI.ap, op=ALU.add), r=[pO, tmpI], w=[yf])
                    else:
                        P.op("dve", lambda e: e.tensor_tensor(out=yd.ap, in0=pO[:, :], in1=tmpI.ap, op=ALU.add), r=[pO, tmpI], w=[yd])
                    pU = self.bank()
                    for g in range(2):
                        P.op("pe", lambda e: e.matmul(pU[:, g * 256:(g + 1) * 256], lhsT=Btok[:, g * 128:(g + 1) * 128], rhs=xdtw[:, g * 4:(g + 1) * 4, :].rearrange("p h c -> p (h c)"),
                                                      start=True, stop=True), r=[Btok, xdtw], w=[pU])
                    P.op("dve", lambda e: e.tensor_tensor(out=Hs.ap.rearrange("p g (h c) -> p (g h) c", h=4), in0=Hs.ap.rearrange("p g (h c) -> p (g h) c", h=4),
                                                          in1=sc[:, 40:48].unsqueeze(2).broadcast_to([128, 8, 64]), op=ALU.mult), r=[Hs, sc], w=[Hs])
                    P.op("dve", lambda e: e.tensor_tensor(out=Hs.ap.rearrange("p g c -> p (g c)"), in0=Hs.ap.rearrange("p g c -> p (g c)"), in1=pU[:, :], op=ALU.add), r=[Hs, pU], w=[Hs])
                    P.op("act", lambda e: e.copy(out=Hb.ap, in_=Hs.ap), r=[Hs], w=[Hb])
                    if d == 1:
                        P.dma("sp", zt.ap, self.UT[ts, O_SZ:O_SZ + 512], w=[zt])
                        P.op("dve", lambda e: e.tensor_tensor(out=yd.ap, in0=yd.ap, in1=yf[:, i, :], op=ALU.add), r=[yd, yf], w=[yd])
                        P.op("dve", lambda e: e.tensor_tensor(out=tmpI.ap.rearrange("p (h c) -> p h c", h=8), in0=xtok.ap.rearrange("p (h c) -> p h c", h=8),
                                                              in1=dsk.ap.unsqueeze(2).broadcast_to([128, 8, 64]), op=ALU.mult), r=[xtok, dsk], w=[tmpI])
                        P.op("dve", lambda e: e.tensor_tensor(out=yd.ap, in0=yd.ap, in1=tmpI.ap, op=ALU.add), r=[yd, tmpI], w=[yd])
                        P.op("act", lambda e: e.activation(out=zt.ap, in_=zt.ap, func=AF.Silu), r=[zt], w=[zt])
                        P.op("dve", lambda e: e.tensor_tensor(out=yd.ap, in0=yd.ap, in1=zt.ap, op=ALU.mult), r=[yd, zt], w=[yd])
                        self.rstd_of(yd.ap, [yd], 512, junk.ap, ss)
                        P.op("dve", lambda e: e.scalar_tensor_tensor(out=yd.ap, in0=yd.ap, scalar=ss[:, 3:4], in1=ng.ap, op0=ALU.mult, op1=ALU.mult), r=[yd, ss, ng], w=[yd])
                        P.dma("sp", self.BR[3, ts, :], yd.ap, r=[yd])
            self.release_banks()

    def phase_hg(self, l):
        P = self.P
        L = self.depth
        with ExitStack() as st:
            lg = self.sb(st, [128, L + 1, 4]); lbT = self.sb(st, [128, 4]); omT = self.sb(st, [128, 4]); smT = self.sb(st, [128, 4])
            omB = self.sb(st, [128, 512]); ngB = self.sb(st, [128, 512])
            P.dma("sp", lg.ap, self.hg_lbT, w=[lg])
            P.dma("sp", ngB.ap, self.hg_norm_g[l:l + 1, :].broadcast_to([128, 512]), w=[ngB])
            P.op("act", lambda e: e.activation(out=lg.ap, in_=lg.ap, func=AF.Exp), r=[lg], w=[lg])
            P.op("dve", lambda e: e.tensor_tensor(out=smT.ap, in0=lg[:, 0, :], in1=lg[:, 1, :], op=ALU.add), r=[lg], w=[smT])
            for j in range(2, L + 1):
                P.op("dve", lambda e: e.tensor_tensor(out=smT.ap, in0=smT.ap, in1=lg[:, j, :], op=ALU.add), r=[lg, smT], w=[smT])
            P.op("dve", lambda e: e.reciprocal(out=smT.ap, in_=smT.ap), r=[smT], w=[smT])
            P.op("dve", lambda e: e.tensor_copy(out=lbT.ap, in_=lg[:, 0, :]), r=[lg], w=[lbT])
            for j in range(1, l + 1):
                P.op("dve", lambda e: e.tensor_tensor(out=lbT.ap, in0=lbT.ap, in1=lg[:, j, :], op=ALU.add), r=[lg, lbT], w=[lbT])
            P.op("dve", lambda e: e.tensor_tensor(out=lbT.ap, in0=lbT.ap, in1=smT.ap, op=ALU.mult), r=[lbT, smT], w=[lbT])
            P.op("dve", lambda e: e.tensor_scalar(out=omT.ap, in0=lbT.ap, scalar1=-1.0, scalar2=1.0, op0=ALU.mult, op1=ALU.add), r=[lbT], w=[omT])
            P.op("dve", lambda e: e.tensor_scalar(out=omB.ap, in0=self.lbB[:, l, :], scalar1=-1.0, scalar2=1.0, op0=ALU.mult, op1=ALU.add), r=[self.lbB], w=[omB])
            yf = self.sb(st, [128, self.NT, 512])
            Ss = self.sb(st, [128, 4, 128]); Sb = self.sb(st, [128, 4, 128], BF16)
            qT = self.sb(st, [128, 4, 128]); fT = self.sb(st, [128, 4, 128]); kTf = self.sb(st, [128, 4, 128])
            ft = self.sb(st, [128, 512]); lf = self.sb(st, [128, 512]); vt = self.sb(st, [128, 512]); vb = self.sb(st, [128, 512], BF16)
            E = self.sb(st, [128, 128]); Ei = self.sb(st, [128, 128]); ec = self.sb(st, [128, 4])
            qtl = self.sb(st, [128, 128], BF16); ktl = self.sb(st, [128, 128], BF16); ktok = self.sb(st, [128, 128], BF16)
            Af = self.sb(st, [128, 128]); AT = self.sb(st, [128, 128], BF16)
            ys = self.sb(st, [128, 512]); sq = self.sb(st, [128, 512]); rs = self.sb(st, [128, 8]); gtk = self.sb(st, [128, 512])
            for d in range(2):
                lc = C_LCF if d == 0 else C_LCB
                tri = C_TRF if d == 0 else C_TRB
                hc0 = 0 if d == 0 else 3
                P.op("dve", lambda e: e.memset(Ss.ap, 0.0), w=[Ss])
                for i in self.order(d):
                    ts = slice(i * 128, (i + 1) * 128)
                    P.dma("sp", qT.ap, self.UF[O_HGQ:O_HGQ + 512, ts].rearrange("(h p) t -> p h t", p=128), w=[qT])
                    P.dma("sp", fT.ap, self.UF[O_HGF + d * 512:O_HGF + (d + 1) * 512, ts].rearrange("(h p) t -> p h t", p=128), w=[fT])
                    P.dma("sp", ft.ap, self.UT[ts, O_HGF + d * 512:O_HGF + (d + 1) * 512], w=[ft])
                    P.dma("sp", vt.ap, self.UT[ts, O_HGI:O_HGI + 512], w=[vt])
                    P.op("act", lambda e: e.activation(out=qT.ap, in_=qT.ap, func=AF.Silu), r=[qT], w=[qT])
                    P.op("act", lambda e: e.activation(out=kTf.ap, in_=fT.ap, func=AF.Sigmoid, scale=-1.0), r=[fT], w=[kTf])
                    for h in range(4):
                        P.op("pool", lambda e: e.tensor_scalar(out=kTf[:, h, :], in0=kTf[:, h, :], scalar1=omT[:, h:h + 1], scalar2=None, op0=ALU.mult), r=[kTf, omT], w=[kTf])
                    P.op("act", lambda e: e.activation(out=ft.ap, in_=ft.ap, func=AF.Sigmoid), r=[ft], w=[ft])
                    P.op("dve", lambda e: e.tensor_tensor(out=ft.ap, in0=ft.ap, in1=omB.ap, op=ALU.mult), r=[ft, omB], w=[ft])
                    P.op("dve", lambda e: e.tensor_tensor(out=ft.ap, in0=ft.ap, in1=self.lbB[:, l, :], op=ALU.add), r=[ft, self.lbB], w=[ft])
                    P.op("act", lambda e: e.activation(out=lf.ap, in_=ft.ap, func=AF.Ln), r=[ft], w=[lf])
                    P.op("pool", lambda e: e.tensor_copy(out=vb.ap, in_=vt.ap), r=[vt], w=[vb])
                    for h in range(4):
                        hs = slice(h * 128, (h + 1) * 128)
                        pG = self.bank()
                        P.op("pe", lambda e: e.matmul(pG[:, 0:128], lhsT=lf[:, hs], rhs=self.cst[:, lc, :], start=True, stop=True), r=[lf, self.cst], w=[pG])
                        P.op("pe", lambda e: e.matmul(pG[:, 128:132], lhsT=lf[:, hs], rhs=self.cst[:, C_HC, hc0:hc0 + 4], start=True, stop=True), r=[lf, self.cst], w=[pG])
                        P.op("act", lambda e: e.activation(out=E.ap, in_=pG[:, 0:128], func=AF.Exp), r=[pG], w=[E])
                        P.op("act", lambda e: e.activation(out=Ei.ap, in_=pG[:, 0:128], func=AF.Exp, scale=-1.0), r=[pG], w=[Ei])
                        P.op("act", lambda e: e.activation(out=ec.ap, in_=pG[:, 128:132], func=AF.Exp), r=[pG], w=[ec])
                        P.op("dve", lambda e: e.tensor_tensor(out=qtl.ap, in0=qT[:, h, :], in1=E.ap, op=ALU.mult), r=[qT, E], w=[qtl])
                        P.op("dve", lambda e: e.tensor_tensor(out=ktl.ap, in0=kTf[:, h, :], in1=Ei.ap, op=ALU.mult), r=[kTf, Ei], w=[ktl])
                        P.op("dve", lambda e: e.tensor_scalar(out=Sb[:, h, :], in0=Ss[:, h, :], scalar1=ec[:, 0:1], scalar2=None, op0=ALU.mult), r=[Ss, ec], w=[Sb])
                        pA = self.bank()
                        P.op("pe", lambda e: e.matmul(pA[:, 0:128], lhsT=ktl.ap, rhs=qtl.ap, start=True, stop=True), r=[ktl, qtl], w=[pA])
                        P.op("dve", lambda e: e.tensor_scalar(out=Af.ap, in0=pA[:, 0:128], scalar1=-1e30, scalar2=1e30, op0=ALU.max, op1=ALU.min), r=[pA], w=[Af])
                        P.op("dve", lambda e: e.tensor_tensor(out=AT.ap, in0=Af.ap, in1=self.cst[:, tri, :], op=ALU.mult), r=[Af, self.cst], w=[AT])
                        pO = self.bank()
                        P.op("pe", lambda e: e.matmul(pO[:, 0:128], lhsT=AT.ap, rhs=vb[:, hs], start=True, stop=False), r=[AT, vb], w=[pO])
                        P.op("pe", lambda e: e.matmul(pO[:, 0:128], lhsT=qtl.ap, rhs=Sb[:, h, :], start=False, stop=True), r=[qtl, Sb], w=[pO])
                        if d == 0:
                            P.op("act", lambda e: e.copy(out=yf[:, i, hs], in_=pO[:, 0:128]), r=[pO], w=[yf])
                        else:
                            P.op("dve", lambda e: e.tensor_tensor(out=ys[:, hs], in0=pO[:, 0:128], in1=yf[:, i, hs], op=ALU.add), r=[pO, yf], w=[ys])
                        pK = self.bank()
                        P.op("pe", lambda e: e.matmul(pK[:, 0:128], lhsT=ktl.ap, rhs=self.idb.ap, start=True, stop=True), r=[ktl, self.idb], w=[pK])
                        P.op("act", lambda e: e.copy(out=ktok.ap, in_=pK[:, 0:128]), r=[pK], w=[ktok])
                        pU = self.bank()
                        P.op("pe", lambda e: e.matmul(pU[:, 0:128], lhsT=ktok.ap, rhs=vb[:, hs], start=True, stop=True), r=[ktok, vb], w=[pU])
                        P.op("dve", lambda e: e.tensor_scalar(out=Ss[:, h, :], in0=Ss[:, h, :], scalar1=ec[:, 1:2], scalar2=None, op0=ALU.mult), r=[Ss, ec], w=[Ss])
                        P.op("dve", lambda e: e.scalar_tensor_tensor(out=Ss[:, h, :], in0=pU[:, 0:128], scalar=ec[:, 2:3], in1=Ss[:, h, :], op0=ALU.mult, op1=ALU.add), r=[pU, ec, Ss], w=[Ss])
                    if d == 1:
                        P.dma("sp", gtk.ap, self.UT[ts, O_HGG:O_HGG + 512], w=[gtk])
                        P.op("dve", lambda e: e.tensor_tensor(out=sq.ap, in0=ys.ap, in1=ys.ap, op=ALU.mult), r=[ys], w=[sq])
                        P.op("dve", lambda e: e.tensor_reduce(out=rs[:, 0:4], in_=sq.ap.rearrange("p (h c) -> p h c", h=4), axis=AX.X, op=ALU.add), r=[sq], w=[rs])
                        P.op("dve", lambda e: e.tensor_scalar(out=rs[:, 0:4], in0=rs[:, 0:4], scalar1=1.0 / 128, scalar2=EPS, op0=ALU.mult, op1=ALU.add), r=[rs], w=[rs])
                        P.op("act", lambda e: e.activation(out=rs[:, 0:4], in_=rs[:, 0:4], func=AF.Sqrt), r=[rs], w=[rs])
                        P.op("dve", lambda e: e.reciprocal(out=rs[:, 4:8], in_=rs[:, 0:4]), r=[rs], w=[rs])
                        P.op("dve", lambda e: e.tensor_tensor(out=ys.ap.rearrange("p (h c) -> p h c", h=4), in0=ys.ap.rearrange("p (h c) -> p h c", h=4),
                                                              in1=rs[:, 4:8].unsqueeze(2).broadcast_to([128, 4, 128]), op=ALU.mult), r=[ys, rs], w=[ys])
                        P.op("dve", lambda e: e.tensor_tensor(out=ys.ap, in0=ys.ap, in1=ngB.ap, op=ALU.mult), r=[ys, ngB], w=[ys])
                        P.op("act", lambda e: e.activation(out=gtk.ap, in_=gtk.ap, func=AF.Sigmoid), r=[gtk], w=[gtk])
                        P.op("dve", lambda e: e.tensor_tensor(out=ys.ap, in0=ys.ap, in1=gtk.ap, op=ALU.mult), r=[ys, gtk], w=[ys])
                        P.dma("sp", self.BR[1, ts, :], ys.ap, r=[ys])

    def phase_mla(self, l):
        P = self.P
        T, NT, NCX = self.T, self.NT, self.n_ctx
        with ExitStack() as st:
            qnT = self.sb(st, [128, 4, T], BF16); qrT = self.sb(st, [128, 4, T], BF16)
            knT = self.sb(st, [128, 4, T], BF16); krT = self.sb(st, [128, T], BF16)
            vaug = self.sb(st, [128, NT, 4, 132], BF16)
            P.op("pool", lambda e: e.memset(vaug.ap, 1.0), w=[vaug])
            with ExitStack() as s2:
                gq = self.sb(s2, [128, 512]); gkv = self.sb(s2, [128, 256])
                P.dma("sp", gq.ap, self.mla_g_q[l:l + 1, :].broadcast_to([128, 512]), w=[gq])
                P.dma("sp", gkv.ap, self.mla_g_kv[l:l + 1, :].broadcast_to([128, 256]), w=[gkv])
                cqT = self.sb(s2, [128, 4, T], BF16); ckvT = self.sb(s2, [128, 2, T], BF16)
                cosT = self.sb(s2, [128, T]); sinT = self.sb(s2, [128, T])
                P.op("dve", lambda e: e.memset(cosT.ap, 0.0), w=[cosT])
                P.op("dve", lambda e: e.memset(sinT.ap, 0.0), w=[sinT])
                P.dma("sp", cosT[0:64, :], self.rope_in[0:64, :], w=[cosT])
                P.dma("sp", sinT[0:64, :], self.rope_in[64:128, :], w=[sinT])
                permb = self.sb(s2, [128, 128], BF16)
                P.op("dve", lambda e: e.tensor_copy(out=permb.ap, in_=self.cst[:, C_PERM, :]), r=[self.cst], w=[permb])
                wst = self.sb(s2, [128, 4, 768]); Wuq = self.sb(s2, [128, 4, 768], BF16)
                wst2 = self.sb(s2, [128, 2, 1024]); Wukv = self.sb(s2, [128, 2, 1024], BF16)
                Wr = self.sb(s2, [128, 4, 4, 128], BF16)
                P.dma("sp", wst.ap, self.mla_w_uq[l].rearrange("(k p) n -> p k n", p=128), w=[wst])
                P.dma("sp", wst2.ap, self.mla_w_ukv[l].rearrange("(k p) n -> p k n", p=128), w=[wst2])
                P.op("dve", lambda e: e.tensor_copy(out=Wuq.ap, in_=wst.ap), r=[wst], w=[Wuq])
                P.op("dve", lambda e: e.tensor_copy(out=Wukv.ap, in_=wst2.ap), r=[wst2], w=[Wukv])
                P.op("dve", lambda e: e.memset(Wr.ap, 0.0), w=[Wr])
                for h in range(4):
                    for kc in range(4):
                        P.op("dve", lambda e: e.tensor_copy(out=Wr[:, kc, h, 0:64], in_=wst[:, kc, h * 192 + 128:(h + 1) * 192]), r=[wst], w=[Wr])
                ct = self.sb(s2, [128, 768]); cb_ = self.sb(s2, [128, 768], BF16); junk = self.sb(s2, [128, 512], BF16)
                ss = self.sb(s2, [128, 4]); ss2 = self.sb(s2, [128, 4])
                for i in range(NT):
                    ts = slice(i * 128, (i + 1) * 128)
                    P.dma("sp", ct.ap, self.UT[ts, O_CQ:O_CQ + 768], w=[ct])
                    self.rstd_of(ct[:, 0:512], [ct], 512, junk.ap, ss)
                    self.rstd_of(ct[:, 512:768], [ct], 256, junk[:, 0:256], ss2)
                    P.op("dve", lambda e: e.scalar_tensor_tensor(out=cb_[:, 0:512], in0=ct[:, 0:512], scalar=ss[:, 3:4], in1=gq.ap, op0=ALU.mult, op1=ALU.mult), r=[ct, ss, gq], w=[cb_])
                    P.op("dve", lambda e: e.scalar_tensor_tensor(out=cb_[:, 512:768], in0=ct[:, 512:768], scalar=ss2[:, 3:4], in1=gkv.ap, op0=ALU.mult, op1=ALU.mult), r=[ct, ss2, gkv], w=[cb_])

                    def dst(q, nb, pb, i=i):
                        for j in range(nb):
                            blk = q + j
                            tgt = cqT[:, blk, i * 128:(i + 1) * 128] if blk < 4 else ckvT[:, blk - 4, i * 128:(i + 1) * 128]
                            P.op("act", lambda e: e.copy(out=tgt, in_=pb[:, j * 128:(j + 1) * 128]), r=[pb], w=[cqT if blk < 4 else ckvT])
                    self.transpose_to(cb_, 768, dst)
                if os.environ.get("MLA_STOP") == "A":
                    return
                xb = self.sb(s2, [128, 512], BF16); t1 = self.sb(s2, [128, 512]); t2 = self.sb(s2, [128, 512])
                xk = self.sb(s2, [128, T])

                def rope(src_ap, src_dep, out_ap, out_tl, t0, n):
                    P.op("act", lambda e: e.copy(out=xb[:, 0:n], in_=src_ap), r=src_dep, w=[xb])
                    pP = self.bank()
                    P.op("pe", lambda e: e.matmul(pP[:, 0:n], lhsT=permb.ap, rhs=xb[:, 0:n], start=True, stop=True), r=[permb, xb], w=[pP])
                    P.op("dve", lambda e: e.tensor_tensor(out=t1[:, 0:n], in0=src_ap, in1=cosT[:, t0:t0 + n], op=ALU.mult), r=src_dep + [cosT], w=[t1])
                    P.op("dve", lambda e: e.tensor_tensor(out=t2[:, 0:n], in0=pP[:, 0:n], in1=sinT[:, t0:t0 + n], op=ALU.mult), r=[pP, sinT], w=[t2])
                    P.op("dve", lambda e: e.tensor_tensor(out=out_ap, in0=t1[:, 0:n], in1=t2[:, 0:n], op=ALU.add), r=[t1, t2], w=[out_tl])

                P.op("dve", lambda e: e.memset(xk.ap, 0.0), w=[xk])
                P.dma("sp", xk[0:64, :], self.UF[O_KR:O_KR + 64, :], w=[xk])
                for (t0, n) in self.tok_blocks(0, T):
                    rope(xk[:, t0:t0 + n], [xk], krT[:, t0:t0 + n], krT, t0, n)
                    if os.environ.get("MLA_STOP") == "B1":
                        return
                    for h in range(4):
                        pq = self.bank()
                        for kc in range(4):
                            P.op("pe", lambda e: e.matmul(pq[:, 0:n], lhsT=Wuq[:, kc, h * 192:h * 192 + 128], rhs=cqT[:, kc, t0:t0 + n], start=(kc == 0), stop=(kc == 3)), r=[Wuq, cqT], w=[pq])
                        P.op("act", lambda e: e.copy(out=qnT[:, h, t0:t0 + n], in_=pq[:, 0:n]), r=[pq], w=[qnT])
                        if os.environ.get("MLA_STOP") == "B2a":
                            return
                        pr = self.bank()
                        for kc in range(4):
                            P.op("pe", lambda e: e.matmul(pr[:, 0:n], lhsT=Wr[:, kc, h, :], rhs=cqT[:, kc, t0:t0 + n], start=(kc == 0), stop=(kc == 3)), r=[Wr, cqT], w=[pr])
                        rope(pr[:, 0:n], [pr], qrT[:, h, t0:t0 + n], qrT, t0, n)
                        if os.environ.get("MLA_STOP") == "B2b":
                            return
                        pk = self.bank()
                        for kc in range(2):
                            P.op("pe", lambda e: e.matmul(pk[:, 0:n], lhsT=Wukv[:, kc, h * 256:h * 256 + 128], rhs=ckvT[:, kc, t0:t0 + n], start=(kc == 0), stop=(kc == 1)), r=[Wukv, ckvT], w=[pk])
                        P.op("act", lambda e: e.copy(out=knT[:, h, t0:t0 + n], in_=pk[:, 0:n]), r=[pk], w=[knT])
                if os.environ.get("MLA_STOP") == "B2":
                    return
                for i in range(NT):
                    pv = self.bank()
                    for h in range(4):
                        for kc in range(2):
                            P.op("pe", lambda e: e.matmul(pv[:, h * 128:(h + 1) * 128], lhsT=ckvT[:, kc, i * 128:(i + 1) * 128], rhs=Wukv[:, kc, h * 256 + 128:(h + 1) * 256],
                                                          start=(kc == 0), stop=(kc == 1)), r=[ckvT, Wukv], w=[pv])
                    P.op("act", lambda e: e.copy(out=vaug[:, i, :, 0:128], in_=pv[:, :].rearrange("p (h c) -> p h c", h=4)), r=[pv], w=[vaug])
                P.barrier()
            if os.environ.get("MLA_STOP") == "B":
                return
            (acc,) = self.take_banks(1)
            PTe = [self.sb(st, [128, 128], BF16) for _ in range(2)]
            ob = [self.sb(st, [128, 512]) for _ in range(2)]
            rc = self.sb(st, [128, 4])
            scale = float((128 + 64) ** -0.5)
            qtiles = [(i, list(range(self.NCT))) for i in range(self.NCT)] + [(i, list(range(NT))) for i in range(self.NCT, NT)]
            pi = 0
            for oi, (qi, ktl) in enumerate(qtiles):
                qsl = slice(qi * 128, (qi + 1) * 128)
                o_ = ob[oi % 2]
                for h in range(4):
                    for kt in ktl:
                        ks = slice(kt * 128, (kt + 1) * 128)
                        pST = self.bank()
                        P.op("pe", lambda e: e.matmul(pST[:, 0:128], lhsT=knT[:, h, ks], rhs=qnT[:, h, qsl], start=True, stop=False), r=[knT, qnT], w=[pST])
                        P.op("pe", lambda e: e.matmul(pST[:, 0:128], lhsT=krT[:, ks], rhs=qrT[:, h, qsl], start=False, stop=True), r=[krT, qrT], w=[pST])
                        pt = PTe[pi % 2]
                        pi += 1
                        P.op("act", lambda e: e.activation(out=pt.ap, in_=pST[:, 0:128], func=AF.Exp, scale=scale), r=[pST], w=[pt])
                        P.op("pe", lambda e: e.matmul(acc[:, 0:129], lhsT=pt.ap, rhs=vaug[:, kt, h, 0:129], start=(kt == ktl[0]), stop=(kt == ktl[-1])), r=[pt, vaug], w=[acc])
                    P.op("dve", lambda e: e.reciprocal(out=rc[:, h:h + 1], in_=acc[:, 128:129]), r=[acc], w=[rc])
                    P.op("dve", lambda e: e.tensor_scalar(out=o_[:, h * 128:(h + 1) * 128], in0=acc[:, 0:128], scalar1=rc[:, h:h + 1], scalar2=None, op0=ALU.mult), r=[acc, rc], w=[o_])
                P.dma("sp", self.BR[2, qsl, :], o_.ap, r=[o_])
            self.release_banks()

    def phase_merge(self, l):
        P = self.P
        T, NT = self.T, self.NT
        with ExitStack() as st:
            self.load_hT(st)
            brT = [self.sb(st, [128, 4, T], BF16) for _ in range(4)]
            with ExitStack() as s2:
                bt = self.sb(s2, [128, 512]); bb = self.sb(s2, [128, 512], BF16)
                for k in range(4):
                    for i in range(NT):
                        P.dma("sp", bt.ap, self.BR[k, i * 128:(i + 1) * 128, :], w=[bt])
                        P.op("dve", lambda e: e.tensor_copy(out=bb.ap, in_=bt.ap), r=[bt], w=[bb])

                        def dst(q, nb, pb, i=i, k=k):
                            P.op("act", lambda e: e.copy(out=brT[k][:, q:q + nb, i * 128:(i + 1) * 128],
                                                         in_=pb[:, 0:nb * 128].rearrange("p (a t) -> p a t", a=nb)), r=[pb], w=[brT[k]])
                        self.transpose_to(bb, 512, dst)
                P.barrier()
            wg = self.wpipe(st, KC, 128)
            wb_ = self.wpipe(st, 4, 128)
            acc = self.sb(st, [128, T]); accb = self.sb(st, [128, T], BF16)
            gs = self.sb(st, [128, 512]); tmp = self.sb(st, [128, 512])
            for n in range(16):
                ns = slice(n * 128, (n + 1) * 128)
                for k in range(4):
                    Wg = self.wload(wg, self.w_gate[l, k, :, ns], KC, 128)
                    Wb = self.wload(wb_, self.w_br[l, k, :, ns], 4, 128)
                    for (t0, nt) in self.tok_blocks(0, T):
                        pg = self.bank()
                        for kc in range(KC):
                            P.op("pe", lambda e: e.matmul(pg[:, 0:nt], lhsT=Wg[:, kc, :], rhs=self.hT[:, kc, t0:t0 + nt], start=(kc == 0), stop=(kc == KC - 1)), r=[Wg, self.hT], w=[pg])
                        P.op("act", lambda e: e.activation(out=gs[:, 0:nt], in_=pg[:, 0:nt], func=AF.Sigmoid), r=[pg], w=[gs])
                        pbk = self.bank()
                        for kc in range(4):
                            P.op("pe", lambda e: e.matmul(pbk[:, 0:nt], lhsT=Wb[:, kc, :], rhs=brT[k][:, kc, t0:t0 + nt], start=(kc == 0), stop=(kc == 3)), r=[Wb, brT[k]], w=[pbk])
                        if k == 0:
                            P.op("dve", lambda e: e.tensor_tensor(out=acc[:, t0:t0 + nt], in0=pbk[:, 0:nt], in1=gs[:, 0:nt], op=ALU.mult), r=[pbk, gs], w=[acc])
                        else:
                            P.op("dve", lambda e: e.tensor_tensor(out=tmp[:, 0:nt], in0=pbk[:, 0:nt], in1=gs[:, 0:nt], op=ALU.mult), r=[pbk, gs], w=[tmp])
                            P.op("dve", lambda e: e.tensor_tensor(out=acc[:, t0:t0 + nt], in0=acc[:, t0:t0 + nt], in1=tmp[:, 0:nt], op=ALU.add), r=[acc, tmp], w=[acc])
                P.op("act", lambda e: e.copy(out=accb.ap, in_=acc.ap), r=[acc], w=[accb])
                P.dma("sp", self.ACC[ns, :], accb.ap, r=[accb])

    def phase_wout(self, l):
        P = self.P
        T, NT = self.T, self.NT
        with ExitStack() as st:
            accT = self.sb(st, [128, KC, T], BF16)
            P.dma("sp", accT.ap, self.ACC.rearrange("(k p) t -> p k t", p=128), w=[accT])
            wp = self.wpipe(st, KC, 256)
            ev = [self.sb(st, [128, 256]) for _ in range(2)]
            ei = 0
            for c in range(8):
                Wo = self.wload(wp, self.w_out[l, :, c * 256:(c + 1) * 256], KC, 256)
                for i in range(NT):
                    pb = self.bank()
                    for kc in range(KC):
                        P.op("pe", lambda e: e.matmul(pb[:, 0:256], lhsT=accT[:, kc, i * 128:(i + 1) * 128], rhs=Wo[:, kc, :], start=(kc == 0), stop=(kc == KC - 1)), r=[accT, Wo], w=[pb])
                    e_ = ev[ei % 2]
                    ei += 1
                    P.op("act", lambda e: e.copy(out=e_.ap, in_=pb[:, 0:256]), r=[pb], w=[e_])
                    P.dma("sp", self.Y[i * 128:(i + 1) * 128, c * 256:(c + 1) * 256], e_.ap, r=[e_])
            P.barrier()
        self.residual(l, "G2")
        P.barrier()
        self.norm_mod_to_hT(l, "A2", "B2")

    def residual(self, l, kind):
        P = self.P
        with ExitStack() as st:
            G = self.load_modvec(st, l, kind)
            yt = [self.sb(st, [128, D]) for _ in range(2)]
            xt = [self.sb(st, [128, D]) for _ in range(2)]
            junk = self.sb(st, [128, D], BF16)
            ss = [self.sb(st, [128, 4]) for _ in range(2)]
            for i in range(self.NT):
                j = 1 if i < self.NCT else 0
                y_, x_, s_ = yt[i % 2], xt[i % 2], ss[i % 2]
                P.dma("sp", y_.ap, self.Y[i * 128:(i + 1) * 128, :], w=[y_])
                P.dma("sp", x_.ap, self.xres[i * 128:(i + 1) * 128, :], w=[x_])
                self.rstd_of(y_.ap, [y_], D, junk.ap, s_)
                P.op("dve", lambda e: e.scalar_tensor_tensor(out=y_.ap, in0=y_.ap, scalar=s_[:, 3:4], in1=G[:, j, :], op0=ALU.mult, op1=ALU.mult), r=[y_, s_, G], w=[y_])
                P.op("dve", lambda e: e.tensor_tensor(out=x_.ap, in0=x_.ap, in1=y_.ap, op=ALU.add), r=[x_, y_], w=[x_])
                P.dma("sp", self.xres[i * 128:(i + 1) * 128, :], x_.ap, r=[x_])

    def phase_moe(self, l):
        P = self.P
        T, NT = self.T, self.NT
        NE = 1 if self.tiny_moe else 32
        PTK = min(NT, 6)
        with ExitStack() as st:
            comb = self.sb(st, [128, NT, 32])
            with ExitStack() as s2:
                self.load_hT(s2)
                ws = self.sb(s2, [128, KC, 36]); wr = self.sb(s2, [128, KC, 36], BF16)
                P.dma("sp", ws[:, :, 0:4], self.w_grp[l].rearrange("(k p) n -> p k n", p=128), w=[ws])
                P.dma("sp", ws[:, :, 4:36], self.w_exp[l].rearrange("(k p) n -> p k n", p=128), w=[ws])
                P.op("dve", lambda e: e.tensor_copy(out=wr.ap, in_=ws.ap), r=[ws], w=[wr])
                lg = self.sb(s2, [128, 36]); sc = self.sb(s2, [128, 16]); gm = self.sb(s2, [128, 4]); ge = self.sb(s2, [128, 4])
                t48 = self.sb(s2, [128, 4, 8]); sel = self.sb(s2, [128, 8]); sel2 = self.sb(s2, [128, 8]); e1 = self.sb(s2, [128, 8]); e2 = self.sb(s2, [128, 8]); c8 = self.sb(s2, [128, 8])
                for i in range(NT):
                    pb = self.bank()
                    for kc in range(KC):
                        P.op("pe", lambda e: e.matmul(pb[:, 0:36], lhsT=self.hT[:, kc, i * 128:(i + 1) * 128], rhs=wr[:, kc, :], start=(kc == 0), stop=(kc == KC - 1)), r=[self.hT, wr], w=[pb])
                    P.op("dve", lambda e: e.tensor_copy(out=lg.ap, in_=pb[:, 0:36]), r=[pb], w=[lg])
                    P.op("dve", lambda e: e.tensor_reduce(out=sc[:, 0:1], in_=lg[:, 0:4], axis=AX.X, op=ALU.max), r=[lg], w=[sc])
                    P.op("dve", lambda e: e.tensor_scalar(out=sc[:, 1:2], in0=sc[:, 0:1], scalar1=-1.0, scalar2=None, op0=ALU.mult), r=[sc], w=[sc])
                    P.op("act", lambda e: e.activation(out=ge.ap, in_=lg[:, 0:4], func=AF.Exp, bias=sc[:, 1:2], scale=1.0), r=[lg, sc], w=[ge])
                    P.op("dve", lambda e: e.tensor_reduce(out=sc[:, 2:3], in_=ge.ap, axis=AX.X, op=ALU.add), r=[ge], w=[sc])
                    P.op("dve", lambda e: e.reciprocal(out=sc[:, 3:4], in_=sc[:, 2:3]), r=[sc], w=[sc])
                    P.op("dve", lambda e: e.tensor_scalar(out=gm.ap, in0=lg[:, 0:4], scalar1=sc[:, 0:1], scalar2=None, op0=ALU.is_equal), r=[lg, sc], w=[gm])
                    P.op("dve", lambda e: e.tensor_tensor(out=t48.ap, in0=lg[:, 4:36].rearrange("p (g e) -> p g e", g=4), in1=gm.ap.unsqueeze(2).broadcast_to([128, 4, 8]), op=ALU.mult), r=[lg, gm], w=[t48])
                    P.op("dve", lambda e: e.tensor_reduce(out=sel.ap, in_=t48.ap.rearrange("p g e -> p e g"), axis=AX.X, op=ALU.add), r=[t48], w=[sel])
                    P.op("dve", lambda e: e.tensor_reduce(out=sc[:, 4:5], in_=sel.ap, axis=AX.X, op=ALU.max), r=[sel], w=[sc])
                    P.op("dve", lambda e: e.tensor_scalar(out=e1.ap, in0=sel.ap, scalar1=sc[:, 4:5], scalar2=None, op0=ALU.is_equal), r=[sel, sc], w=[e1])
                    P.op("dve", lambda e: e.scalar_tensor_tensor(out=sel2.ap, in0=e1.ap, scalar=-1e30, in1=sel.ap, op0=ALU.mult, op1=ALU.add), r=[e1, sel], w=[sel2])
                    P.op("dve", lambda e: e.tensor_reduce(out=sc[:, 5:6], in_=sel2.ap, axis=AX.X, op=ALU.max), r=[sel2], w=[sc])
                    P.op("dve", lambda e: e.tensor_scalar(out=e2.ap, in0=sel2.ap, scalar1=sc[:, 5:6], scalar2=None, op0=ALU.is_equal), r=[sel2, sc], w=[e2])
                    P.op("dve", lambda e: e.tensor_tensor(out=sc[:, 6:7], in0=sc[:, 5:6], in1=sc[:, 4:5], op=ALU.subtract), r=[sc], w=[sc])
                    P.op("act", lambda e: e.activation(out=sc[:, 6:7], in_=sc[:, 6:7], func=AF.Exp), r=[sc], w=[sc])
                    P.op("dve", lambda e: e.tensor_scalar(out=sc[:, 6:7], in0=sc[:, 6:7], scalar1=1.0, scalar2=None, op0=ALU.add), r=[sc], w=[sc])
                    P.op("dve", lambda e: e.reciprocal(out=sc[:, 7:8], in_=sc[:, 6:7]), r=[sc], w=[sc])
                    P.op("dve", lambda e: e.tensor_tensor(out=sc[:, 8:9], in0=sc[:, 7:8], in1=sc[:, 3:4], op=ALU.mult), r=[sc], w=[sc])
                    P.op("dve", lambda e: e.tensor_tensor(out=sc[:, 9:10], in0=sc[:, 3:4], in1=sc[:, 8:9], op=ALU.subtract), r=[sc], w=[sc])
                    P.op("dve", lambda e: e.tensor_scalar(out=c8.ap, in0=e1.ap, scalar1=sc[:, 8:9], scalar2=None, op0=ALU.mult), r=[e1, sc], w=[c8])
                    P.op("dve", lambda e: e.scalar_tensor_tensor(out=c8.ap, in0=e2.ap, scalar=sc[:, 9:10], in1=c8.ap, op0=ALU.mult, op1=ALU.add), r=[e2, sc, c8], w=[c8])
                    for g in range(4):
                        P.op("dve", lambda e: e.tensor_scalar(out=comb[:, i, g * 8:(g + 1) * 8], in0=c8.ap, scalar1=gm[:, g:g + 1], scalar2=None, op0=ALU.mult), r=[c8, gm], w=[comb])
                P.barrier()
            w13 = self.wpipe(st, KC, 256, nbuf=2)
            w2p = self.wpipe(st, 4, 512, nbuf=2)
            hTp = self.sb(st, [128, KC, PTK * 128], BF16)
            aT = self.sb(st, [128, 4, PTK * 128], BF16)
            acc = self.sb(st, [128, PTK, D])
            s1 = self.sb(st, [128, 512])
            for p0 in range(0, NT, PTK):
                np_ = min(PTK, NT - p0)
                ntok = np_ * 128
                P.dma("sp", hTp[:, :, 0:ntok], self.HTd[:, :, p0 * 128:p0 * 128 + ntok], w=[hTp])
                for ex in range(NE):
                    for c in range(2):
                        W1 = self.wload(w13, self.w1[l, ex, :, c * 256:(c + 1) * 256], KC, 256)
                        W3 = self.wload(w13, self.w3[l, ex, :, c * 256:(c + 1) * 256], KC, 256)
                        for j in range(2):
                            for (t0, nt) in self.tok_blocks(0, ntok):
                                p1 = self.bank()
                                for kc in range(KC):
                                    P.op("pe", lambda e: e.matmul(p1[:, 0:nt], lhsT=W1[:, kc, j * 128:(j + 1) * 128], rhs=hTp[:, kc, t0:t0 + nt], start=(kc == 0), stop=(kc == KC - 1)), r=[W1, hTp], w=[p1])
                                p3 = self.bank()
                                for kc in range(KC):
                                    P.op("pe", lambda e: e.matmul(p3[:, 0:nt], lhsT=W3[:, kc, j * 128:(j + 1) * 128], rhs=hTp[:, kc, t0:t0 + nt], start=(kc == 0), stop=(kc == KC - 1)), r=[W3, hTp], w=[p3])
                                P.op("act", lambda e: e.activation(out=s1[:, 0:nt], in_=p1[:, 0:nt], func=AF.Silu), r=[p1], w=[s1])
                                P.op("dve", lambda e: e.tensor_tensor(out=aT[:, c * 2 + j, t0:t0 + nt], in0=p3[:, 0:nt], in1=s1[:, 0:nt], op=ALU.mult), r=[p3, s1], w=[aT])
                    for cc in range(4):
                        W2 = self.wload(w2p, self.w2[l, ex, :, cc * 512:(cc + 1) * 512], 4, 512)
                        for t in range(np_):
                            py = self.bank()
                            for kc in range(4):
                                P.op("pe", lambda e: e.matmul(py[:, :], lhsT=aT[:, kc, t * 128:(t + 1) * 128], rhs=W2[:, kc, :], start=(kc == 0), stop=(kc == 3)), r=[aT, W2], w=[py])
                            cw_ = comb[:, p0 + t, ex:ex + 1]
                            if ex == 0:
                                P.op("dve", lambda e: e.tensor_scalar(out=acc[:, t, cc * 512:(cc + 1) * 512], in0=py[:, :], scalar1=cw_, scalar2=None, op0=ALU.mult), r=[py, comb], w=[acc])
                            else:
                                P.op("dve", lambda e: e.scalar_tensor_tensor(out=acc[:, t, cc * 512:(cc + 1) * 512], in0=py[:, :], scalar=cw_, in1=acc[:, t, cc * 512:(cc + 1) * 512],
                                                                             op0=ALU.mult, op1=ALU.add), r=[py, comb, acc], w=[acc])
                for t in range(np_):
                    P.dma("sp", self.Y[(p0 + t) * 128:(p0 + t + 1) * 128, :], acc[:, t, :], r=[acc])

    def phase_fin(self, l):
        self.residual(l, "G4")


def core_inputs(inputs, b, n_ctx, n_lat, consts, rope, tiny_moe=False):
    m = {}
    f = lambda a: np.ascontiguousarray(a, dtype=np.float32)
    m["x"] = f(inputs["x"][b, :n_lat])
    m["ctx"] = f(inputs["ctx"][b, :n_ctx])
    m["c"] = f(f(inputs["c"][b]).reshape(16, 128).T)
    m["c_ctx"] = f(f(inputs["c_ctx"]).reshape(16, 128).T)
    for k in ["w_ada", "b_ada", "norm_g", "w_in", "hg_lb_logits", "hg_norm_g", "mla_g_q", "mla_g_kv", "mla_w_uq",
              "mla_w_ukv", "ssd_conv_w", "ssd_conv_b", "ssd_norm_g", "w_gate", "w_br", "w_out", "moe_w_grp",
              "moe_w_exp", "moe_w1", "moe_w3", "moe_w2", "ssd_d"]:
        m[k] = f(inputs[k])
    L = inputs["w_in"].shape[0]
    m["ssd_conv_w"] = f(f(inputs["ssd_conv_w"]).reshape(L, 3, 8, 128).transpose(0, 3, 2, 1))
    m["ssd_conv_b"] = f(f(inputs["ssd_conv_b"]).reshape(L, 8, 128).transpose(0, 2, 1))
    m["hg_lbT"] = f(f(inputs["hg_lb_logits"]).reshape(L + 1, 4, 128).transpose(2, 0, 1))
    m["ml_f_bias"] = f(inputs["ml_f_bias"]).reshape(L, 8)
    m["ssd_a_log"] = f(inputs["ssd_a_log"]).reshape(L, 16)
    m["ssd_dt_bias"] = f(inputs["ssd_dt_bias"]).reshape(L, 16)
    if tiny_moe:
        for k in ["moe_w1", "moe_w3", "moe_w2"]:
            m[k] = np.ascontiguousarray(m[k][:, 0:1])
    m["consts"] = consts
    m["rope"] = rope
    return m


def kernel(**inputs):
    B, n_lat, _ = inputs["x"].shape
    n_ctx = inputs["ctx"].shape[1]
    mk = MK(n_ctx, n_lat)
    nc = mk.build()
    consts, rope = make_consts(n_ctx, n_lat)
    in_maps = [core_inputs(inputs, b, n_ctx, n_lat, consts, rope) for b in range(B)]
    res = run_bass_kernel_spmd(nc, in_maps, core_ids=list(range(B)))
    return np.stack([np.asarray(r["out"]) for r in res.results], axis=0).astype(np.float32)
```

```python
import os
import numpy as np
from contextlib import ExitStack
import concourse.bass as bass
import concourse.mybir as mybir
from concourse.bass_utils import run_bass_kernel_spmd

F32 = mybir.dt.float32
BF16 = mybir.dt.bfloat16
AF = mybir.ActivationFunctionType
ALU = mybir.AluOpType
AX = mybir.AxisListType

D = 2048
KC = 16
DIN = 7008
EPS = 1e-6
NEG = -30000.0
EPOCH = 20000
N_DMA_SEMS = 24

O_MLQ, O_MLK, O_MLV, O_MLO, O_MLG = 0, 512, 1024, 1536, 2048
O_HGQ, O_HGI, O_HGG, O_HGF = 2064, 2576, 3088, 3600
O_CQ, O_CKV, O_KR = 4624, 5136, 5392
O_SZ, O_XBC, O_DT = 5456, 5968, 6992


class Buf:
    __slots__ = ("w", "r", "excl")

    def __init__(self):
        self.w = None
        self.r = {}
        self.excl = False


class Tl:
    def __init__(self, t):
        self.t = t
        self.b = Buf()

    def __getitem__(self, idx):
        return self.t.ap()[idx]

    @property
    def ap(self):
        return self.t.ap()


class Prog:
    def __init__(self, nc):
        self.nc = nc
        self.engs = {"pe": nc.tensor, "act": nc.scalar, "dve": nc.vector,
                     "pool": nc.gpsimd, "sp": nc.sync}
        self.sem = {}
        self.cnt = {}
        self.nsem = 0
        self.last = {}
        for e in self.engs:
            self._new_epoch(e)
        self.waited = {e: {} for e in self.engs}
        self.dma_sems = []
        for i in range(N_DMA_SEMS):
            s = nc.alloc_semaphore(f"dsem{i}")
            self.dma_sems.append([s, 0, ("d", i)])
        self.dma_rr = 0
        self.ninst = 0

    def _new_epoch(self, e):
        self.nsem += 1
        s = self.nc.alloc_semaphore(f"s_{e}_{self.nsem}")
        self.sem[e] = (s, ("e", e, self.nsem))
        self.cnt[e] = 0

    def _wait(self, eng, evs):
        best = {}
        for ev in evs:
            if ev is None:
                continue
            sem, val, key, src = ev
            if eng == "pe" and src == "pe":
                continue
            if self.waited[eng].get(key, 0) >= val:
                continue
            if key not in best or best[key][1] < val:
                best[key] = ev
        for key, (sem, val, _, _) in best.items():
            self.engs[eng].wait_ge(sem, val)
            self.waited[eng][key] = val

    def _tick(self, eng, inst):
        if self.cnt[eng] >= EPOCH:
            self._new_epoch(eng)
        sem, key = self.sem[eng]
        self.cnt[eng] += 1
        inst.then_inc(sem, 1)
        self.ninst += 1
        ev = (sem, self.cnt[eng], key, eng)
        self.last[key] = ev
        return ev

    @staticmethod
    def _deps(r, w):
        evs = []
        for b in r:
            evs.append(b.w)
            if b.excl:
                evs.extend(b.r.values())
        for b in w:
            evs.append(b.w)
            evs.extend(b.r.values())
        return evs

    @staticmethod
    def _commit(ev, r, w):
        for b in r:
            old = b.r.get(ev[2])
            if old is None or old[1] < ev[1]:
                b.r[ev[2]] = ev
        for b in w:
            b.w = ev
            b.r = {}

    def op(self, eng, fn, r=(), w=()):
        r = [x.b if isinstance(x, Tl) else x for x in r]
        w = [x.b if isinstance(x, Tl) else x for x in w]
        self._wait(eng, self._deps(r, w))
        inst = fn(self.engs[eng])
        ev = self._tick(eng, inst)
        self._commit(ev, r, w)
        return ev

    def dma(self, q, out, in_, r=(), w=(), **kw):
        r = [x.b if isinstance(x, Tl) else x for x in r]
        w = [x.b if isinstance(x, Tl) else x for x in w]
        ent = self.dma_sems[self.dma_rr]
        self.dma_rr = (self.dma_rr + 1) % len(self.dma_sems)
        sem, val, key = ent
        evs = self._deps(r, w)
        if val > 0:
            evs.append((sem, val, key, "dma"))
        self._wait(q, evs)
        inst = self.engs[q].dma_start(out=out, in_=in_, **kw)
        ent[1] = val + 16
        inst.then_inc(sem, 16)
        ev = (sem, val + 16, key, "dma")
        self._commit(ev, r, w)
        self.last[key] = ev
        self.ninst += 1
        return ev

    def barrier(self):
        evs = list(self.last.values())
        for e in self.engs:
            self._wait(e, evs)


C_ID, C_TRF, C_TRB, C_MNF, C_MNB, C_ONE, C_LCF, C_LCB, C_HC, C_PERM = range(10)
NCST = 10


def make_consts(n_ctx, n_lat):
    s = np.arange(128)[:, None]
    t = np.arange(128)[None, :]
    c = np.zeros((NCST, 128, 128), np.float32)
    c[C_ID] = np.eye(128)
    c[C_TRF] = (s <= t)
    c[C_TRB] = (s >= t)
    c[C_MNF] = np.where(s <= t, 0.0, NEG)
    c[C_MNB] = np.where(s >= t, 0.0, NEG)
    c[C_ONE] = 1.0
    c[C_LCF] = (s <= t).astype(np.float32) - (s <= 63)
    c[C_LCB] = (s >= t).astype(np.float32) - (s >= 64)
    sv = np.arange(128)
    c[C_HC][:, 0] = (sv <= 63)
    c[C_HC][:, 1] = 1.0
    c[C_HC][:, 2] = 1.0 - (sv <= 63)
    c[C_HC][:, 3] = (sv >= 64)
    c[C_HC][:, 4] = 1.0
    c[C_HC][:, 5] = 1.0 - (sv >= 64)
    Pm = np.zeros((64, 64), np.float32)
    for base in (0, 32):
        for i in range(16):
            Pm[base + i, base + 16 + i] = -1.0
            Pm[base + 16 + i, base + i] = 1.0
    c[C_PERM][:64, :64] = Pm.T
    consts = np.ascontiguousarray(c.transpose(1, 0, 2).reshape(128, NCST * 128))
    T = n_ctx + n_lat
    tok = np.arange(n_lat)
    pr, pc = tok // 64, tok % 64
    inv = (10000.0 ** (-np.arange(16, dtype=np.float32) / 16)).astype(np.float32)
    cos = np.ones((64, T), np.float32)
    sin = np.zeros((64, T), np.float32)
    for base, pos in ((0, pr), (32, pc)):
        ang = pos.astype(np.float32)[None, :] * inv[:, None]
        cos[base:base + 16, n_ctx:] = np.cos(ang)
        cos[base + 16:base + 32, n_ctx:] = np.cos(ang)
        sin[base:base + 16, n_ctx:] = np.sin(ang)
        sin[base + 16:base + 32, n_ctx:] = np.sin(ang)
    return consts, np.concatenate([cos, sin], axis=0)


class MK:
    def __init__(self, n_ctx, n_lat, depth=2, dbg=False, upto=None, tiny_moe=False):
        self.n_ctx, self.n_lat, self.depth, self.dbg, self.upto = n_ctx, n_lat, depth, dbg, upto
        self.tiny_moe = tiny_moe
        self.T = n_ctx + n_lat
        self.NT = self.T // 128
        self.NCT = n_ctx // 128
        nc = self.nc = bass.Bass("TRN2", target_bir_lowering=False)
        self.P = Prog(nc)
        self.ins = {}
        self.uid = 0
        self.dumped = set()

    def din(self, name, shape):
        a = self.nc.dram_tensor(name, list(shape), F32, kind="ExternalInput").ap()
        self.ins[name] = a
        return a

    def dscr(self, name, shape, dt=F32):
        kind = "ExternalOutput" if self.dbg else "Internal"
        return self.nc.dram_tensor(name, list(shape), dt, kind=kind).ap()

    def sb(self, st, shape, dt=F32, name=None):
        self.uid += 1
        return Tl(st.enter_context(self.nc.sbuf_tensor(f"{name or 't'}{self.uid}", list(shape), dt)))

    def dump(self, name, tl, ap=None, dt=F32):
        if not self.dbg or name in self.dumped:
            return
        self.dumped.add(name)
        ap = tl.ap if ap is None else ap
        dst = self.nc.dram_tensor("dbg_" + name, list(ap.shape), dt, kind="ExternalOutput").ap()
        self.P.dma("sp", dst, ap, r=[tl])

    def take_banks(self, k):
        self.free_banks = list(range(8 - k))
        self.bank_rr = 0
        return [self.ps[8 - k + i] for i in range(k)]

    def release_banks(self):
        self.free_banks = list(range(8))
        self.bank_rr = 0

    def bank(self, pool=None):
        if pool is not None:
            pool["rr"] = (pool["rr"] + 1) % len(pool["banks"])
            return self.ps[pool["banks"][pool["rr"]]]
        self.bank_rr = (self.bank_rr + 1) % len(self.free_banks)
        return self.ps[self.free_banks[self.bank_rr]]

    @staticmethod
    def drive(gens):
        gens = list(gens)
        while gens:
            for g in list(gens):
                try:
                    next(g)
                except StopIteration:
                    gens.remove(g)

    def tok_blocks(self, t0, t1, bs=512):
        out = []
        while t0 < t1:
            n = min(bs, t1 - t0)
            out.append((t0, n))
            t0 += n
        return out

    def build(self):
        nc, P = self.nc, self.P
        T, NT = self.T, self.NT
        L = self.depth
        I = self.din
        self.x_in = I("x", [self.n_lat, D])
        self.ctx_in = I("ctx", [self.n_ctx, D])
        self.c_in = I("c", [128, KC])
        self.cctx_in = I("c_ctx", [128, KC])
        self.w_ada = I("w_ada", [L, D, 6 * D])
        self.b_ada = I("b_ada", [L, 6 * D])
        self.norm_g = I("norm_g", [L, 4, D])
        self.w_in = I("w_in", [L, D, DIN])
        self.ml_f_bias = I("ml_f_bias", [L, 8])
        self.hg_lb = I("hg_lb_logits", [L + 1, 512])
        self.hg_norm_g = I("hg_norm_g", [L, 512])
        self.hg_lbT = I("hg_lbT", [128, L + 1, 4])
        self.mla_g_q = I("mla_g_q", [L, 512])
        self.mla_g_kv = I("mla_g_kv", [L, 256])
        self.mla_w_uq = I("mla_w_uq", [L, 512, 768])
        self.mla_w_ukv = I("mla_w_ukv", [L, 256, 1024])
        self.ssd_conv_w = I("ssd_conv_w", [L, 128, 8, 3])
        self.ssd_conv_b = I("ssd_conv_b", [L, 128, 8])
        self.ssd_a_log = I("ssd_a_log", [L, 16])
        self.ssd_dt_bias = I("ssd_dt_bias", [L, 16])
        self.ssd_d = I("ssd_d", [L, 8])
        self.ssd_norm_g = I("ssd_norm_g", [L, 512])
        self.w_gate = I("w_gate", [L, 4, D, D])
        self.w_br = I("w_br", [L, 4, 512, D])
        self.w_out = I("w_out", [L, D, D])
        self.w_grp = I("moe_w_grp", [L, D, 4])
        self.w_exp = I("moe_w_exp", [L, D, 32])
        ne = 1 if self.tiny_moe else 32
        self.w1 = I("moe_w1", [L, ne, D, 512])
        self.w3 = I("moe_w3", [L, ne, D, 512])
        self.w2 = I("moe_w2", [L, ne, 512, D])
        self.consts_in = I("consts", [128, NCST * 128])
        self.rope_in = I("rope", [128, T])
        self.out = nc.dram_tensor("out", [self.n_lat, D], F32, kind="ExternalOutput").ap()

        S = self.dscr
        self.xres = S("xres", [T, D])
        self.modv = S("modv", [L, 2, 6, D])
        self.lbd = S("lbd", [L, 512])
        self.UT = S("UT", [T, DIN])
        self.UF = S("UF", [DIN, T])
        self.BR = S("BR", [4, T, 512])
        self.ACC = S("ACC", [D, T], BF16)
        self.Y = S("Y", [T, D])
        self.HTd = S("HTd", [128, KC, T], BF16)

        with ExitStack() as top:
            self.ps = [Tl(top.enter_context(nc.psum_tensor(f"ps{i}", [128, 512], F32))) for i in range(8)]
            for p_ in self.ps:
                p_.b.excl = True
            self.free_banks = list(range(8))
            self.bank_rr = 0
            self.cst = self.sb(top, [128, NCST, 128], F32, "cst")
            self.idb = self.sb(top, [128, 128], BF16, "idb")
            self.lbB = self.sb(top, [128, L, 512], F32, "lbB")
            P.dma("sp", self.cst.ap, self.consts_in.rearrange("p (c n) -> p c n", c=NCST), w=[self.cst])
            P.op("dve", lambda e: e.tensor_copy(out=self.idb.ap, in_=self.cst[:, C_ID, :]), r=[self.cst], w=[self.idb])
            P.dma("sp", self.xres[0:self.n_ctx, :], self.ctx_in)
            P.dma("sp", self.xres[self.n_ctx:T, :], self.x_in)
            steps = [("lb", lambda: self.setup_lb())]
            for l in range(L):
                steps.append((f"mod{l}", lambda l=l: self.phase_mod(l)))
            stages = ["norm1", "win", "ml", "hg", "mla", "ssd", "merge", "wout", "moe", "fin"]
            for l in range(L):
                for st_ in stages:
                    steps.append(((l, st_), lambda l=l, st_=st_: getattr(self, "phase_" + st_)(l)))
            P.barrier()
            if self.upto != "init":
                for name, fn in steps:
                    fn()
                    P.barrier()
                    if self.upto == name:
                        break
            P.dma("sp", self.out, self.xres[self.n_ctx:T, :])
            P.barrier()
        return nc

    def setup_lb(self):
        P, L = self.P, self.depth
        with ExitStack() as st:
            lg = self.sb(st, [128, L + 1, 512])
            ex = self.sb(st, [128, L + 1, 512])
            sm = self.sb(st, [128, 512])
            rc = self.sb(st, [128, 512])
            cum = self.sb(st, [128, 512])
            for j in range(L + 1):
                P.dma("sp", lg[:, j, :], self.hg_lb[j:j + 1, :].broadcast_to([128, 512]), w=[lg])
            P.op("act", lambda e: e.activation(out=ex.ap, in_=lg.ap, func=AF.Exp), r=[lg], w=[ex])
            P.op("dve", lambda e: e.tensor_tensor(out=sm.ap, in0=ex[:, 0, :], in1=ex[:, 1, :], op=ALU.add), r=[ex], w=[sm])
            for j in range(2, L + 1):
                P.op("dve", lambda e: e.tensor_tensor(out=sm.ap, in0=sm.ap, in1=ex[:, j, :], op=ALU.add), r=[ex, sm], w=[sm])
            P.op("dve", lambda e: e.reciprocal(out=rc.ap, in_=sm.ap), r=[sm], w=[rc])
            for l in range(L):
                if l == 0:
                    P.op("dve", lambda e: e.tensor_copy(out=cum.ap, in_=ex[:, 0, :]), r=[ex], w=[cum])
                else:
                    P.op("dve", lambda e: e.tensor_tensor(out=cum.ap, in0=cum.ap, in1=ex[:, l, :], op=ALU.add), r=[ex, cum], w=[cum])
                P.op("dve", lambda e: e.tensor_tensor(out=self.lbB[:, l, :], in0=cum.ap, in1=rc.ap, op=ALU.mult), r=[cum, rc], w=[self.lbB])
                P.dma("sp", self.lbd[l:l + 1, :], self.lbB[0:1, l, :], r=[self.lbB])
            P.barrier()

    def phase_mod(self, l):
        P, nc = self.P, self.nc
        with ExitStack() as st:
            cv = self.sb(st, [128, 2, KC])
            cs = self.sb(st, [128, 2, KC])
            SB = self.sb(st, [128, 2, KC, 128])
            bb = [self.sb(st, [1, 512]) for _ in range(2)]
            mo = [self.sb(st, [1, 2, 512]) for _ in range(2)]
            wst = [self.sb(st, [128, KC, 512]) for _ in range(2)]
            P.dma("sp", cv[:, 0, :], self.c_in, w=[cv])
            P.dma("sp", cv[:, 1, :], self.cctx_in, w=[cv])
            P.op("act", lambda e: e.activation(out=cs.ap, in_=cv.ap, func=AF.Silu), r=[cv], w=[cs])
            for j in range(2):
                for kc in range(KC):
                    P.op("dve", lambda e: e.tensor_copy(out=SB[:, j, kc, :], in_=cs[:, j, kc:kc + 1].broadcast_to([128, 128])), r=[cs], w=[SB])
            for ch in range(24):
                w_, b_, m_ = wst[ch % 2], bb[ch % 2], mo[ch % 2]
                P.dma("sp", w_.ap, self.w_ada[l, :, ch * 512:(ch + 1) * 512].rearrange("(k p) n -> p k n", p=128), w=[w_])
                P.dma("sp", b_.ap, self.b_ada[l:l + 1, ch * 512:(ch + 1) * 512], w=[b_])
                s_, c0 = divmod(ch * 512, D)
                for j in range(2):
                    pb = self.bank()
                    for kc in range(KC):
                        P.op("pe", lambda e: e.matmul(pb[:, :], lhsT=SB[:, j, kc, :], rhs=w_[:, kc, :], start=(kc == 0), stop=(kc == KC - 1)),
                             r=[SB, w_], w=[pb])
                    P.op("dve", lambda e: e.tensor_tensor(out=m_[:, j, :], in0=pb[0:1, :], in1=b_.ap, op=ALU.add), r=[pb, b_], w=[m_])
                P.dma("sp", self.modv[l:l + 1, :, s_, c0:c0 + 512], m_.ap, r=[m_])
            P.barrier()

    def load_modvec(self, st, l, kind):
        P = self.P
        mi, gi, add1 = {"A1": (1, 0, True), "B1": (0, None, False), "G2": (2, 1, False),
                        "A2": (4, 2, True), "B2": (3, None, False), "G4": (5, 3, False)}[kind]
        t = self.sb(st, [128, 2, D])
        for j in range(2):
            P.dma("sp", t[:, j, :], self.modv[l, j:j + 1, mi, :].broadcast_to([128, D]), w=[t])
        if gi is not None:
            with ExitStack() as s2:
                g = self.sb(s2, [128, D])
                P.dma("sp", g.ap, self.norm_g[l, gi:gi + 1, :].broadcast_to([128, D]), w=[g])
                for j in range(2):
                    if add1:
                        P.op("dve", lambda e: e.scalar_tensor_tensor(out=t[:, j, :], in0=t[:, j, :], scalar=1.0, in1=g.ap,
                                                                     op0=ALU.add, op1=ALU.mult), r=[t, g], w=[t])
                    else:
                        P.op("dve", lambda e: e.tensor_tensor(out=t[:, j, :], in0=t[:, j, :], in1=g.ap, op=ALU.mult), r=[t, g], w=[t])
                P.barrier()
        return t

    def rstd_of(self, src_ap, src_dep, n, junk, ss):
        P = self.P
        P.op("act", lambda e: e.activation(out=junk, in_=src_ap, func=AF.Square, accum_out=ss[:, 0:1]), r=src_dep, w=[ss])
        P.op("dve", lambda e: e.tensor_scalar(out=ss[:, 1:2], in0=ss[:, 0:1], scalar1=1.0 / n, scalar2=EPS, op0=ALU.mult, op1=ALU.add), r=[ss], w=[ss])
        P.op("act", lambda e: e.activation(out=ss[:, 2:3], in_=ss[:, 1:2], func=AF.Sqrt), r=[ss], w=[ss])
        P.op("dve", lambda e: e.reciprocal(out=ss[:, 3:4], in_=ss[:, 2:3]), r=[ss], w=[ss])

    def transpose_to(self, src, ncol, dst_fn, r_extra=()):
        P = self.P
        nblk = ncol // 128
        for q in range(0, nblk, 4):
            nb = min(4, nblk - q)
            pb = self.bank()
            for j in range(nb):
                P.op("pe", lambda e: e.matmul(pb[:, j * 128:(j + 1) * 128], lhsT=src[:, (q + j) * 128:(q + j + 1) * 128], rhs=self.idb.ap,
                                              start=True, stop=True), r=[src, self.idb], w=[pb])
            dst_fn(q, nb, pb)

    def norm_mod_to_hT(self, l, ia, ib):
        P = self.P
        with ExitStack() as st:
            A = self.load_modvec(st, l, ia)
            B = self.load_modvec(st, l, ib)
            xt = [self.sb(st, [128, D]) for _ in range(2)]
            tmp = self.sb(st, [128, D])
            hb = [self.sb(st, [128, D], BF16) for _ in range(2)]
            junk = self.sb(st, [128, D], BF16)
            ss = [self.sb(st, [128, 4]) for _ in range(2)]
            self.hT = self.sb(st, [128, KC, self.T], BF16, "hT")
            for i in range(self.NT):
                j = 1 if i < self.NCT else 0
                x_, h_, s_ = xt[i % 2], hb[i % 2], ss[i % 2]
                P.dma("sp", x_.ap, self.xres[i * 128:(i + 1) * 128, :], w=[x_])
                self.rstd_of(x_.ap, [x_], D, junk.ap, s_)
                P.op("dve", lambda e: e.scalar_tensor_tensor(out=tmp.ap, in0=x_.ap, scalar=s_[:, 3:4], in1=A[:, j, :], op0=ALU.mult, op1=ALU.mult),
                     r=[x_, s_, A], w=[tmp])
                P.op("pool", lambda e: e.tensor_tensor(out=h_.ap, in0=tmp.ap, in1=B[:, j, :], op=ALU.add), r=[tmp, B], w=[h_])

                def dst(q, nb, pb, i=i):
                    P.op("act", lambda e: e.copy(out=self.hT[:, q:q + nb, i * 128:(i + 1) * 128],
                                                 in_=pb[:, 0:nb * 128].rearrange("p (a t) -> p a t", a=nb)), r=[pb], w=[self.hT])
                self.transpose_to(h_, D, dst)
            P.dma("sp", self.HTd, self.hT.ap, r=[self.hT])
            P.barrier()

    def load_hT(self, st):
        self.hT = self.sb(st, [128, KC, self.T], BF16, "hT")
        self.P.dma("sp", self.hT.ap, self.HTd, w=[self.hT])

    def phase_norm1(self, l):
        self.norm_mod_to_hT(l, "A1", "B1")

    def wpipe(self, st, kc, ncol, nbuf=2):
        return {"stg": [self.sb(st, [128, kc, ncol]) for _ in range(nbuf)],
                "wb": [self.sb(st, [128, kc, ncol], BF16) for _ in range(nbuf)], "i": 0, "n": nbuf}

    def wload(self, wp, dram_ap, kc, ncol):
        P = self.P
        i = wp["i"] % wp["n"]
        wp["i"] += 1
        s_, b_ = wp["stg"][i], wp["wb"][i]
        P.dma("sp", s_[:, 0:kc, 0:ncol], dram_ap.rearrange("(k p) n -> p k n", p=128), w=[s_])
        P.op("pool", lambda e: e.tensor_copy(out=b_[:, 0:kc, 0:ncol], in_=s_[:, 0:kc, 0:ncol]), r=[s_], w=[b_])
        return b_

    def phase_win(self, l):
        P = self.P
        T = self.T
        groups = [(O_MLQ, 512, "F"), (O_MLK, 512, "FT"), (O_MLV, 512, "T"), (O_MLO, 512, "T"), (O_MLG, 16, "T"),
                  (O_HGQ, 512, "F"), (O_HGI, 512, "T"), (O_HGG, 512, "T"), (O_HGF, 1024, "FT"),
                  (O_CQ, 512, "T"), (O_CKV, 256, "T"), (O_KR, 64, "F"),
                  (O_SZ, 512, "T"), (O_XBC, 1024, "F"), (O_DT, 16, "T")]
        with ExitStack() as st:
            self.load_hT(st)
            wp = self.wpipe(st, KC, 256)
            ev = [self.sb(st, [128, 512]) for _ in range(4)]
            ei = 0
            for (c0, n, mode) in groups:
                for cc in range(c0, c0 + n, 256):
                    nn = min(256, c0 + n - cc)
                    wb = self.wload(wp, self.w_in[l, :, cc:cc + nn], KC, nn)
                    if "F" in mode:
                        for m0 in range(0, nn, 128):
                            mm = min(128, nn - m0)
                            for (t0, nt) in self.tok_blocks(0, T):
                                pb = self.bank()
                                for kc in range(KC):
                                    P.op("pe", lambda e: e.matmul(pb[0:mm, 0:nt], lhsT=wb[:, kc, m0:m0 + mm], rhs=self.hT[:, kc, t0:t0 + nt],
                                                                  start=(kc == 0), stop=(kc == KC - 1)), r=[wb, self.hT], w=[pb])
                                e_ = ev[ei % 4]
                                eng = "act" if ei % 2 == 0 else "dve"
                                ei += 1
                                if eng == "act":
                                    P.op("act", lambda e: e.copy(out=e_[0:mm, 0:nt], in_=pb[0:mm, 0:nt]), r=[pb], w=[e_])
                                else:
                                    P.op("dve", lambda e: e.tensor_copy(out=e_[0:mm, 0:nt], in_=pb[0:mm, 0:nt]), r=[pb], w=[e_])
                                P.dma("sp", self.UF[cc + m0:cc + m0 + mm, t0:t0 + nt], e_[0:mm, 0:nt], r=[e_])
                    if "T" in mode:
                        for i in range(self.NT):
                            pb = self.bank()
                            for kc in range(KC):
                                P.op("pe", lambda e: e.matmul(pb[:, 0:nn], lhsT=self.hT[:, kc, i * 128:(i + 1) * 128], rhs=wb[:, kc, 0:nn],
                                                              start=(kc == 0), stop=(kc == KC - 1)), r=[wb, self.hT], w=[pb])
                            e_ = ev[ei % 4]
                            eng = "act" if ei % 2 == 0 else "dve"
                            ei += 1
                            if eng == "act":
                                P.op("act", lambda e: e.copy(out=e_[:, 0:nn], in_=pb[:, 0:nn]), r=[pb], w=[e_])
                            else:
                                P.op("dve", lambda e: e.tensor_copy(out=e_[:, 0:nn], in_=pb[:, 0:nn]), r=[pb], w=[e_])
                            P.dma("sp", self.UT[i * 128:(i + 1) * 128, cc:cc + nn], e_[:, 0:nn], r=[e_])

    def order(self, d):
        c = list(range(self.NCT))
        la = list(range(self.NCT, self.NT))
        return c + la if d == 0 else c[::-1] + la[::-1]

    def decay_scalars(self, sc, lf_ap, lf_dep, n, d, pool=None):
        P = self.P
        tri = C_TRF if d == 0 else C_TRB
        pb = self.bank(pool)
        P.op("pe", lambda e: e.matmul(pb[:, 0:n], lhsT=self.cst[:, tri, :], rhs=lf_ap, start=True, stop=True), r=[self.cst] + lf_dep, w=[pb])
        P.op("pe", lambda e: e.matmul(pb[:, n:2 * n], lhsT=self.cst[:, C_ONE, :], rhs=lf_ap, start=True, stop=True), r=[self.cst] + lf_dep, w=[pb])
        P.op("dve", lambda e: e.tensor_copy(out=sc[:, 0:2 * n], in_=pb[:, 0:2 * n]), r=[pb], w=[sc])
        P.op("dve", lambda e: e.tensor_scalar(out=sc[:, 2 * n:3 * n], in0=sc[:, 0:n], scalar1=-1.0, scalar2=None, op0=ALU.mult), r=[sc], w=[sc])
        P.op("dve", lambda e: e.tensor_tensor(out=sc[:, 3 * n:4 * n], in0=sc[:, n:2 * n], in1=sc[:, 0:n], op=ALU.subtract), r=[sc], w=[sc])
        P.op("act", lambda e: e.activation(out=sc[:, 3 * n:4 * n], in_=sc[:, 3 * n:4 * n], func=AF.Exp), r=[sc], w=[sc])
        P.op("act", lambda e: e.activation(out=sc[:, 4 * n:6 * n], in_=sc[:, 0:2 * n], func=AF.Exp), r=[sc], w=[sc])

    def decay_matrix(self, lf_col, lf_dep, bias_col, bias_dep, d, lfB, Dm, pool=None):
        P = self.P
        tri = C_TRF if d == 0 else C_TRB
        mn = C_MNF if d == 0 else C_MNB
        P.op("dve", lambda e: e.tensor_copy(out=lfB.ap, in_=lf_col.broadcast_to([128, 128])), r=lf_dep, w=[lfB])
        pb = self.bank(pool)
        P.op("pe", lambda e: e.matmul(pb[:, 0:128], lhsT=lfB.ap, rhs=self.cst[:, tri, :], start=True, stop=False), r=[lfB, self.cst], w=[pb])
        P.op("pe", lambda e: e.matmul(pb[:, 0:128], lhsT=self.cst[:, C_ID, :], rhs=self.cst[:, mn, :], start=False, stop=True), r=[self.cst], w=[pb])
        P.op("act", lambda e: e.activation(out=Dm.ap, in_=pb[:, 0:128], func=AF.Exp, bias=bias_col, scale=1.0), r=[pb] + bias_dep, w=[Dm])

    def phase_ml(self, l):
        P = self.P
        with ExitStack() as st:
            bias = self.sb(st, [128, 8])
            P.dma("sp", bias.ap, self.ml_f_bias[l:l + 1, :].broadcast_to([128, 8]), w=[bias])
            ydir = [self.sb(st, [128, self.NT, 512]) for _ in range(2)]
            self.drive([self.ml_chain(st, l, d, bias, ydir[d]) for d in range(2)])
            ot = self.sb(st, [128, 512]); sg = self.sb(st, [128, 512])
            for i in range(self.NT):
                ts = slice(i * 128, (i + 1) * 128)
                P.dma("sp", ot.ap, self.UT[ts, O_MLO:O_MLO + 512], w=[ot])
                P.op("act", lambda e: e.activation(out=sg.ap, in_=ot.ap, func=AF.Sigmoid), r=[ot], w=[sg])
                P.op("dve", lambda e: e.tensor_tensor(out=ot.ap, in0=ydir[0][:, i, :], in1=ydir[1][:, i, :], op=ALU.add), r=[ydir[0], ydir[1]], w=[ot])
                P.op("dve", lambda e: e.tensor_tensor(out=sg.ap, in0=sg.ap, in1=ot.ap, op=ALU.mult), r=[sg, ot], w=[sg])
                P.dma("sp", self.BR[0, ts, :], sg.ap, r=[sg])

    def ml_chain(self, st, l, d, bias, yo):
        P = self.P
        pool = {"banks": list(range(4 * d, 4 * d + 4)), "rr": 0}
        Cs = self.sb(st, [128, 4, 132]); Cb = self.sb(st, [128, 4, 132], BF16)
        qT = self.sb(st, [128, 4, 128]); kT = self.sb(st, [128, 4, 128])
        qTb = self.sb(st, [128, 4, 128], BF16); kTb = self.sb(st, [128, 4, 128], BF16)
        kt = self.sb(st, [128, 512]); vt = self.sb(st, [128, 512])
        vaug = self.sb(st, [128, 4, 132], BF16)
        gt = self.sb(st, [128, 16]); xg = self.sb(st, [128, 4]); lf = self.sb(st, [128, 4]); ab = self.sb(st, [128, 8])
        sc = self.sb(st, [128, 24])
        lfB = self.sb(st, [128, 128]); Dm = self.sb(st, [128, 128]); PT = self.sb(st, [128, 128], BF16)
        tmpI = self.sb(st, [128, 132]); tot = self.sb(st, [128, 132]); dn = self.sb(st, [128, 2])
        kw = self.sb(st, [128, 128], BF16)
        P.op("pool", lambda e: e.memset(vaug.ap, 1.0), w=[vaug])
        P.op("dve", lambda e: e.memset(Cs.ap, 0.0), w=[Cs])
        P.op("pool", lambda e: e.memset(Cb.ap, 0.0), w=[Cb])
        yield
        for i in self.order(d):
            ts = slice(i * 128, (i + 1) * 128)
            P.dma("sp", qT.ap, self.UF[O_MLQ:O_MLQ + 512, ts].rearrange("(h p) t -> p h t", p=128), w=[qT])
            P.dma("sp", kT.ap, self.UF[O_MLK:O_MLK + 512, ts].rearrange("(h p) t -> p h t", p=128), w=[kT])
            P.dma("sp", kt.ap, self.UT[ts, O_MLK:O_MLK + 512], w=[kt])
            P.dma("sp", vt.ap, self.UT[ts, O_MLV:O_MLV + 512], w=[vt])
            P.dma("sp", gt.ap, self.UT[ts, O_MLG:O_MLG + 16], w=[gt])
            yield
            P.op("act", lambda e: e.mul(out=qTb.ap, in_=qT.ap, mul=128.0 ** -0.5), r=[qT], w=[qTb])
            P.op("pool", lambda e: e.tensor_copy(out=kTb.ap, in_=kT.ap), r=[kT], w=[kTb])
            P.op("pool", lambda e: e.tensor_copy(out=vaug[:, :, 0:128], in_=vt.ap.rearrange("p (h d) -> p h d", h=4)), r=[vt], w=[vaug])
            yield
            P.op("dve", lambda e: e.tensor_tensor(out=xg.ap, in0=gt[:, d * 8 + 4:d * 8 + 8], in1=bias[:, d * 4:d * 4 + 4], op=ALU.add), r=[gt, bias], w=[xg])
            yield
            P.op("act", lambda e: e.activation(out=xg.ap, in_=xg.ap, func=AF.Exp, scale=-1.0), r=[xg], w=[xg])
            yield
            P.op("act", lambda e: e.activation(out=xg.ap, in_=xg.ap, func=AF.Ln, bias=1.0), r=[xg], w=[xg])
            yield
            P.op("dve", lambda e: e.tensor_scalar(out=lf.ap, in0=xg.ap, scalar1=-1.0, scalar2=None, op0=ALU.mult), r=[xg], w=[lf])
            yield
            self.decay_scalars(sc, lf.ap, [lf], 4, d, pool)
            yield
            P.op("dve", lambda e: e.tensor_tensor(out=ab[:, 0:4], in0=gt[:, d * 8:d * 8 + 4], in1=sc[:, 0:4], op=ALU.subtract), r=[gt, sc], w=[ab])
            P.op("dve", lambda e: e.tensor_tensor(out=ab[:, 4:8], in0=ab[:, 0:4], in1=sc[:, 4:8], op=ALU.add), r=[ab, sc], w=[ab])
            yield
            P.op("act", lambda e: e.activation(out=ab[:, 4:8], in_=ab[:, 4:8], func=AF.Exp), r=[ab], w=[ab])
            yield
            for h in range(4):
                hs = slice(h * 128, (h + 1) * 128)
                pS = self.bank(pool)
                P.op("pe", lambda e: e.matmul(pS[:, 0:128], lhsT=kTb[:, h, :], rhs=qTb[:, h, :], start=True, stop=True), r=[kTb, qTb], w=[pS])
                yield
                self.decay_matrix(lf[:, h:h + 1], [lf], ab[:, h:h + 1], [ab], d, lfB, Dm, pool)
                yield
                P.op("dve", lambda e: e.tensor_tensor(out=PT.ap, in0=pS[:, 0:128], in1=Dm.ap, op=ALU.mult), r=[pS, Dm], w=[PT])
                yield
                pO = self.bank(pool)
                P.op("pe", lambda e: e.matmul(pO[:, 0:129], lhsT=PT.ap, rhs=vaug[:, h, 0:129], start=True, stop=True), r=[PT, vaug], w=[pO])
                pI = self.bank(pool)
                P.op("pe", lambda e: e.matmul(pI[:, 0:129], lhsT=qTb[:, h, :], rhs=Cb[:, h, 0:129], start=True, stop=True), r=[qTb, Cb], w=[pI])
                yield
                P.op("dve", lambda e: e.tensor_scalar(out=tmpI[:, 0:129], in0=pI[:, 0:129], scalar1=sc[:, 16 + h:17 + h], scalar2=None, op0=ALU.mult), r=[pI, sc], w=[tmpI])
                yield
                P.op("dve", lambda e: e.tensor_tensor(out=tot[:, 0:129], in0=pO[:, 0:129], in1=tmpI[:, 0:129], op=ALU.add), r=[pO, tmpI], w=[tot])
                yield
                P.op("act", lambda e: e.activation(out=dn[:, 0:1], in_=tot[:, 128:129], func=AF.Abs), r=[tot], w=[dn])
                yield
                P.op("dve", lambda e: e.tensor_scalar(out=dn[:, 0:1], in0=dn[:, 0:1], scalar1=1.0, scalar2=None, op0=ALU.max), r=[dn], w=[dn])
                yield
                P.op("dve", lambda e: e.reciprocal(out=dn[:, 1:2], in_=dn[:, 0:1]), r=[dn], w=[dn])
                yield
                P.op("dve", lambda e: e.tensor_scalar(out=yo[:, i, hs], in0=tot[:, 0:128], scalar1=dn[:, 1:2], scalar2=None, op0=ALU.mult), r=[tot, dn], w=[yo])
                yield
                P.op("pool", lambda e: e.tensor_scalar(out=kw.ap, in0=kt[:, hs], scalar1=ab[:, 4 + h:5 + h], scalar2=None, op0=ALU.mult), r=[kt, ab], w=[kw])
                yield
                pU = self.bank(pool)
                P.op("pe", lambda e: e.matmul(pU[:, 0:129], lhsT=kw.ap, rhs=vaug[:, h, 0:129], start=True, stop=True), r=[kw, vaug], w=[pU])
                yield
                P.op("dve", lambda e: e.scalar_tensor_tensor(out=Cs[:, h, 0:129], in0=Cs[:, h, 0:129], scalar=sc[:, 20 + h:21 + h], in1=pU[:, 0:129], op0=ALU.mult, op1=ALU.add),
                     r=[Cs, sc, pU], w=[Cs])
                yield
                P.op("act", lambda e: e.copy(out=Cb[:, h, 0:129], in_=Cs[:, h, 0:129]), r=[Cs], w=[Cb])
                yield

    def phase_ssd(self, l):
        P = self.P
        T, NCX = self.T, self.n_ctx
        with ExitStack() as st:
            xbcT = self.sb(st, [128, 8, T], BF16)
            cw = self.sb(st, [128, 8, 3]); cb = self.sb(st, [128, 8])
            dtb = self.sb(st, [128, 16]); aneg = self.sb(st, [128, 16]); dsk = self.sb(st, [128, 8]); ng = self.sb(st, [128, 512])
            P.dma("sp", cw.ap, self.ssd_conv_w[l], w=[cw])
            P.dma("sp", cb.ap, self.ssd_conv_b[l], w=[cb])
            P.dma("sp", dtb.ap, self.ssd_dt_bias[l:l + 1, :].broadcast_to([128, 16]), w=[dtb])
            P.dma("sp", aneg.ap, self.ssd_a_log[l:l + 1, :].broadcast_to([128, 16]), w=[aneg])
            P.dma("sp", dsk.ap, self.ssd_d[l:l + 1, :].broadcast_to([128, 8]), w=[dsk])
            P.dma("sp", ng.ap, self.ssd_norm_g[l:l + 1, :].broadcast_to([128, 512]), w=[ng])
            P.op("act", lambda e: e.activation(out=aneg.ap, in_=aneg.ap, func=AF.Exp), r=[aneg], w=[aneg])
            P.op("dve", lambda e: e.tensor_scalar(out=aneg.ap, in0=aneg.ap, scalar1=-1.0, scalar2=None, op0=ALU.mult), r=[aneg], w=[aneg])
            with ExitStack() as s2:
                xin = [self.sb(s2, [128, T]) for _ in range(2)]
                acc = self.sb(s2, [128, T])
                for g in range(8):
                    x_ = xin[g % 2]
                    P.dma("sp", x_.ap, self.UF[O_XBC + g * 128:O_XBC + (g + 1) * 128, :], w=[x_])
                    P.op("dve", lambda e: e.tensor_scalar(out=acc.ap, in0=x_.ap, scalar1=cw[:, g, 1:2], scalar2=None, op0=ALU.mult), r=[x_, cw], w=[acc])
                    for (s0, s1) in ((0, NCX), (NCX, T)):
                        P.op("dve", lambda e: e.scalar_tensor_tensor(out=acc[:, s0 + 1:s1], in0=x_[:, s0:s1 - 1], scalar=cw[:, g, 0:1], in1=acc[:, s0 + 1:s1],
                                                                     op0=ALU.mult, op1=ALU.add), r=[x_, cw, acc], w=[acc])
                        P.op("dve", lambda e: e.scalar_tensor_tensor(out=acc[:, s0:s1 - 1], in0=x_[:, s0 + 1:s1], scalar=cw[:, g, 2:3], in1=acc[:, s0:s1 - 1],
                                                                     op0=ALU.mult, op1=ALU.add), r=[x_, cw, acc], w=[acc])
                    P.op("act", lambda e: e.activation(out=xbcT[:, g, :], in_=acc.ap, func=AF.Silu, bias=cb[:, g:g + 1], scale=1.0), r=[acc, cb], w=[xbcT])
                P.barrier()
            yf = self.sb(st, [128, self.NT, 512])
            Hs = self.sb(st, [128, 2, 256]); Hb = self.sb(st, [128, 2, 256], BF16)
            xtok = self.sb(st, [128, 512]); Btok = self.sb(st, [128, 256], BF16)
            xdt = self.sb(st, [128, 8, 64], BF16); xdtw = self.sb(st, [128, 8, 64], BF16)
            gt = self.sb(st, [128, 8]); dt = self.sb(st, [128, 8]); da = self.sb(st, [128, 8])
            sc = self.sb(st, [128, 48])
            lfB = self.sb(st, [128, 128]); Dm = self.sb(st, [128, 128]); PT = self.sb(st, [128, 128], BF16)
            tmpI = self.sb(st, [128, 512]); yd = self.sb(st, [128, 512]); zt = self.sb(st, [128, 512])
            junk = self.sb(st, [128, 512], BF16); ss = self.sb(st, [128, 4])
            pS, pI, pO = self.take_banks(3)
            for d in range(2):
                P.op("dve", lambda e: e.memset(Hs.ap, 0.0), w=[Hs])
                P.op("pool", lambda e: e.memset(Hb.ap, 0.0), w=[Hb])
                for i in self.order(d):
                    ts = slice(i * 128, (i + 1) * 128)
                    pb = self.bank()
                    for g in range(4):
                        P.op("pe", lambda e: e.matmul(pb[:, g * 128:(g + 1) * 128], lhsT=xbcT[:, g, ts], rhs=self.idb.ap, start=True, stop=True), r=[xbcT, self.idb], w=[pb])
                    P.op("act", lambda e: e.copy(out=xtok.ap, in_=pb[:, :]), r=[pb], w=[xtok])
                    pb = self.bank()
                    for g in range(2):
                        P.op("pe", lambda e: e.matmul(pb[:, g * 128:(g + 1) * 128], lhsT=xbcT[:, 4 + g, ts], rhs=self.idb.ap, start=True, stop=True), r=[xbcT, self.idb], w=[pb])
                    P.op("act", lambda e: e.copy(out=Btok.ap, in_=pb[:, 0:256]), r=[pb], w=[Btok])
                    P.dma("sp", gt.ap, self.UT[ts, O_DT + d * 8:O_DT + d * 8 + 8], w=[gt])
                    P.op("dve", lambda e: e.tensor_tensor(out=dt.ap, in0=gt.ap, in1=dtb[:, d * 8:d * 8 + 8], op=ALU.add), r=[gt, dtb], w=[dt])
                    P.op("act", lambda e: e.activation(out=dt.ap, in_=dt.ap, func=AF.Exp), r=[dt], w=[dt])
                    P.op("act", lambda e: e.activation(out=dt.ap, in_=dt.ap, func=AF.Ln, bias=1.0), r=[dt], w=[dt])
                    P.op("dve", lambda e: e.tensor_tensor(out=da.ap, in0=dt.ap, in1=aneg[:, d * 8:d * 8 + 8], op=ALU.mult), r=[dt, aneg], w=[da])
                    self.decay_scalars(sc, da.ap, [da], 8, d)
                    P.op("dve", lambda e: e.tensor_tensor(out=xdt.ap, in0=xtok.ap.rearrange("p (h c) -> p h c", h=8), in1=dt.ap.unsqueeze(2).broadcast_to([128, 8, 64]), op=ALU.mult),
                         r=[xtok, dt], w=[xdt])
                    P.op("pool", lambda e: e.tensor_tensor(out=xdtw.ap, in0=xdt.ap, in1=sc[:, 24:32].unsqueeze(2).broadcast_to([128, 8, 64]), op=ALU.mult), r=[xdt, sc], w=[xdtw])
                    for g in range(2):
                        P.op("pe", lambda e: e.matmul(pS[:, g * 128:(g + 1) * 128], lhsT=xbcT[:, 4 + g, ts], rhs=xbcT[:, 6 + g, ts], start=True, stop=True), r=[xbcT], w=[pS])
                        P.op("pe", lambda e: e.matmul(pI[:, g * 256:(g + 1) * 256], lhsT=xbcT[:, 6 + g, ts], rhs=Hb[:, g, :], start=True, stop=True), r=[xbcT, Hb], w=[pI])
                    P.op("dve", lambda e: e.tensor_tensor(out=tmpI.ap.rearrange("p (h c) -> p h c", h=8), in0=pI[:, :].rearrange("p (h c) -> p h c", h=8),
                                                          in1=sc[:, 32:40].unsqueeze(2).broadcast_to([128, 8, 64]), op=ALU.mult), r=[pI, sc], w=[tmpI])
                    for j in range(8):
                        g = j // 4
                        self.decay_matrix(da[:, j:j + 1], [da], sc[:, 16 + j:17 + j], [sc], d, lfB, Dm)
                        P.op("dve", lambda e: e.tensor_tensor(out=PT.ap, in0=pS[:, g * 128:(g + 1) * 128], in1=Dm.ap, op=ALU.mult), r=[pS, Dm], w=[PT])
                        P.op("pe", lambda e: e.matmul(pO[:, j * 64:(j + 1) * 64], lhsT=PT.ap, rhs=xdt[:, j, :], start=True, stop=True), r=[PT, xdt], w=[pO])
                    if d == 0:
                        P.op("dve", lambda e: e.tensor_tensor(out=yf[:, i, :], in0=pO[:, :], in1=tmpI.ap, op=ALU.add), r=[pO, tmpI], w=[yf])
                    else:
                        P.op("dve", lambda e: e.tensor_tensor(out=yd.ap, in0=pO[:, :], in1=tmpI.ap, op=ALU.add), r=[pO, tmpI], w=[yd])
                    pU = self.bank()
                    for g in range(2):
                        P.op("pe", lambda e: e.matmul(pU[:, g * 256:(g + 1) * 256], lhsT=Btok[:, g * 128:(g + 1) * 128], rhs=xdtw[:, g * 4:(g + 1) * 4, :].rearrange("p h c -> p (h c)"),
                                                      start=True, stop=True), r=[Btok, xdtw], w=[pU])
                    P.op("dve", lambda e: e.tensor_tensor(out=Hs.ap.rearrange("p g (h c) -> p (g h) c", h=4), in0=Hs.ap.rearrange("p g (h c) -> p (g h) c", h=4),
                                                          in1=sc[:, 40:48].unsqueeze(2).broadcast_to([128, 8, 64]), op=ALU.mult), r=[Hs, sc], w=[Hs])
                    P.op("dve", lambda e: e.tensor_tensor(out=Hs.ap.rearrange("p g c -> p (g c)"), in0=Hs.ap.rearrange("p g c -> p (g c)"), in1=pU[:, :], op=ALU.add), r=[Hs, pU], w=[Hs])
                    P.op("act", lambda e: e.copy(out=Hb.ap, in_=Hs.ap), r=[Hs], w=[Hb])
                    if d == 1:
                        P.dma("sp", zt.ap, self.UT[ts, O_SZ:O_SZ + 512], w=[zt])
                        P.op("dve", lambda e: e.tensor_tensor(out=yd.ap, in0=yd.ap, in1=yf[:, i, :], op=ALU.add), r=[yd, yf], w=[yd])
                        P.op("dve", lambda e: e.tensor_tensor(out=tmpI.ap.rearrange("p (h c) -> p h c", h=8), in0=xtok.ap.rearrange("p (h c) -> p h c", h=8),
                                                              in1=dsk.ap.unsqueeze(2).broadcast_to([128, 8, 64]), op=ALU.mult), r=[xtok, dsk], w=[tmpI])
                        P.op("dve", lambda e: e.tensor_tensor(out=yd.ap, in0=yd.ap, in1=tmpI.ap, op=ALU.add), r=[yd, tmpI], w=[yd])
                        P.op("act", lambda e: e.activation(out=zt.ap, in_=zt.ap, func=AF.Silu), r=[zt], w=[zt])
                        P.op("dve", lambda e: e.tensor_tensor(out=yd.ap, in0=yd.ap, in1=zt.ap, op=ALU.mult), r=[yd, zt], w=[yd])
                        self.rstd_of(yd.ap, [yd], 512, junk.ap, ss)
                        P.op("dve", lambda e: e.scalar_tensor_tensor(out=yd.ap, in0=yd.ap, scalar=ss[:, 3:4], in1=ng.ap, op0=ALU.mult, op1=ALU.mult), r=[yd, ss, ng], w=[yd])
                        P.dma("sp", self.BR[3, ts, :], yd.ap, r=[yd])
            self.release_banks()

    def phase_hg(self, l):
        P = self.P
        L = self.depth
        with ExitStack() as st:
            lg = self.sb(st, [128, L + 1, 4]); lbT = self.sb(st, [128, 4]); omT = self.sb(st, [128, 4]); smT = self.sb(st, [128, 4])
            omB = self.sb(st, [128, 512]); ngB = self.sb(st, [128, 512])
            P.dma("sp", lg.ap, self.hg_lbT, w=[lg])
            P.dma("sp", ngB.ap, self.hg_norm_g[l:l + 1, :].broadcast_to([128, 512]), w=[ngB])
            P.op("act", lambda e: e.activation(out=lg.ap, in_=lg.ap, func=AF.Exp), r=[lg], w=[lg])
            P.op("dve", lambda e: e.tensor_tensor(out=smT.ap, in0=lg[:, 0, :], in1=lg[:, 1, :], op=ALU.add), r=[lg], w=[smT])
            for j in range(2, L + 1):
                P.op("dve", lambda e: e.tensor_tensor(out=smT.ap, in0=smT.ap, in1=lg[:, j, :], op=ALU.add), r=[lg, smT], w=[smT])
            P.op("dve", lambda e: e.reciprocal(out=smT.ap, in_=smT.ap), r=[smT], w=[smT])
            P.op("dve", lambda e: e.tensor_copy(out=lbT.ap, in_=lg[:, 0, :]), r=[lg], w=[lbT])
            for j in range(1, l + 1):
                P.op("dve", lambda e: e.tensor_tensor(out=lbT.ap, in0=lbT.ap, in1=lg[:, j, :], op=ALU.add), r=[lg, lbT], w=[lbT])
            P.op("dve", lambda e: e.tensor_tensor(out=lbT.ap, in0=lbT.ap, in1=smT.ap, op=ALU.mult), r=[lbT, smT], w=[lbT])
            P.op("dve", lambda e: e.tensor_scalar(out=omT.ap, in0=lbT.ap, scalar1=-1.0, scalar2=1.0, op0=ALU.mult, op1=ALU.add), r=[lbT], w=[omT])
            P.op("dve", lambda e: e.tensor_scalar(out=omB.ap, in0=self.lbB[:, l, :], scalar1=-1.0, scalar2=1.0, op0=ALU.mult, op1=ALU.add), r=[self.lbB], w=[omB])
            ydir = [self.sb(st, [128, self.NT, 512]) for _ in range(2)]
            self.drive([self.hg_chain(st, l, d, omT, omB, ydir[d]) for d in range(2)])
            ys = self.sb(st, [128, 512]); sq = self.sb(st, [128, 512]); rs = self.sb(st, [128, 8]); gtk = self.sb(st, [128, 512])
            for i in range(self.NT):
                ts = slice(i * 128, (i + 1) * 128)
                P.dma("sp", gtk.ap, self.UT[ts, O_HGG:O_HGG + 512], w=[gtk])
                P.op("dve", lambda e: e.tensor_tensor(out=ys.ap, in0=ydir[0][:, i, :], in1=ydir[1][:, i, :], op=ALU.add), r=[ydir[0], ydir[1]], w=[ys])
                P.op("dve", lambda e: e.tensor_tensor(out=sq.ap, in0=ys.ap, in1=ys.ap, op=ALU.mult), r=[ys], w=[sq])
                P.op("dve", lambda e: e.tensor_reduce(out=rs[:, 0:4], in_=sq.ap.rearrange("p (h c) -> p h c", h=4), axis=AX.X, op=ALU.add), r=[sq], w=[rs])
                P.op("dve", lambda e: e.tensor_scalar(out=rs[:, 0:4], in0=rs[:, 0:4], scalar1=1.0 / 128, scalar2=EPS, op0=ALU.mult, op1=ALU.add), r=[rs], w=[rs])
                P.op("act", lambda e: e.activation(out=rs[:, 0:4], in_=rs[:, 0:4], func=AF.Sqrt), r=[rs], w=[rs])
                P.op("dve", lambda e: e.reciprocal(out=rs[:, 4:8], in_=rs[:, 0:4]), r=[rs], w=[rs])
                P.op("dve", lambda e: e.tensor_tensor(out=ys.ap.rearrange("p (h c) -> p h c", h=4), in0=ys.ap.rearrange("p (h c) -> p h c", h=4),
                                                      in1=rs[:, 4:8].unsqueeze(2).broadcast_to([128, 4, 128]), op=ALU.mult), r=[ys, rs], w=[ys])
                P.op("dve", lambda e: e.tensor_tensor(out=ys.ap, in0=ys.ap, in1=ngB.ap, op=ALU.mult), r=[ys, ngB], w=[ys])
                P.op("act", lambda e: e.activation(out=gtk.ap, in_=gtk.ap, func=AF.Sigmoid), r=[gtk], w=[gtk])
                P.op("dve", lambda e: e.tensor_tensor(out=ys.ap, in0=ys.ap, in1=gtk.ap, op=ALU.mult), r=[ys, gtk], w=[ys])
                P.dma("sp", self.BR[1, ts, :], ys.ap, r=[ys])

    def hg_chain(self, st, l, d, omT, omB, yo):
        P = self.P
        pool = {"banks": list(range(4 * d, 4 * d + 4)), "rr": 0}
        Ss = self.sb(st, [128, 4, 128]); Sb = self.sb(st, [128, 4, 128], BF16)
        qT = self.sb(st, [128, 4, 128]); fT = self.sb(st, [128, 4, 128]); kTf = self.sb(st, [128, 4, 128])
        ft = self.sb(st, [128, 512]); lf = self.sb(st, [128, 512]); vt = self.sb(st, [128, 512]); vb = self.sb(st, [128, 512], BF16)
        E = self.sb(st, [128, 128]); Ei = self.sb(st, [128, 128]); ec = self.sb(st, [128, 4])
        qtl = self.sb(st, [128, 128], BF16); ktl = self.sb(st, [128, 128], BF16); ktok = self.sb(st, [128, 128], BF16)
        Af = self.sb(st, [128, 128]); AT = self.sb(st, [128, 128], BF16)
        lc = C_LCF if d == 0 else C_LCB
        tri = C_TRF if d == 0 else C_TRB
        hc0 = 0 if d == 0 else 3
        P.op("dve", lambda e: e.memset(Ss.ap, 0.0), w=[Ss])
        yield
        for i in self.order(d):
            ts = slice(i * 128, (i + 1) * 128)
            P.dma("sp", qT.ap, self.UF[O_HGQ:O_HGQ + 512, ts].rearrange("(h p) t -> p h t", p=128), w=[qT])
            P.dma("sp", fT.ap, self.UF[O_HGF + d * 512:O_HGF + (d + 1) * 512, ts].rearrange("(h p) t -> p h t", p=128), w=[fT])
            P.dma("sp", ft.ap, self.UT[ts, O_HGF + d * 512:O_HGF + (d + 1) * 512], w=[ft])
            P.dma("sp", vt.ap, self.UT[ts, O_HGI:O_HGI + 512], w=[vt])
            yield
            P.op("act", lambda e: e.activation(out=qT.ap, in_=qT.ap, func=AF.Silu), r=[qT], w=[qT])
            yield
            P.op("act", lambda e: e.activation(out=kTf.ap, in_=fT.ap, func=AF.Sigmoid, scale=-1.0), r=[fT], w=[kTf])
            yield
            for h in range(4):
                P.op("pool", lambda e: e.tensor_scalar(out=kTf[:, h, :], in0=kTf[:, h, :], scalar1=omT[:, h:h + 1], scalar2=None, op0=ALU.mult), r=[kTf, omT], w=[kTf])
            yield
            P.op("act", lambda e: e.activation(out=ft.ap, in_=ft.ap, func=AF.Sigmoid), r=[ft], w=[ft])
            yield
            P.op("dve", lambda e: e.tensor_tensor(out=ft.ap, in0=ft.ap, in1=omB.ap, op=ALU.mult), r=[ft, omB], w=[ft])
            yield
            P.op("dve", lambda e: e.tensor_tensor(out=ft.ap, in0=ft.ap, in1=self.lbB[:, l, :], op=ALU.add), r=[ft, self.lbB], w=[ft])
            yield
            P.op("act", lambda e: e.activation(out=lf.ap, in_=ft.ap, func=AF.Ln), r=[ft], w=[lf])
            P.op("pool", lambda e: e.tensor_copy(out=vb.ap, in_=vt.ap), r=[vt], w=[vb])
            yield
            for h in range(4):
                hs = slice(h * 128, (h + 1) * 128)
                pG = self.bank(pool)
                P.op("pe", lambda e: e.matmul(pG[:, 0:128], lhsT=lf[:, hs], rhs=self.cst[:, lc, :], start=True, stop=True), r=[lf, self.cst], w=[pG])
                P.op("pe", lambda e: e.matmul(pG[:, 128:132], lhsT=lf[:, hs], rhs=self.cst[:, C_HC, hc0:hc0 + 4], start=True, stop=True), r=[lf, self.cst], w=[pG])
                yield
                P.op("act", lambda e: e.activation(out=E.ap, in_=pG[:, 0:128], func=AF.Exp), r=[pG], w=[E])
                yield
                P.op("act", lambda e: e.activation(out=Ei.ap, in_=pG[:, 0:128], func=AF.Exp, scale=-1.0), r=[pG], w=[Ei])
                yield
                P.op("act", lambda e: e.activation(out=ec.ap, in_=pG[:, 128:132], func=AF.Exp), r=[pG], w=[ec])
                yield
                P.op("dve", lambda e: e.tensor_tensor(out=qtl.ap, in0=qT[:, h, :], in1=E.ap, op=ALU.mult), r=[qT, E], w=[qtl])
                yield
                P.op("dve", lambda e: e.tensor_tensor(out=ktl.ap, in0=kTf[:, h, :], in1=Ei.ap, op=ALU.mult), r=[kTf, Ei], w=[ktl])
                yield
                P.op("dve", lambda e: e.tensor_scalar(out=Sb[:, h, :], in0=Ss[:, h, :], scalar1=ec[:, 0:1], scalar2=None, op0=ALU.mult), r=[Ss, ec], w=[Sb])
                yield
                pA = self.bank(pool)
                P.op("pe", lambda e: e.matmul(pA[:, 0:128], lhsT=ktl.ap, rhs=qtl.ap, start=True, stop=True), r=[ktl, qtl], w=[pA])
                pK = self.bank(pool)
                P.op("pe", lambda e: e.matmul(pK[:, 0:128], lhsT=ktl.ap, rhs=self.idb.ap, start=True, stop=True), r=[ktl, self.idb], w=[pK])
                yield
                P.op("dve", lambda e: e.tensor_scalar(out=Af.ap, in0=pA[:, 0:128], scalar1=-1e30, scalar2=1e30, op0=ALU.max, op1=ALU.min), r=[pA], w=[Af])
                P.op("act", lambda e: e.copy(out=ktok.ap, in_=pK[:, 0:128]), r=[pK], w=[ktok])
                yield
                P.op("dve", lambda e: e.tensor_tensor(out=AT.ap, in0=Af.ap, in1=self.cst[:, tri, :], op=ALU.mult), r=[Af, self.cst], w=[AT])
                yield
                pO = self.bank(pool)
                P.op("pe", lambda e: e.matmul(pO[:, 0:128], lhsT=AT.ap, rhs=vb[:, hs], start=True, stop=False), r=[AT, vb], w=[pO])
                P.op("pe", lambda e: e.matmul(pO[:, 0:128], lhsT=qtl.ap, rhs=Sb[:, h, :], start=False, stop=True), r=[qtl, Sb], w=[pO])
                yield
                P.op("act", lambda e: e.copy(out=yo[:, i, hs], in_=pO[:, 0:128]), r=[pO], w=[yo])
                yield
                pU = self.bank(pool)
                P.op("pe", lambda e: e.matmul(pU[:, 0:128], lhsT=ktok.ap, rhs=vb[:, hs], start=True, stop=True), r=[ktok, vb], w=[pU])
                yield
                P.op("dve", lambda e: e.tensor_scalar(out=Ss[:, h, :], in0=Ss[:, h, :], scalar1=ec[:, 1:2], scalar2=None, op0=ALU.mult), r=[Ss, ec], w=[Ss])
                yield
                P.op("dve", lambda e: e.scalar_tensor_tensor(out=Ss[:, h, :], in0=pU[:, 0:128], scalar=ec[:, 2:3], in1=Ss[:, h, :], op0=ALU.mult, op1=ALU.add), r=[pU, ec, Ss], w=[Ss])
                yield

    def phase_mla(self, l):
        P = self.P
        T, NT, NCX = self.T, self.NT, self.n_ctx
        with ExitStack() as st:
            qnT = self.sb(st, [128, 4, T], BF16); qrT = self.sb(st, [128, 4, T], BF16)
            knT = self.sb(st, [128, 4, T], BF16); krT = self.sb(st, [128, T], BF16)
            vaug = self.sb(st, [128, NT, 4, 132], BF16)
            P.op("pool", lambda e: e.memset(vaug.ap, 1.0), w=[vaug])
            with ExitStack() as s2:
                gq = self.sb(s2, [128, 512]); gkv = self.sb(s2, [128, 256])
                P.dma("sp", gq.ap, self.mla_g_q[l:l + 1, :].broadcast_to([128, 512]), w=[gq])
                P.dma("sp", gkv.ap, self.mla_g_kv[l:l + 1, :].broadcast_to([128, 256]), w=[gkv])
                cqT = self.sb(s2, [128, 4, T], BF16); ckvT = self.sb(s2, [128, 2, T], BF16)
                cosT = self.sb(s2, [128, T]); sinT = self.sb(s2, [128, T])
                P.op("dve", lambda e: e.memset(cosT.ap, 0.0), w=[cosT])
                P.op("dve", lambda e: e.memset(sinT.ap, 0.0), w=[sinT])
                P.dma("sp", cosT[0:64, :], self.rope_in[0:64, :], w=[cosT])
                P.dma("sp", sinT[0:64, :], self.rope_in[64:128, :], w=[sinT])
                permb = self.sb(s2, [128, 128], BF16)
                P.op("dve", lambda e: e.tensor_copy(out=permb.ap, in_=self.cst[:, C_PERM, :]), r=[self.cst], w=[permb])
                wst = self.sb(s2, [128, 4, 768]); Wuq = self.sb(s2, [128, 4, 768], BF16)
                wst2 = self.sb(s2, [128, 2, 1024]); Wukv = self.sb(s2, [128, 2, 1024], BF16)
                Wr = self.sb(s2, [128, 4, 4, 128], BF16)
                P.dma("sp", wst.ap, self.mla_w_uq[l].rearrange("(k p) n -> p k n", p=128), w=[wst])
                P.dma("sp", wst2.ap, self.mla_w_ukv[l].rearrange("(k p) n -> p k n", p=128), w=[wst2])
                P.op("dve", lambda e: e.tensor_copy(out=Wuq.ap, in_=wst.ap), r=[wst], w=[Wuq])
                P.op("dve", lambda e: e.tensor_copy(out=Wukv.ap, in_=wst2.ap), r=[wst2], w=[Wukv])
                P.op("dve", lambda e: e.memset(Wr.ap, 0.0), w=[Wr])
                for h in range(4):
                    for kc in range(4):
                        P.op("dve", lambda e: e.tensor_copy(out=Wr[:, kc, h, 0:64], in_=wst[:, kc, h * 192 + 128:(h + 1) * 192]), r=[wst], w=[Wr])
                ct = self.sb(s2, [128, 768]); cb_ = self.sb(s2, [128, 768], BF16); junk = self.sb(s2, [128, 512], BF16)
                ss = self.sb(s2, [128, 4]); ss2 = self.sb(s2, [128, 4])
                for i in range(NT):
                    ts = slice(i * 128, (i + 1) * 128)
                    P.dma("sp", ct.ap, self.UT[ts, O_CQ:O_CQ + 768], w=[ct])
                    self.rstd_of(ct[:, 0:512], [ct], 512, junk.ap, ss)
                    self.rstd_of(ct[:, 512:768], [ct], 256, junk[:, 0:256], ss2)
                    P.op("dve", lambda e: e.scalar_tensor_tensor(out=cb_[:, 0:512], in0=ct[:, 0:512], scalar=ss[:, 3:4], in1=gq.ap, op0=ALU.mult, op1=ALU.mult), r=[ct, ss, gq], w=[cb_])
                    P.op("dve", lambda e: e.scalar_tensor_tensor(out=cb_[:, 512:768], in0=ct[:, 512:768], scalar=ss2[:, 3:4], in1=gkv.ap, op0=ALU.mult, op1=ALU.mult), r=[ct, ss2, gkv], w=[cb_])

                    def dst(q, nb, pb, i=i):
                        for j in range(nb):
                            blk = q + j
                            tgt = cqT[:, blk, i * 128:(i + 1) * 128] if blk < 4 else ckvT[:, blk - 4, i * 128:(i + 1) * 128]
                            P.op("act", lambda e: e.copy(out=tgt, in_=pb[:, j * 128:(j + 1) * 128]), r=[pb], w=[cqT if blk < 4 else ckvT])
                    self.transpose_to(cb_, 768, dst)
                if os.environ.get("MLA_STOP") == "A":
                    return
                xb = self.sb(s2, [128, 512], BF16); t1 = self.sb(s2, [128, 512]); t2 = self.sb(s2, [128, 512])
                xk = self.sb(s2, [128, T])

                def rope(src_ap, src_dep, out_ap, out_tl, t0, n):
                    P.op("act", lambda e: e.copy(out=xb[:, 0:n], in_=src_ap), r=src_dep, w=[xb])
                    pP = self.bank()
                    P.op("pe", lambda e: e.matmul(pP[:, 0:n], lhsT=permb.ap, rhs=xb[:, 0:n], start=True, stop=True), r=[permb, xb], w=[pP])
                    P.op("dve", lambda e: e.tensor_tensor(out=t1[:, 0:n], in0=src_ap, in1=cosT[:, t0:t0 + n], op=ALU.mult), r=src_dep + [cosT], w=[t1])
                    P.op("dve", lambda e: e.tensor_tensor(out=t2[:, 0:n], in0=pP[:, 0:n], in1=sinT[:, t0:t0 + n], op=ALU.mult), r=[pP, sinT], w=[t2])
                    P.op("dve", lambda e: e.tensor_tensor(out=out_ap, in0=t1[:, 0:n], in1=t2[:, 0:n], op=ALU.add), r=[t1, t2], w=[out_tl])

                P.op("dve", lambda e: e.memset(xk.ap, 0.0), w=[xk])
                P.dma("sp", xk[0:64, :], self.UF[O_KR:O_KR + 64, :], w=[xk])
                for (t0, n) in self.tok_blocks(0, T):
                    rope(xk[:, t0:t0 + n], [xk], krT[:, t0:t0 + n], krT, t0, n)
                    if os.environ.get("MLA_STOP") == "B1":
                        return
                    for h in range(4):
                        pq = self.bank()
                        for kc in range(4):
                            P.op("pe", lambda e: e.matmul(pq[:, 0:n], lhsT=Wuq[:, kc, h * 192:h * 192 + 128], rhs=cqT[:, kc, t0:t0 + n], start=(kc == 0), stop=(kc == 3)), r=[Wuq, cqT], w=[pq])
                        P.op("act", lambda e: e.copy(out=qnT[:, h, t0:t0 + n], in_=pq[:, 0:n]), r=[pq], w=[qnT])
                        if os.environ.get("MLA_STOP") == "B2a":
                            return
                        pr = self.bank()
                        for kc in range(4):
                            P.op("pe", lambda e: e.matmul(pr[:, 0:n], lhsT=Wr[:, kc, h, :], rhs=cqT[:, kc, t0:t0 + n], start=(kc == 0), stop=(kc == 3)), r=[Wr, cqT], w=[pr])
                        rope(pr[:, 0:n], [pr], qrT[:, h, t0:t0 + n], qrT, t0, n)
                        if os.environ.get("MLA_STOP") == "B2b":
                            return
                        pk = self.bank()
                        for kc in range(2):
                            P.op("pe", lambda e: e.matmul(pk[:, 0:n], lhsT=Wukv[:, kc, h * 256:h * 256 + 128], rhs=ckvT[:, kc, t0:t0 + n], start=(kc == 0), stop=(kc == 1)), r=[Wukv, ckvT], w=[pk])
                        P.op("act", lambda e: e.copy(out=knT[:, h, t0:t0 + n], in_=pk[:, 0:n]), r=[pk], w=[knT])
                if os.environ.get("MLA_STOP") == "B2":
                    return
                for i in range(NT):
                    pv = self.bank()
                    for h in range(4):
                        for kc in range(2):
                            P.op("pe", lambda e: e.matmul(pv[:, h * 128:(h + 1) * 128], lhsT=ckvT[:, kc, i * 128:(i + 1) * 128], rhs=Wukv[:, kc, h * 256 + 128:(h + 1) * 256],
                                                          start=(kc == 0), stop=(kc == 1)), r=[ckvT, Wukv], w=[pv])
                    P.op("act", lambda e: e.copy(out=vaug[:, i, :, 0:128], in_=pv[:, :].rearrange("p (h c) -> p h c", h=4)), r=[pv], w=[vaug])
                P.barrier()
            if os.environ.get("MLA_STOP") == "B":
                return
            (acc,) = self.take_banks(1)
            PTe = [self.sb(st, [128, 128], BF16) for _ in range(2)]
            ob = [self.sb(st, [128, 512]) for _ in range(2)]
            rc = self.sb(st, [128, 4])
            scale = float((128 + 64) ** -0.5)
            qtiles = [(i, list(range(self.NCT))) for i in range(self.NCT)] + [(i, list(range(NT))) for i in range(self.NCT, NT)]
            pi = 0
            for oi, (qi, ktl) in enumerate(qtiles):
                qsl = slice(qi * 128, (qi + 1) * 128)
                o_ = ob[oi % 2]
                for h in range(4):
                    for kt in ktl:
                        ks = slice(kt * 128, (kt + 1) * 128)
                        pST = self.bank()
                        P.op("pe", lambda e: e.matmul(pST[:, 0:128], lhsT=knT[:, h, ks], rhs=qnT[:, h, qsl], start=True, stop=False), r=[knT, qnT], w=[pST])
                        P.op("pe", lambda e: e.matmul(pST[:, 0:128], lhsT=krT[:, ks], rhs=qrT[:, h, qsl], start=False, stop=True), r=[krT, qrT], w=[pST])
                        pt = PTe[pi % 2]
                        pi += 1
                        P.op("act", lambda e: e.activation(out=pt.ap, in_=pST[:, 0:128], func=AF.Exp, scale=scale), r=[pST], w=[pt])
                        P.op("pe", lambda e: e.matmul(acc[:, 0:129], lhsT=pt.ap, rhs=vaug[:, kt, h, 0:129], start=(kt == ktl[0]), stop=(kt == ktl[-1])), r=[pt, vaug], w=[acc])
                    P.op("dve", lambda e: e.reciprocal(out=rc[:, h:h + 1], in_=acc[:, 128:129]), r=[acc], w=[rc])
                    P.op("dve", lambda e: e.tensor_scalar(out=o_[:, h * 128:(h + 1) * 128], in0=acc[:, 0:128], scalar1=rc[:, h:h + 1], scalar2=None, op0=ALU.mult), r=[acc, rc], w=[o_])
                P.dma("sp", self.BR[2, qsl, :], o_.ap, r=[o_])
            self.release_banks()

    def phase_merge(self, l):
        P = self.P
        T, NT = self.T, self.NT
        with ExitStack() as st:
            self.load_hT(st)
            brT = [self.sb(st, [128, 4, T], BF16) for _ in range(4)]
            with ExitStack() as s2:
                bt = self.sb(s2, [128, 512]); bb = self.sb(s2, [128, 512], BF16)
                for k in range(4):
                    for i in range(NT):
                        P.dma("sp", bt.ap, self.BR[k, i * 128:(i + 1) * 128, :], w=[bt])
                        P.op("dve", lambda e: e.tensor_copy(out=bb.ap, in_=bt.ap), r=[bt], w=[bb])

                        def dst(q, nb, pb, i=i, k=k):
                            P.op("act", lambda e: e.copy(out=brT[k][:, q:q + nb, i * 128:(i + 1) * 128],
                                                         in_=pb[:, 0:nb * 128].rearrange("p (a t) -> p a t", a=nb)), r=[pb], w=[brT[k]])
                        self.transpose_to(bb, 512, dst)
                P.barrier()
            wg = self.wpipe(st, KC, 128)
            wb_ = self.wpipe(st, 4, 128)
            acc = self.sb(st, [128, T]); accb = self.sb(st, [128, T], BF16)
            gs = self.sb(st, [128, 512]); tmp = self.sb(st, [128, 512])
            for n in range(16):
                ns = slice(n * 128, (n + 1) * 128)
                for k in range(4):
                    Wg = self.wload(wg, self.w_gate[l, k, :, ns], KC, 128)
                    Wb = self.wload(wb_, self.w_br[l, k, :, ns], 4, 128)
                    for (t0, nt) in self.tok_blocks(0, T):
                        pg = self.bank()
                        for kc in range(KC):
                            P.op("pe", lambda e: e.matmul(pg[:, 0:nt], lhsT=Wg[:, kc, :], rhs=self.hT[:, kc, t0:t0 + nt], start=(kc == 0), stop=(kc == KC - 1)), r=[Wg, self.hT], w=[pg])
                        P.op("act", lambda e: e.activation(out=gs[:, 0:nt], in_=pg[:, 0:nt], func=AF.Sigmoid), r=[pg], w=[gs])
                        pbk = self.bank()
                        for kc in range(4):
                            P.op("pe", lambda e: e.matmul(pbk[:, 0:nt], lhsT=Wb[:, kc, :], rhs=brT[k][:, kc, t0:t0 + nt], start=(kc == 0), stop=(kc == 3)), r=[Wb, brT[k]], w=[pbk])
                        if k == 0:
                            P.op("dve", lambda e: e.tensor_tensor(out=acc[:, t0:t0 + nt], in0=pbk[:, 0:nt], in1=gs[:, 0:nt], op=ALU.mult), r=[pbk, gs], w=[acc])
                        else:
                            P.op("dve", lambda e: e.tensor_tensor(out=tmp[:, 0:nt], in0=pbk[:, 0:nt], in1=gs[:, 0:nt], op=ALU.mult), r=[pbk, gs], w=[tmp])
                            P.op("dve", lambda e: e.tensor_tensor(out=acc[:, t0:t0 + nt], in0=acc[:, t0:t0 + nt], in1=tmp[:, 0:nt], op=ALU.add), r=[acc, tmp], w=[acc])
                P.op("act", lambda e: e.copy(out=accb.ap, in_=acc.ap), r=[acc], w=[accb])
                P.dma("sp", self.ACC[ns, :], accb.ap, r=[accb])

    def phase_wout(self, l):
        P = self.P
        T, NT = self.T, self.NT
        with ExitStack() as st:
            accT = self.sb(st, [128, KC, T], BF16)
            P.dma("sp", accT.ap, self.ACC.rearrange("(k p) t -> p k t", p=128), w=[accT])
            wp = self.wpipe(st, KC, 256)
            ev = [self.sb(st, [128, 256]) for _ in range(2)]
            ei = 0
            for c in range(8):
                Wo = self.wload(wp, self.w_out[l, :, c * 256:(c + 1) * 256], KC, 256)
                for i in range(NT):
                    pb = self.bank()
                    for kc in range(KC):
                        P.op("pe", lambda e: e.matmul(pb[:, 0:256], lhsT=accT[:, kc, i * 128:(i + 1) * 128], rhs=Wo[:, kc, :], start=(kc == 0), stop=(kc == KC - 1)), r=[accT, Wo], w=[pb])
                    e_ = ev[ei % 2]
                    ei += 1
                    P.op("act", lambda e: e.copy(out=e_.ap, in_=pb[:, 0:256]), r=[pb], w=[e_])
                    P.dma("sp", self.Y[i * 128:(i + 1) * 128, c * 256:(c + 1) * 256], e_.ap, r=[e_])
            P.barrier()
        self.residual(l, "G2")
        P.barrier()
        self.norm_mod_to_hT(l, "A2", "B2")

    def residual(self, l, kind):
        P = self.P
        with ExitStack() as st:
            G = self.load_modvec(st, l, kind)
            yt = [self.sb(st, [128, D]) for _ in range(2)]
            xt = [self.sb(st, [128, D]) for _ in range(2)]
            junk = self.sb(st, [128, D], BF16)
            ss = [self.sb(st, [128, 4]) for _ in range(2)]
            for i in range(self.NT):
                j = 1 if i < self.NCT else 0
                y_, x_, s_ = yt[i % 2], xt[i % 2], ss[i % 2]
                P.dma("sp", y_.ap, self.Y[i * 128:(i + 1) * 128, :], w=[y_])
                P.dma("sp", x_.ap, self.xres[i * 128:(i + 1) * 128, :], w=[x_])
                self.rstd_of(y_.ap, [y_], D, junk.ap, s_)
                P.op("dve", lambda e: e.scalar_tensor_tensor(out=y_.ap, in0=y_.ap, scalar=s_[:, 3:4], in1=G[:, j, :], op0=ALU.mult, op1=ALU.mult), r=[y_, s_, G], w=[y_])
                P.op("dve", lambda e: e.tensor_tensor(out=x_.ap, in0=x_.ap, in1=y_.ap, op=ALU.add), r=[x_, y_], w=[x_])
                P.dma("sp", self.xres[i * 128:(i + 1) * 128, :], x_.ap, r=[x_])

    def phase_moe(self, l):
        P = self.P
        T, NT = self.T, self.NT
        NE = 1 if self.tiny_moe else 32
        PTK = min(NT, 9)
        with ExitStack() as st:
            comb = self.sb(st, [128, NT, 32])
            with ExitStack() as s2:
                self.load_hT(s2)
                ws = self.sb(s2, [128, KC, 36]); wr = self.sb(s2, [128, KC, 36], BF16)
                P.dma("sp", ws[:, :, 0:4], self.w_grp[l].rearrange("(k p) n -> p k n", p=128), w=[ws])
                P.dma("sp", ws[:, :, 4:36], self.w_exp[l].rearrange("(k p) n -> p k n", p=128), w=[ws])
                P.op("dve", lambda e: e.tensor_copy(out=wr.ap, in_=ws.ap), r=[ws], w=[wr])
                lg = self.sb(s2, [128, 36]); sc = self.sb(s2, [128, 16]); gm = self.sb(s2, [128, 4]); ge = self.sb(s2, [128, 4])
                t48 = self.sb(s2, [128, 4, 8]); sel = self.sb(s2, [128, 8]); sel2 = self.sb(s2, [128, 8]); e1 = self.sb(s2, [128, 8]); e2 = self.sb(s2, [128, 8]); c8 = self.sb(s2, [128, 8])
                for i in range(NT):
                    pb = self.bank()
                    for kc in range(KC):
                        P.op("pe", lambda e: e.matmul(pb[:, 0:36], lhsT=self.hT[:, kc, i * 128:(i + 1) * 128], rhs=wr[:, kc, :], start=(kc == 0), stop=(kc == KC - 1)), r=[self.hT, wr], w=[pb])
                    P.op("dve", lambda e: e.tensor_copy(out=lg.ap, in_=pb[:, 0:36]), r=[pb], w=[lg])
                    P.op("dve", lambda e: e.tensor_reduce(out=sc[:, 0:1], in_=lg[:, 0:4], axis=AX.X, op=ALU.max), r=[lg], w=[sc])
                    P.op("dve", lambda e: e.tensor_scalar(out=sc[:, 1:2], in0=sc[:, 0:1], scalar1=-1.0, scalar2=None, op0=ALU.mult), r=[sc], w=[sc])
                    P.op("act", lambda e: e.activation(out=ge.ap, in_=lg[:, 0:4], func=AF.Exp, bias=sc[:, 1:2], scale=1.0), r=[lg, sc], w=[ge])
                    P.op("dve", lambda e: e.tensor_reduce(out=sc[:, 2:3], in_=ge.ap, axis=AX.X, op=ALU.add), r=[ge], w=[sc])
                    P.op("dve", lambda e: e.reciprocal(out=sc[:, 3:4], in_=sc[:, 2:3]), r=[sc], w=[sc])
                    P.op("dve", lambda e: e.tensor_scalar(out=gm.ap, in0=lg[:, 0:4], scalar1=sc[:, 0:1], scalar2=None, op0=ALU.is_equal), r=[lg, sc], w=[gm])
                    P.op("dve", lambda e: e.tensor_tensor(out=t48.ap, in0=lg[:, 4:36].rearrange("p (g e) -> p g e", g=4), in1=gm.ap.unsqueeze(2).broadcast_to([128, 4, 8]), op=ALU.mult), r=[lg, gm], w=[t48])
                    P.op("dve", lambda e: e.tensor_reduce(out=sel.ap, in_=t48.ap.rearrange("p g e -> p e g"), axis=AX.X, op=ALU.add), r=[t48], w=[sel])
                    P.op("dve", lambda e: e.tensor_reduce(out=sc[:, 4:5], in_=sel.ap, axis=AX.X, op=ALU.max), r=[sel], w=[sc])
                    P.op("dve", lambda e: e.tensor_scalar(out=e1.ap, in0=sel.ap, scalar1=sc[:, 4:5], scalar2=None, op0=ALU.is_equal), r=[sel, sc], w=[e1])
                    P.op("dve", lambda e: e.scalar_tensor_tensor(out=sel2.ap, in0=e1.ap, scalar=-1e30, in1=sel.ap, op0=ALU.mult, op1=ALU.add), r=[e1, sel], w=[sel2])
                    P.op("dve", lambda e: e.tensor_reduce(out=sc[:, 5:6], in_=sel2.ap, axis=AX.X, op=ALU.max), r=[sel2], w=[sc])
                    P.op("dve", lambda e: e.tensor_scalar(out=e2.ap, in0=sel2.ap, scalar1=sc[:, 5:6], scalar2=None, op0=ALU.is_equal), r=[sel2, sc], w=[e2])
                    P.op("dve", lambda e: e.tensor_tensor(out=sc[:, 6:7], in0=sc[:, 5:6], in1=sc[:, 4:5], op=ALU.subtract), r=[sc], w=[sc])
                    P.op("act", lambda e: e.activation(out=sc[:, 6:7], in_=sc[:, 6:7], func=AF.Exp), r=[sc], w=[sc])
                    P.op("dve", lambda e: e.tensor_scalar(out=sc[:, 6:7], in0=sc[:, 6:7], scalar1=1.0, scalar2=None, op0=ALU.add), r=[sc], w=[sc])
                    P.op("dve", lambda e: e.reciprocal(out=sc[:, 7:8], in_=sc[:, 6:7]), r=[sc], w=[sc])
                    P.op("dve", lambda e: e.tensor_tensor(out=sc[:, 8:9], in0=sc[:, 7:8], in1=sc[:, 3:4], op=ALU.mult), r=[sc], w=[sc])
                    P.op("dve", lambda e: e.tensor_tensor(out=sc[:, 9:10], in0=sc[:, 3:4], in1=sc[:, 8:9], op=ALU.subtract), r=[sc], w=[sc])
                    P.op("dve", lambda e: e.tensor_scalar(out=c8.ap, in0=e1.ap, scalar1=sc[:, 8:9], scalar2=None, op0=ALU.mult), r=[e1, sc], w=[c8])
                    P.op("dve", lambda e: e.scalar_tensor_tensor(out=c8.ap, in0=e2.ap, scalar=sc[:, 9:10], in1=c8.ap, op0=ALU.mult, op1=ALU.add), r=[e2, sc, c8], w=[c8])
                    for g in range(4):
                        P.op("dve", lambda e: e.tensor_scalar(out=comb[:, i, g * 8:(g + 1) * 8], in0=c8.ap, scalar1=gm[:, g:g + 1], scalar2=None, op0=ALU.mult), r=[c8, gm], w=[comb])
                P.barrier()
            w13 = self.wpipe(st, KC, 256, nbuf=2)
            w2p = self.wpipe(st, 4, 512, nbuf=2)
            hTp = self.sb(st, [128, KC, PTK * 128], BF16)
            aT = self.sb(st, [128, 4, PTK * 128], BF16)
            acc = self.sb(st, [128, PTK, D])
            s1 = self.sb(st, [128, 512])
            for p0 in range(0, NT, PTK):
                np_ = min(PTK, NT - p0)
                ntok = np_ * 128
                P.dma("sp", hTp[:, :, 0:ntok], self.HTd[:, :, p0 * 128:p0 * 128 + ntok], w=[hTp])
                for ex in range(NE):
                    for c in range(2):
                        W1 = self.wload(w13, self.w1[l, ex, :, c * 256:(c + 1) * 256], KC, 256)
                        W3 = self.wload(w13, self.w3[l, ex, :, c * 256:(c + 1) * 256], KC, 256)
                        for j in range(2):
                            for (t0, nt) in self.tok_blocks(0, ntok):
                                p1 = self.bank()
                                for kc in range(KC):
                                    P.op("pe", lambda e: e.matmul(p1[:, 0:nt], lhsT=W1[:, kc, j * 128:(j + 1) * 128], rhs=hTp[:, kc, t0:t0 + nt], start=(kc == 0), stop=(kc == KC - 1)), r=[W1, hTp], w=[p1])
                                p3 = self.bank()
                                for kc in range(KC):
                                    P.op("pe", lambda e: e.matmul(p3[:, 0:nt], lhsT=W3[:, kc, j * 128:(j + 1) * 128], rhs=hTp[:, kc, t0:t0 + nt], start=(kc == 0), stop=(kc == KC - 1)), r=[W3, hTp], w=[p3])
                                P.op("act", lambda e: e.activation(out=s1[:, 0:nt], in_=p1[:, 0:nt], func=AF.Silu), r=[p1], w=[s1])
                                P.op("dve", lambda e: e.tensor_tensor(out=aT[:, c * 2 + j, t0:t0 + nt], in0=p3[:, 0:nt], in1=s1[:, 0:nt], op=ALU.mult), r=[p3, s1], w=[aT])
                    for cc in range(4):
                        W2 = self.wload(w2p, self.w2[l, ex, :, cc * 512:(cc + 1) * 512], 4, 512)
                        for t in range(np_):
                            py = self.bank()
                            for kc in range(4):
                                P.op("pe", lambda e: e.matmul(py[:, :], lhsT=aT[:, kc, t * 128:(t + 1) * 128], rhs=W2[:, kc, :], start=(kc == 0), stop=(kc == 3)), r=[aT, W2], w=[py])
                            cw_ = comb[:, p0 + t, ex:ex + 1]
                            if ex == 0:
                                P.op("dve", lambda e: e.tensor_scalar(out=acc[:, t, cc * 512:(cc + 1) * 512], in0=py[:, :], scalar1=cw_, scalar2=None, op0=ALU.mult), r=[py, comb], w=[acc])
                            else:
                                P.op("dve", lambda e: e.scalar_tensor_tensor(out=acc[:, t, cc * 512:(cc + 1) * 512], in0=py[:, :], scalar=cw_, in1=acc[:, t, cc * 512:(cc + 1) * 512],
                                                                             op0=ALU.mult, op1=ALU.add), r=[py, comb, acc], w=[acc])
                for t in range(np_):
                    P.dma("sp", self.Y[(p0 + t) * 128:(p0 + t + 1) * 128, :], acc[:, t, :], r=[acc])

    def phase_fin(self, l):
        self.residual(l, "G4")


def core_inputs(inputs, b, n_ctx, n_lat, consts, rope, tiny_moe=False):
    m = {}
    f = lambda a: np.ascontiguousarray(a, dtype=np.float32)
    m["x"] = f(inputs["x"][b, :n_lat])
    m["ctx"] = f(inputs["ctx"][b, :n_ctx])
    m["c"] = f(f(inputs["c"][b]).reshape(16, 128).T)
    m["c_ctx"] = f(f(inputs["c_ctx"]).reshape(16, 128).T)
    for k in ["w_ada", "b_ada", "norm_g", "w_in", "hg_lb_logits", "hg_norm_g", "mla_g_q", "mla_g_kv", "mla_w_uq",
              "mla_w_ukv", "ssd_conv_w", "ssd_conv_b", "ssd_norm_g", "w_gate", "w_br", "w_out", "moe_w_grp",
              "moe_w_exp", "moe_w1", "moe_w3", "moe_w2", "ssd_d"]:
        m[k] = f(inputs[k])
    L = inputs["w_in"].shape[0]
    m["ssd_conv_w"] = f(f(inputs["ssd_conv_w"]).reshape(L, 3, 8, 128).transpose(0, 3, 2, 1))
    m["ssd_conv_b"] = f(f(inputs["ssd_conv_b"]).reshape(L, 8, 128).transpose(0, 2, 1))
    m["hg_lbT"] = f(f(inputs["hg_lb_logits"]).reshape(L + 1, 4, 128).transpose(2, 0, 1))
    m["ml_f_bias"] = f(inputs["ml_f_bias"]).reshape(L, 8)
    m["ssd_a_log"] = f(inputs["ssd_a_log"]).reshape(L, 16)
    m["ssd_dt_bias"] = f(inputs["ssd_dt_bias"]).reshape(L, 16)
    if tiny_moe:
        for k in ["moe_w1", "moe_w3", "moe_w2"]:
            m[k] = np.ascontiguousarray(m[k][:, 0:1])
    m["consts"] = consts
    m["rope"] = rope
    return m


def kernel(**inputs):
    B, n_lat, _ = inputs["x"].shape
    n_ctx = inputs["ctx"].shape[1]
    mk = MK(n_ctx, n_lat)
    nc = mk.build()
    consts, rope = make_consts(n_ctx, n_lat)
    in_maps = [core_inputs(inputs, b, n_ctx, n_lat, consts, rope) for b in range(B)]
    res = run_bass_kernel_spmd(nc, in_maps, core_ids=list(range(B)))
    return np.stack([np.asarray(r["out"]) for r in res.results], axis=0).astype(np.float32)
```

```python
import os
import numpy as np
from contextlib import ExitStack
import concourse.bass as bass
import concourse.mybir as mybir
from concourse.bass_utils import run_bass_kernel_spmd

F32 = mybir.dt.float32
BF16 = mybir.dt.bfloat16
AF = mybir.ActivationFunctionType
ALU = mybir.AluOpType
AX = mybir.AxisListType

D = 2048
KC = 16
DIN = 7008
EPS = 1e-6
NEG = -30000.0
EPOCH = 20000
N_DMA_SEMS = 24

O_MLQ, O_MLK, O_MLV, O_MLO, O_MLG = 0, 512, 1024, 1536, 2048
O_HGQ, O_HGI, O_HGG, O_HGF = 2064, 2576, 3088, 3600
O_CQ, O_CKV, O_KR = 4624, 5136, 5392
O_SZ, O_XBC, O_DT = 5456, 5968, 6992


class Buf:
    __slots__ = ("w", "r", "excl")

    def __init__(self):
        self.w = None
        self.r = {}
        self.excl = False


class Tl:
    def __init__(self, t):
        self.t = t
        self.b = Buf()

    def __getitem__(self, idx):
        return self.t.ap()[idx]

    @property
    def ap(self):
        return self.t.ap()


class Prog:
    def __init__(self, nc):
        self.nc = nc
        self.engs = {"pe": nc.tensor, "act": nc.scalar, "dve": nc.vector,
                     "pool": nc.gpsimd, "sp": nc.sync}
        self.sem = {}
        self.cnt = {}
        self.nsem = 0
        self.last = {}
        for e in self.engs:
            self._new_epoch(e)
        self.waited = {e: {} for e in self.engs}
        self.dma_sems = []
        for i in range(N_DMA_SEMS):
            s = nc.alloc_semaphore(f"dsem{i}")
            self.dma_sems.append([s, 0, ("d", i)])
        self.dma_rr = 0
        self.ninst = 0

    def _new_epoch(self, e):
        self.nsem += 1
        s = self.nc.alloc_semaphore(f"s_{e}_{self.nsem}")
        self.sem[e] = (s, ("e", e, self.nsem))
        self.cnt[e] = 0

    def _wait(self, eng, evs):
        best = {}
        for ev in evs:
            if ev is None:
                continue
            sem, val, key, src = ev
            if eng == "pe" and src == "pe":
                continue
            if self.waited[eng].get(key, 0) >= val:
                continue
            if key not in best or best[key][1] < val:
                best[key] = ev
        for key, (sem, val, _, _) in best.items():
            self.engs[eng].wait_ge(sem, val)
            self.waited[eng][key] = val

    def _tick(self, eng, inst):
        if self.cnt[eng] >= EPOCH:
            self._new_epoch(eng)
        sem, key = self.sem[eng]
        self.cnt[eng] += 1
        inst.then_inc(sem, 1)
        self.ninst += 1
        ev = (sem, self.cnt[eng], key, eng)
        self.last[key] = ev
        return ev

    @staticmethod
    def _deps(r, w):
        evs = []
        for b in r:
            evs.append(b.w)
            if b.excl:
                evs.extend(b.r.values())
        for b in w:
            evs.append(b.w)
            evs.extend(b.r.values())
        return evs

    @staticmethod
    def _commit(ev, r, w):
        for b in r:
            old = b.r.get(ev[2])
            if old is None or old[1] < ev[1]:
                b.r[ev[2]] = ev
        for b in w:
            b.w = ev
            b.r = {}

    def op(self, eng, fn, r=(), w=()):
        r = [x.b if isinstance(x, Tl) else x for x in r]
        w = [x.b if isinstance(x, Tl) else x for x in w]
        self._wait(eng, self._deps(r, w))
        inst = fn(self.engs[eng])
        ev = self._tick(eng, inst)
        self._commit(ev, r, w)
        return ev

    def dma(self, q, out, in_, r=(), w=(), **kw):
        r = [x.b if isinstance(x, Tl) else x for x in r]
        w = [x.b if isinstance(x, Tl) else x for x in w]
        ent = self.dma_sems[self.dma_rr]
        self.dma_rr = (self.dma_rr + 1) % len(self.dma_sems)
        sem, val, key = ent
        evs = self._deps(r, w)
        if val > 0:
            evs.append((sem, val, key, "dma"))
        self._wait(q, evs)
        inst = self.engs[q].dma_start(out=out, in_=in_, **kw)
        ent[1] = val + 16
        inst.then_inc(sem, 16)
        ev = (sem, val + 16, key, "dma")
        self._commit(ev, r, w)
        self.last[key] = ev
        self.ninst += 1
        return ev

    def barrier(self):
        evs = list(self.last.values())
        for e in self.engs:
            self._wait(e, evs)


C_ID, C_TRF, C_TRB, C_MNF, C_MNB, C_ONE, C_LCF, C_LCB, C_HC, C_PERM = range(10)
NCST = 10


def make_consts(n_ctx, n_lat):
    s = np.arange(128)[:, None]
    t = np.arange(128)[None, :]
    c = np.zeros((NCST, 128, 128), np.float32)
    c[C_ID] = np.eye(128)
    c[C_TRF] = (s <= t)
    c[C_TRB] = (s >= t)
    c[C_MNF] = np.where(s <= t, 0.0, NEG)
    c[C_MNB] = np.where(s >= t, 0.0, NEG)
    c[C_ONE] = 1.0
    c[C_LCF] = (s <= t).astype(np.float32) - (s <= 63)
    c[C_LCB] = (s >= t).astype(np.float32) - (s >= 64)
    sv = np.arange(128)
    c[C_HC][:, 0] = (sv <= 63)
    c[C_HC][:, 1] = 1.0
    c[C_HC][:, 2] = 1.0 - (sv <= 63)
    c[C_HC][:, 3] = (sv >= 64)
    c[C_HC][:, 4] = 1.0
    c[C_HC][:, 5] = 1.0 - (sv >= 64)
    Pm = np.zeros((64, 64), np.float32)
    for base in (0, 32):
        for i in range(16):
            Pm[base + i, base + 16 + i] = -1.0
            Pm[base + 16 + i, base + i] = 1.0
    c[C_PERM][:64, :64] = Pm.T
    consts = np.ascontiguousarray(c.transpose(1, 0, 2).reshape(128, NCST * 128))
    T = n_ctx + n_lat
    tok = np.arange(n_lat)
    pr, pc = tok // 64, tok % 64
    inv = (10000.0 ** (-np.arange(16, dtype=np.float32) / 16)).astype(np.float32)
    cos = np.ones((64, T), np.float32)
    sin = np.zeros((64, T), np.float32)
    for base, pos in ((0, pr), (32, pc)):
        ang = pos.astype(np.float32)[None, :] * inv[:, None]
        cos[base:base + 16, n_ctx:] = np.cos(ang)
        cos[base + 16:base + 32, n_ctx:] = np.cos(ang)
        sin[base:base + 16, n_ctx:] = np.sin(ang)
        sin[base + 16:base + 32, n_ctx:] = np.sin(ang)
    return consts, np.concatenate([cos, sin], axis=0)


class MK:
    def __init__(self, n_ctx, n_lat, depth=2, dbg=False, upto=None, tiny_moe=False):
        self.n_ctx, self.n_lat, self.depth, self.dbg, self.upto = n_ctx, n_lat, depth, dbg, upto
        self.tiny_moe = tiny_moe
        self.T = n_ctx + n_lat
        self.NT = self.T // 128
        self.NCT = n_ctx // 128
        nc = self.nc = bass.Bass("TRN2", target_bir_lowering=False)
        self.P = Prog(nc)
        self.ins = {}
        self.uid = 0
        self.dumped = set()

    def din(self, name, shape):
        a = self.nc.dram_tensor(name, list(shape), F32, kind="ExternalInput").ap()
        self.ins[name] = a
        return a

    def dscr(self, name, shape, dt=F32):
        kind = "ExternalOutput" if self.dbg else "Internal"
        return self.nc.dram_tensor(name, list(shape), dt, kind=kind).ap()

    def sb(self, st, shape, dt=F32, name=None):
        self.uid += 1
        return Tl(st.enter_context(self.nc.sbuf_tensor(f"{name or 't'}{self.uid}", list(shape), dt)))

    def dump(self, name, tl, ap=None, dt=F32):
        if not self.dbg or name in self.dumped:
            return
        self.dumped.add(name)
        ap = tl.ap if ap is None else ap
        dst = self.nc.dram_tensor("dbg_" + name, list(ap.shape), dt, kind="ExternalOutput").ap()
        self.P.dma("sp", dst, ap, r=[tl])

    def take_banks(self, k):
        self.free_banks = list(range(8 - k))
        self.bank_rr = 0
        return [self.ps[8 - k + i] for i in range(k)]

    def release_banks(self):
        self.free_banks = list(range(8))
        self.bank_rr = 0

    def bank(self, pool=None):
        if pool is not None:
            pool["rr"] = (pool["rr"] + 1) % len(pool["banks"])
            return self.ps[pool["banks"][pool["rr"]]]
        self.bank_rr = (self.bank_rr + 1) % len(self.free_banks)
        return self.ps[self.free_banks[self.bank_rr]]

    @staticmethod
    def drive(gens):
        gens = list(gens)
        while gens:
            for g in list(gens):
                try:
                    next(g)
                except StopIteration:
                    gens.remove(g)

    def tile_lo(self, l):
        return 0

    def tok_blocks(self, t0, t1, bs=512):
        out = []
        while t0 < t1:
            n = min(bs, t1 - t0)
            out.append((t0, n))
            t0 += n
        return out

    def build(self):
        nc, P = self.nc, self.P
        T, NT = self.T, self.NT
        L = self.depth
        I = self.din
        self.x_in = I("x", [self.n_lat, D])
        self.ctx_in = I("ctx", [self.n_ctx, D])
        self.c_in = I("c", [128, KC])
        self.cctx_in = I("c_ctx", [128, KC])
        self.w_ada = I("w_ada", [L, D, 6 * D])
        self.b_ada = I("b_ada", [L, 6 * D])
        self.norm_g = I("norm_g", [L, 4, D])
        self.w_in = I("w_in", [L, D, DIN])
        self.ml_f_bias = I("ml_f_bias", [L, 8])
        self.hg_lb = I("hg_lb_logits", [L + 1, 512])
        self.hg_norm_g = I("hg_norm_g", [L, 512])
        self.hg_lbT = I("hg_lbT", [128, L + 1, 4])
        self.mla_g_q = I("mla_g_q", [L, 512])
        self.mla_g_kv = I("mla_g_kv", [L, 256])
        self.mla_w_uq = I("mla_w_uq", [L, 512, 768])
        self.mla_w_ukv = I("mla_w_ukv", [L, 256, 1024])
        self.ssd_conv_w = I("ssd_conv_w", [L, 128, 8, 3])
        self.ssd_conv_b = I("ssd_conv_b", [L, 128, 8])
        self.ssd_a_log = I("ssd_a_log", [L, 16])
        self.ssd_dt_bias = I("ssd_dt_bias", [L, 16])
        self.ssd_d = I("ssd_d", [L, 8])
        self.ssd_norm_g = I("ssd_norm_g", [L, 512])
        self.w_gate = I("w_gate", [L, 4, D, D])
        self.w_br = I("w_br", [L, 4, 512, D])
        self.w_out = I("w_out", [L, D, D])
        self.w_grp = I("moe_w_grp", [L, D, 4])
        self.w_exp = I("moe_w_exp", [L, D, 32])
        ne = 1 if self.tiny_moe else 32
        self.w1 = I("moe_w1", [L, ne, D, 512])
        self.w3 = I("moe_w3", [L, ne, D, 512])
        self.w2 = I("moe_w2", [L, ne, 512, D])
        self.consts_in = I("consts", [128, NCST * 128])
        self.rope_in = I("rope", [128, T])
        self.out = nc.dram_tensor("out", [self.n_lat, D], F32, kind="ExternalOutput").ap()

        S = self.dscr
        self.xres = S("xres", [T, D])
        self.modv = S("modv", [L, 2, 6, D])
        self.lbd = S("lbd", [L, 512])
        self.UT = S("UT", [T, DIN])
        self.UF = S("UF", [DIN, T])
        self.BR = S("BR", [4, T, 512])
        self.ACC = S("ACC", [D, T], BF16)
        self.Y = S("Y", [T, D])
        self.HTd = S("HTd", [128, KC, T], BF16)
        self.YD = S("YD", [2, 2, T, 512])

        with ExitStack() as top:
            self.ps = [Tl(top.enter_context(nc.psum_tensor(f"ps{i}", [128, 512], F32))) for i in range(8)]
            for p_ in self.ps:
                p_.b.excl = True
            self.free_banks = list(range(8))
            self.bank_rr = 0
            self.cst = self.sb(top, [128, NCST, 128], F32, "cst")
            self.idb = self.sb(top, [128, 128], BF16, "idb")
            self.lbB = self.sb(top, [128, L, 512], F32, "lbB")
            P.dma("sp", self.cst.ap, self.consts_in.rearrange("p (c n) -> p c n", c=NCST), w=[self.cst])
            P.op("dve", lambda e: e.tensor_copy(out=self.idb.ap, in_=self.cst[:, C_ID, :]), r=[self.cst], w=[self.idb])
            P.dma("sp", self.xres[0:self.n_ctx, :], self.ctx_in)
            P.dma("sp", self.xres[self.n_ctx:T, :], self.x_in)
            steps = [("lb", lambda: self.setup_lb())]
            for l in range(L):
                steps.append((f"mod{l}", lambda l=l: self.phase_mod(l)))
            stages = ["norm1", "win", "ml", "hg", "mla", "ssd", "merge", "wout", "moe", "fin"]
            for l in range(L):
                for st_ in stages:
                    steps.append(((l, st_), lambda l=l, st_=st_: getattr(self, "phase_" + st_)(l)))
            P.barrier()
            if self.upto != "init":
                for name, fn in steps:
                    fn()
                    P.barrier()
                    if self.upto == name:
                        break
            P.dma("sp", self.out, self.xres[self.n_ctx:T, :])
            P.barrier()
        return nc

    def setup_lb(self):
        P, L = self.P, self.depth
        with ExitStack() as st:
            lg = self.sb(st, [128, L + 1, 512])
            ex = self.sb(st, [128, L + 1, 512])
            sm = self.sb(st, [128, 512])
            rc = self.sb(st, [128, 512])
            cum = self.sb(st, [128, 512])
            for j in range(L + 1):
                P.dma("sp", lg[:, j, :], self.hg_lb[j:j + 1, :].broadcast_to([128, 512]), w=[lg])
            P.op("act", lambda e: e.activation(out=ex.ap, in_=lg.ap, func=AF.Exp), r=[lg], w=[ex])
            P.op("dve", lambda e: e.tensor_tensor(out=sm.ap, in0=ex[:, 0, :], in1=ex[:, 1, :], op=ALU.add), r=[ex], w=[sm])
            for j in range(2, L + 1):
                P.op("dve", lambda e: e.tensor_tensor(out=sm.ap, in0=sm.ap, in1=ex[:, j, :], op=ALU.add), r=[ex, sm], w=[sm])
            P.op("dve", lambda e: e.reciprocal(out=rc.ap, in_=sm.ap), r=[sm], w=[rc])
            for l in range(L):
                if l == 0:
                    P.op("dve", lambda e: e.tensor_copy(out=cum.ap, in_=ex[:, 0, :]), r=[ex], w=[cum])
                else:
                    P.op("dve", lambda e: e.tensor_tensor(out=cum.ap, in0=cum.ap, in1=ex[:, l, :], op=ALU.add), r=[ex, cum], w=[cum])
                P.op("dve", lambda e: e.tensor_tensor(out=self.lbB[:, l, :], in0=cum.ap, in1=rc.ap, op=ALU.mult), r=[cum, rc], w=[self.lbB])
                P.dma("sp", self.lbd[l:l + 1, :], self.lbB[0:1, l, :], r=[self.lbB])
            P.barrier()

    def phase_mod(self, l):
        P, nc = self.P, self.nc
        with ExitStack() as st:
            cv = self.sb(st, [128, 2, KC])
            cs = self.sb(st, [128, 2, KC])
            SB = self.sb(st, [128, 2, KC, 128])
            bb = [self.sb(st, [1, 512]) for _ in range(2)]
            mo = [self.sb(st, [1, 2, 512]) for _ in range(2)]
            wst = [self.sb(st, [128, KC, 512]) for _ in range(2)]
            P.dma("sp", cv[:, 0, :], self.c_in, w=[cv])
            P.dma("sp", cv[:, 1, :], self.cctx_in, w=[cv])
            P.op("act", lambda e: e.activation(out=cs.ap, in_=cv.ap, func=AF.Silu), r=[cv], w=[cs])
            for j in range(2):
                for kc in range(KC):
                    P.op("dve", lambda e: e.tensor_copy(out=SB[:, j, kc, :], in_=cs[:, j, kc:kc + 1].broadcast_to([128, 128])), r=[cs], w=[SB])
            for ch in range(24):
                w_, b_, m_ = wst[ch % 2], bb[ch % 2], mo[ch % 2]
                P.dma("sp", w_.ap, self.w_ada[l, :, ch * 512:(ch + 1) * 512].rearrange("(k p) n -> p k n", p=128), w=[w_])
                P.dma("sp", b_.ap, self.b_ada[l:l + 1, ch * 512:(ch + 1) * 512], w=[b_])
                s_, c0 = divmod(ch * 512, D)
                for j in range(2):
                    pb = self.bank()
                    for kc in range(KC):
                        P.op("pe", lambda e: e.matmul(pb[:, :], lhsT=SB[:, j, kc, :], rhs=w_[:, kc, :], start=(kc == 0), stop=(kc == KC - 1)),
                             r=[SB, w_], w=[pb])
                    P.op("dve", lambda e: e.tensor_tensor(out=m_[:, j, :], in0=pb[0:1, :], in1=b_.ap, op=ALU.add), r=[pb, b_], w=[m_])
                P.dma("sp", self.modv[l:l + 1, :, s_, c0:c0 + 512], m_.ap, r=[m_])
            P.barrier()

    def load_modvec(self, st, l, kind):
        P = self.P
        mi, gi, add1 = {"A1": (1, 0, True), "B1": (0, None, False), "G2": (2, 1, False),
                        "A2": (4, 2, True), "B2": (3, None, False), "G4": (5, 3, False)}[kind]
        t = self.sb(st, [128, 2, D])
        for j in range(2):
            P.dma("sp", t[:, j, :], self.modv[l, j:j + 1, mi, :].broadcast_to([128, D]), w=[t])
        if gi is not None:
            with ExitStack() as s2:
                g = self.sb(s2, [128, D])
                P.dma("sp", g.ap, self.norm_g[l, gi:gi + 1, :].broadcast_to([128, D]), w=[g])
                for j in range(2):
                    if add1:
                        P.op("dve", lambda e: e.scalar_tensor_tensor(out=t[:, j, :], in0=t[:, j, :], scalar=1.0, in1=g.ap,
                                                                     op0=ALU.add, op1=ALU.mult), r=[t, g], w=[t])
                    else:
                        P.op("dve", lambda e: e.tensor_tensor(out=t[:, j, :], in0=t[:, j, :], in1=g.ap, op=ALU.mult), r=[t, g], w=[t])
                P.barrier()
        return t

    def rstd_of(self, src_ap, src_dep, n, junk, ss):
        P = self.P
        P.op("act", lambda e: e.activation(out=junk, in_=src_ap, func=AF.Square, accum_out=ss[:, 0:1]), r=src_dep, w=[ss])
        P.op("dve", lambda e: e.tensor_scalar(out=ss[:, 1:2], in0=ss[:, 0:1], scalar1=1.0 / n, scalar2=EPS, op0=ALU.mult, op1=ALU.add), r=[ss], w=[ss])
        P.op("act", lambda e: e.activation(out=ss[:, 2:3], in_=ss[:, 1:2], func=AF.Sqrt), r=[ss], w=[ss])
        P.op("dve", lambda e: e.reciprocal(out=ss[:, 3:4], in_=ss[:, 2:3]), r=[ss], w=[ss])

    def transpose_to(self, src, ncol, dst_fn, r_extra=()):
        P = self.P
        nblk = ncol // 128
        for q in range(0, nblk, 4):
            nb = min(4, nblk - q)
            pb = self.bank()
            for j in range(nb):
                P.op("pe", lambda e: e.matmul(pb[:, j * 128:(j + 1) * 128], lhsT=src[:, (q + j) * 128:(q + j + 1) * 128], rhs=self.idb.ap,
                                              start=True, stop=True), r=[src, self.idb], w=[pb])
            dst_fn(q, nb, pb)

    def norm_mod_to_hT(self, l, ia, ib, tile_lo=0):
        P = self.P
        with ExitStack() as st:
            A = self.load_modvec(st, l, ia)
            B = self.load_modvec(st, l, ib)
            xt = [self.sb(st, [128, D]) for _ in range(2)]
            tmp = self.sb(st, [128, D])
            hb = [self.sb(st, [128, D], BF16) for _ in range(2)]
            junk = self.sb(st, [128, D], BF16)
            ss = [self.sb(st, [128, 4]) for _ in range(2)]
            self.hT = self.sb(st, [128, KC, self.T], BF16, "hT")
            if tile_lo > 0:
                P.op("pool", lambda e: e.memset(self.hT[:, :, 0:tile_lo * 128], 0.0), w=[self.hT])
            for i in range(tile_lo, self.NT):
                j = 1 if i < self.NCT else 0
                x_, h_, s_ = xt[i % 2], hb[i % 2], ss[i % 2]
                P.dma("sp", x_.ap, self.xres[i * 128:(i + 1) * 128, :], w=[x_])
                self.rstd_of(x_.ap, [x_], D, junk.ap, s_)
                P.op("dve", lambda e: e.scalar_tensor_tensor(out=tmp.ap, in0=x_.ap, scalar=s_[:, 3:4], in1=A[:, j, :], op0=ALU.mult, op1=ALU.mult),
                     r=[x_, s_, A], w=[tmp])
                P.op("pool", lambda e: e.tensor_tensor(out=h_.ap, in0=tmp.ap, in1=B[:, j, :], op=ALU.add), r=[tmp, B], w=[h_])

                def dst(q, nb, pb, i=i):
                    P.op("act", lambda e: e.copy(out=self.hT[:, q:q + nb, i * 128:(i + 1) * 128],
                                                 in_=pb[:, 0:nb * 128].rearrange("p (a t) -> p a t", a=nb)), r=[pb], w=[self.hT])
                self.transpose_to(h_, D, dst)
            P.dma("sp", self.HTd, self.hT.ap, r=[self.hT])
            P.barrier()

    def load_hT(self, st):
        self.hT = self.sb(st, [128, KC, self.T], BF16, "hT")
        self.P.dma("sp", self.hT.ap, self.HTd, w=[self.hT])

    def phase_norm1(self, l):
        self.norm_mod_to_hT(l, "A1", "B1")

    def wpipe(self, st, kc, ncol, nbuf=2):
        return {"stg": [self.sb(st, [128, kc, ncol]) for _ in range(nbuf)],
                "wb": [self.sb(st, [128, kc, ncol], BF16) for _ in range(nbuf)], "i": 0, "n": nbuf}

    def wload(self, wp, dram_ap, kc, ncol):
        P = self.P
        i = wp["i"] % wp["n"]
        wp["i"] += 1
        s_, b_ = wp["stg"][i], wp["wb"][i]
        P.dma("sp", s_[:, 0:kc, 0:ncol], dram_ap.rearrange("(k p) n -> p k n", p=128), w=[s_])
        P.op("pool", lambda e: e.tensor_copy(out=b_[:, 0:kc, 0:ncol], in_=s_[:, 0:kc, 0:ncol]), r=[s_], w=[b_])
        return b_

    def phase_win(self, l):
        P = self.P
        T = self.T
        groups = [(O_MLQ, 512, "F"), (O_MLK, 512, "FT"), (O_MLV, 512, "T"), (O_MLO, 512, "T"), (O_MLG, 16, "T"),
                  (O_HGQ, 512, "F"), (O_HGI, 512, "T"), (O_HGG, 512, "T"), (O_HGF, 1024, "FT"),
                  (O_CQ, 512, "T"), (O_CKV, 256, "T"), (O_KR, 64, "F"),
                  (O_SZ, 512, "T"), (O_XBC, 1024, "F"), (O_DT, 16, "T")]
        with ExitStack() as st:
            self.load_hT(st)
            wp = self.wpipe(st, KC, 256)
            ev = [self.sb(st, [128, 512]) for _ in range(4)]
            ei = 0
            for (c0, n, mode) in groups:
                for cc in range(c0, c0 + n, 256):
                    nn = min(256, c0 + n - cc)
                    wb = self.wload(wp, self.w_in[l, :, cc:cc + nn], KC, nn)
                    if "F" in mode:
                        for m0 in range(0, nn, 128):
                            mm = min(128, nn - m0)
                            for (t0, nt) in self.tok_blocks(0, T):
                                pb = self.bank()
                                for kc in range(KC):
                                    P.op("pe", lambda e: e.matmul(pb[0:mm, 0:nt], lhsT=wb[:, kc, m0:m0 + mm], rhs=self.hT[:, kc, t0:t0 + nt],
                                                                  start=(kc == 0), stop=(kc == KC - 1)), r=[wb, self.hT], w=[pb])
                                e_ = ev[ei % 4]
                                eng = "act" if ei % 2 == 0 else "dve"
                                ei += 1
                                if eng == "act":
                                    P.op("act", lambda e: e.copy(out=e_[0:mm, 0:nt], in_=pb[0:mm, 0:nt]), r=[pb], w=[e_])
                                else:
                                    P.op("dve", lambda e: e.tensor_copy(out=e_[0:mm, 0:nt], in_=pb[0:mm, 0:nt]), r=[pb], w=[e_])
                                P.dma("sp", self.UF[cc + m0:cc + m0 + mm, t0:t0 + nt], e_[0:mm, 0:nt], r=[e_])
                    if "T" in mode:
                        for i in range(self.NT):
                            pb = self.bank()
                            for kc in range(KC):
                                P.op("pe", lambda e: e.matmul(pb[:, 0:nn], lhsT=self.hT[:, kc, i * 128:(i + 1) * 128], rhs=wb[:, kc, 0:nn],
                                                              start=(kc == 0), stop=(kc == KC - 1)), r=[wb, self.hT], w=[pb])
                            e_ = ev[ei % 4]
                            eng = "act" if ei % 2 == 0 else "dve"
                            ei += 1
                            if eng == "act":
                                P.op("act", lambda e: e.copy(out=e_[:, 0:nn], in_=pb[:, 0:nn]), r=[pb], w=[e_])
                            else:
                                P.op("dve", lambda e: e.tensor_copy(out=e_[:, 0:nn], in_=pb[:, 0:nn]), r=[pb], w=[e_])
                            P.dma("sp", self.UT[i * 128:(i + 1) * 128, cc:cc + nn], e_[:, 0:nn], r=[e_])

    def order(self, d):
        c = list(range(self.NCT))
        la = list(range(self.NCT, self.NT))
        return c + la if d == 0 else c[::-1] + la[::-1]

    def decay_scalars(self, sc, lf_ap, lf_dep, n, d, pool=None):
        P = self.P
        tri = C_TRF if d == 0 else C_TRB
        pb = self.bank(pool)
        P.op("pe", lambda e: e.matmul(pb[:, 0:n], lhsT=self.cst[:, tri, :], rhs=lf_ap, start=True, stop=True), r=[self.cst] + lf_dep, w=[pb])
        P.op("pe", lambda e: e.matmul(pb[:, n:2 * n], lhsT=self.cst[:, C_ONE, :], rhs=lf_ap, start=True, stop=True), r=[self.cst] + lf_dep, w=[pb])
        P.op("dve", lambda e: e.tensor_copy(out=sc[:, 0:2 * n], in_=pb[:, 0:2 * n]), r=[pb], w=[sc])
        P.op("dve", lambda e: e.tensor_scalar(out=sc[:, 2 * n:3 * n], in0=sc[:, 0:n], scalar1=-1.0, scalar2=None, op0=ALU.mult), r=[sc], w=[sc])
        P.op("dve", lambda e: e.tensor_tensor(out=sc[:, 3 * n:4 * n], in0=sc[:, n:2 * n], in1=sc[:, 0:n], op=ALU.subtract), r=[sc], w=[sc])
        P.op("act", lambda e: e.activation(out=sc[:, 3 * n:4 * n], in_=sc[:, 3 * n:4 * n], func=AF.Exp), r=[sc], w=[sc])
        P.op("act", lambda e: e.activation(out=sc[:, 4 * n:6 * n], in_=sc[:, 0:2 * n], func=AF.Exp), r=[sc], w=[sc])

    def decay_matrix(self, lf_col, lf_dep, bias_col, bias_dep, d, lfB, Dm, pool=None):
        P = self.P
        tri = C_TRF if d == 0 else C_TRB
        mn = C_MNF if d == 0 else C_MNB
        P.op("dve", lambda e: e.tensor_copy(out=lfB.ap, in_=lf_col.broadcast_to([128, 128])), r=lf_dep, w=[lfB])
        pb = self.bank(pool)
        P.op("pe", lambda e: e.matmul(pb[:, 0:128], lhsT=lfB.ap, rhs=self.cst[:, tri, :], start=True, stop=False), r=[lfB, self.cst], w=[pb])
        P.op("pe", lambda e: e.matmul(pb[:, 0:128], lhsT=self.cst[:, C_ID, :], rhs=self.cst[:, mn, :], start=False, stop=True), r=[self.cst], w=[pb])
        P.op("act", lambda e: e.activation(out=Dm.ap, in_=pb[:, 0:128], func=AF.Exp, bias=bias_col, scale=1.0), r=[pb] + bias_dep, w=[Dm])

    def phase_ml(self, l):
        P = self.P
        L = self.depth
        with ExitStack() as st:
            bias = self.sb(st, [128, 8])
            P.dma("sp", bias.ap, self.ml_f_bias[l:l + 1, :].broadcast_to([128, 8]), w=[bias])
            lg = self.sb(st, [128, L + 1, 4]); lbT = self.sb(st, [128, 4]); omT = self.sb(st, [128, 4]); smT = self.sb(st, [128, 4])
            omB = self.sb(st, [128, 512]); ngB = self.sb(st, [128, 512])
            P.dma("sp", lg.ap, self.hg_lbT, w=[lg])
            P.dma("sp", ngB.ap, self.hg_norm_g[l:l + 1, :].broadcast_to([128, 512]), w=[ngB])
            P.op("act", lambda e: e.activation(out=lg.ap, in_=lg.ap, func=AF.Exp), r=[lg], w=[lg])
            P.op("dve", lambda e: e.tensor_tensor(out=smT.ap, in0=lg[:, 0, :], in1=lg[:, 1, :], op=ALU.add), r=[lg], w=[smT])
            for j in range(2, L + 1):
                P.op("dve", lambda e: e.tensor_tensor(out=smT.ap, in0=smT.ap, in1=lg[:, j, :], op=ALU.add), r=[lg, smT], w=[smT])
            P.op("dve", lambda e: e.reciprocal(out=smT.ap, in_=smT.ap), r=[smT], w=[smT])
            P.op("dve", lambda e: e.tensor_copy(out=lbT.ap, in_=lg[:, 0, :]), r=[lg], w=[lbT])
            for j in range(1, l + 1):
                P.op("dve", lambda e: e.tensor_tensor(out=lbT.ap, in0=lbT.ap, in1=lg[:, j, :], op=ALU.add), r=[lg, lbT], w=[lbT])
            P.op("dve", lambda e: e.tensor_tensor(out=lbT.ap, in0=lbT.ap, in1=smT.ap, op=ALU.mult), r=[lbT, smT], w=[lbT])
            P.op("dve", lambda e: e.tensor_scalar(out=omT.ap, in0=lbT.ap, scalar1=-1.0, scalar2=1.0, op0=ALU.mult, op1=ALU.add), r=[lbT], w=[omT])
            P.op("dve", lambda e: e.tensor_scalar(out=omB.ap, in0=self.lbB[:, l, :], scalar1=-1.0, scalar2=1.0, op0=ALU.mult, op1=ALU.add), r=[self.lbB], w=[omB])
            chains = [self.ml_chain(st, l, d, bias, (0, d), [2 * d, 2 * d + 1]) for d in range(2)]
            chains += [self.hg_chain(st, l, d, omT, omB, (1, d), [4 + 2 * d, 5 + 2 * d]) for d in range(2)]
            self.drive(chains)
            P.barrier()
            y0 = self.sb(st, [128, 512]); y1 = self.sb(st, [128, 512]); ot = self.sb(st, [128, 512])
            sq = self.sb(st, [128, 512]); rs = self.sb(st, [128, 8])
            for i in range(self.NT):
                ts = slice(i * 128, (i + 1) * 128)
                P.dma("sp", ot.ap, self.UT[ts, O_MLO:O_MLO + 512], w=[ot])
                P.dma("sp", y0.ap, self.YD[0, 0, ts, :], w=[y0])
                P.dma("sp", y1.ap, self.YD[0, 1, ts, :], w=[y1])
                P.op("act", lambda e: e.activation(out=ot.ap, in_=ot.ap, func=AF.Sigmoid), r=[ot], w=[ot])
                P.op("dve", lambda e: e.tensor_tensor(out=y0.ap, in0=y0.ap, in1=y1.ap, op=ALU.add), r=[y0, y1], w=[y0])
                P.op("dve", lambda e: e.tensor_tensor(out=y0.ap, in0=y0.ap, in1=ot.ap, op=ALU.mult), r=[y0, ot], w=[y0])
                P.dma("sp", self.BR[0, ts, :], y0.ap, r=[y0])
            for i in range(self.NT):
                ts = slice(i * 128, (i + 1) * 128)
                P.dma("sp", ot.ap, self.UT[ts, O_HGG:O_HGG + 512], w=[ot])
                P.dma("sp", y0.ap, self.YD[1, 0, ts, :], w=[y0])
                P.dma("sp", y1.ap, self.YD[1, 1, ts, :], w=[y1])
                P.op("dve", lambda e: e.tensor_tensor(out=y0.ap, in0=y0.ap, in1=y1.ap, op=ALU.add), r=[y0, y1], w=[y0])
                P.op("dve", lambda e: e.tensor_tensor(out=sq.ap, in0=y0.ap, in1=y0.ap, op=ALU.mult), r=[y0], w=[sq])
                P.op("dve", lambda e: e.tensor_reduce(out=rs[:, 0:4], in_=sq.ap.rearrange("p (h c) -> p h c", h=4), axis=AX.X, op=ALU.add), r=[sq], w=[rs])
                P.op("dve", lambda e: e.tensor_scalar(out=rs[:, 0:4], in0=rs[:, 0:4], scalar1=1.0 / 128, scalar2=EPS, op0=ALU.mult, op1=ALU.add), r=[rs], w=[rs])
                P.op("act", lambda e: e.activation(out=rs[:, 0:4], in_=rs[:, 0:4], func=AF.Sqrt), r=[rs], w=[rs])
                P.op("dve", lambda e: e.reciprocal(out=rs[:, 4:8], in_=rs[:, 0:4]), r=[rs], w=[rs])
                P.op("dve", lambda e: e.tensor_tensor(out=y0.ap.rearrange("p (h c) -> p h c", h=4), in0=y0.ap.rearrange("p (h c) -> p h c", h=4),
                                                      in1=rs[:, 4:8].unsqueeze(2).broadcast_to([128, 4, 128]), op=ALU.mult), r=[y0, rs], w=[y0])
                P.op("dve", lambda e: e.tensor_tensor(out=y0.ap, in0=y0.ap, in1=ngB.ap, op=ALU.mult), r=[y0, ngB], w=[y0])
                P.op("act", lambda e: e.activation(out=ot.ap, in_=ot.ap, func=AF.Sigmoid), r=[ot], w=[ot])
                P.op("dve", lambda e: e.tensor_tensor(out=y0.ap, in0=y0.ap, in1=ot.ap, op=ALU.mult), r=[y0, ot], w=[y0])
                P.dma("sp", self.BR[1, ts, :], y0.ap, r=[y0])

    def ml_chain(self, st, l, d, bias, ydst, banks):
        P = self.P
        pool = {"banks": list(banks), "rr": 0}
        yo = self.sb(st, [128, 512])
        Cs = self.sb(st, [128, 4, 132]); Cb = self.sb(st, [128, 4, 132], BF16)
        qT = self.sb(st, [128, 4, 128]); kT = self.sb(st, [128, 4, 128])
        qTb = self.sb(st, [128, 4, 128], BF16); kTb = self.sb(st, [128, 4, 128], BF16)
        kt = self.sb(st, [128, 512]); vt = self.sb(st, [128, 512])
        vaug = self.sb(st, [128, 4, 132], BF16)
        gt = self.sb(st, [128, 16]); xg = self.sb(st, [128, 4]); lf = self.sb(st, [128, 4]); ab = self.sb(st, [128, 8])
        sc = self.sb(st, [128, 24])
        lfB = self.sb(st, [128, 128]); Dm = self.sb(st, [128, 128]); PT = self.sb(st, [128, 128], BF16)
        tmpI = self.sb(st, [128, 132]); tot = self.sb(st, [128, 132]); dn = self.sb(st, [128, 2])
        kw = self.sb(st, [128, 128], BF16)
        P.op("pool", lambda e: e.memset(vaug.ap, 1.0), w=[vaug])
        P.op("dve", lambda e: e.memset(Cs.ap, 0.0), w=[Cs])
        P.op("pool", lambda e: e.memset(Cb.ap, 0.0), w=[Cb])
        yield
        for i in self.order(d):
            ts = slice(i * 128, (i + 1) * 128)
            P.dma("sp", qT.ap, self.UF[O_MLQ:O_MLQ + 512, ts].rearrange("(h p) t -> p h t", p=128), w=[qT])
            P.dma("sp", kT.ap, self.UF[O_MLK:O_MLK + 512, ts].rearrange("(h p) t -> p h t", p=128), w=[kT])
            P.dma("sp", kt.ap, self.UT[ts, O_MLK:O_MLK + 512], w=[kt])
            P.dma("sp", vt.ap, self.UT[ts, O_MLV:O_MLV + 512], w=[vt])
            P.dma("sp", gt.ap, self.UT[ts, O_MLG:O_MLG + 16], w=[gt])
            yield
            P.op("act", lambda e: e.mul(out=qTb.ap, in_=qT.ap, mul=128.0 ** -0.5), r=[qT], w=[qTb])
            P.op("pool", lambda e: e.tensor_copy(out=kTb.ap, in_=kT.ap), r=[kT], w=[kTb])
            P.op("pool", lambda e: e.tensor_copy(out=vaug[:, :, 0:128], in_=vt.ap.rearrange("p (h d) -> p h d", h=4)), r=[vt], w=[vaug])
            yield
            P.op("dve", lambda e: e.tensor_tensor(out=xg.ap, in0=gt[:, d * 8 + 4:d * 8 + 8], in1=bias[:, d * 4:d * 4 + 4], op=ALU.add), r=[gt, bias], w=[xg])
            yield
            P.op("act", lambda e: e.activation(out=xg.ap, in_=xg.ap, func=AF.Exp, scale=-1.0), r=[xg], w=[xg])
            yield
            P.op("act", lambda e: e.activation(out=xg.ap, in_=xg.ap, func=AF.Ln, bias=1.0), r=[xg], w=[xg])
            yield
            P.op("dve", lambda e: e.tensor_scalar(out=lf.ap, in0=xg.ap, scalar1=-1.0, scalar2=None, op0=ALU.mult), r=[xg], w=[lf])
            yield
            self.decay_scalars(sc, lf.ap, [lf], 4, d, pool)
            yield
            P.op("dve", lambda e: e.tensor_tensor(out=ab[:, 0:4], in0=gt[:, d * 8:d * 8 + 4], in1=sc[:, 0:4], op=ALU.subtract), r=[gt, sc], w=[ab])
            P.op("dve", lambda e: e.tensor_tensor(out=ab[:, 4:8], in0=ab[:, 0:4], in1=sc[:, 4:8], op=ALU.add), r=[ab, sc], w=[ab])
            yield
            P.op("act", lambda e: e.activation(out=ab[:, 4:8], in_=ab[:, 4:8], func=AF.Exp), r=[ab], w=[ab])
            yield
            for h in range(4):
                hs = slice(h * 128, (h + 1) * 128)
                self.decay_matrix(lf[:, h:h + 1], [lf], ab[:, h:h + 1], [ab], d, lfB, Dm, pool)
                yield
                pS = self.bank(pool)
                P.op("pe", lambda e: e.matmul(pS[:, 0:128], lhsT=kTb[:, h, :], rhs=qTb[:, h, :], start=True, stop=True), r=[kTb, qTb], w=[pS])
                yield
                P.op("dve", lambda e: e.tensor_tensor(out=PT.ap, in0=pS[:, 0:128], in1=Dm.ap, op=ALU.mult), r=[pS, Dm], w=[PT])
                yield
                pI = self.bank(pool)
                P.op("pe", lambda e: e.matmul(pI[:, 0:129], lhsT=qTb[:, h, :], rhs=Cb[:, h, 0:129], start=True, stop=True), r=[qTb, Cb], w=[pI])
                yield
                P.op("dve", lambda e: e.tensor_scalar(out=tmpI[:, 0:129], in0=pI[:, 0:129], scalar1=sc[:, 16 + h:17 + h], scalar2=None, op0=ALU.mult), r=[pI, sc], w=[tmpI])
                yield
                pO = self.bank(pool)
                P.op("pe", lambda e: e.matmul(pO[:, 0:129], lhsT=PT.ap, rhs=vaug[:, h, 0:129], start=True, stop=True), r=[PT, vaug], w=[pO])
                yield
                P.op("dve", lambda e: e.tensor_tensor(out=tot[:, 0:129], in0=pO[:, 0:129], in1=tmpI[:, 0:129], op=ALU.add), r=[pO, tmpI], w=[tot])
                yield
                P.op("act", lambda e: e.activation(out=dn[:, 0:1], in_=tot[:, 128:129], func=AF.Abs), r=[tot], w=[dn])
                yield
                P.op("dve", lambda e: e.tensor_scalar(out=dn[:, 0:1], in0=dn[:, 0:1], scalar1=1.0, scalar2=None, op0=ALU.max), r=[dn], w=[dn])
                yield
                P.op("dve", lambda e: e.reciprocal(out=dn[:, 1:2], in_=dn[:, 0:1]), r=[dn], w=[dn])
                yield
                P.op("dve", lambda e: e.tensor_scalar(out=yo[:, hs], in0=tot[:, 0:128], scalar1=dn[:, 1:2], scalar2=None, op0=ALU.mult), r=[tot, dn], w=[yo])
                yield
                P.op("pool", lambda e: e.tensor_scalar(out=kw.ap, in0=kt[:, hs], scalar1=ab[:, 4 + h:5 + h], scalar2=None, op0=ALU.mult), r=[kt, ab], w=[kw])
                yield
                pU = self.bank(pool)
                P.op("pe", lambda e: e.matmul(pU[:, 0:129], lhsT=kw.ap, rhs=vaug[:, h, 0:129], start=True, stop=True), r=[kw, vaug], w=[pU])
                yield
                P.op("dve", lambda e: e.scalar_tensor_tensor(out=Cs[:, h, 0:129], in0=Cs[:, h, 0:129], scalar=sc[:, 20 + h:21 + h], in1=pU[:, 0:129], op0=ALU.mult, op1=ALU.add),
                     r=[Cs, sc, pU], w=[Cs])
                yield
                P.op("act", lambda e: e.copy(out=Cb[:, h, 0:129], in_=Cs[:, h, 0:129]), r=[Cs], w=[Cb])
                yield
            P.dma("sp", self.YD[ydst[0], ydst[1], ts, :], yo.ap, r=[yo])
            yield

    def phase_ssd(self, l):
        P = self.P
        T, NCX = self.T, self.n_ctx
        with ExitStack() as st:
            xbcT = self.sb(st, [128, 8, T], BF16)
            cw = self.sb(st, [128, 8, 3]); cb = self.sb(st, [128, 8])
            dtb = self.sb(st, [128, 16]); aneg = self.sb(st, [128, 16]); dsk = self.sb(st, [128, 8]); ng = self.sb(st, [128, 512])
            P.dma("sp", cw.ap, self.ssd_conv_w[l], w=[cw])
            P.dma("sp", cb.ap, self.ssd_conv_b[l], w=[cb])
            P.dma("sp", dtb.ap, self.ssd_dt_bias[l:l + 1, :].broadcast_to([128, 16]), w=[dtb])
            P.dma("sp", aneg.ap, self.ssd_a_log[l:l + 1, :].broadcast_to([128, 16]), w=[aneg])
            P.dma("sp", dsk.ap, self.ssd_d[l:l + 1, :].broadcast_to([128, 8]), w=[dsk])
            P.dma("sp", ng.ap, self.ssd_norm_g[l:l + 1, :].broadcast_to([128, 512]), w=[ng])
            P.op("act", lambda e: e.activation(out=aneg.ap, in_=aneg.ap, func=AF.Exp), r=[aneg], w=[aneg])
            P.op("dve", lambda e: e.tensor_scalar(out=aneg.ap, in0=aneg.ap, scalar1=-1.0, scalar2=None, op0=ALU.mult), r=[aneg], w=[aneg])
            with ExitStack() as s2:
                xin = [self.sb(s2, [128, T]) for _ in range(2)]
                acc = self.sb(s2, [128, T])
                for g in range(8):
                    x_ = xin[g % 2]
                    P.dma("sp", x_.ap, self.UF[O_XBC + g * 128:O_XBC + (g + 1) * 128, :], w=[x_])
                    P.op("dve", lambda e: e.tensor_scalar(out=acc.ap, in0=x_.ap, scalar1=cw[:, g, 1:2], scalar2=None, op0=ALU.mult), r=[x_, cw], w=[acc])
                    for (s0, s1) in ((0, NCX), (NCX, T)):
                        P.op("dve", lambda e: e.scalar_tensor_tensor(out=acc[:, s0 + 1:s1], in0=x_[:, s0:s1 - 1], scalar=cw[:, g, 0:1], in1=acc[:, s0 + 1:s1],
                                                                     op0=ALU.mult, op1=ALU.add), r=[x_, cw, acc], w=[acc])
                        P.op("dve", lambda e: e.scalar_tensor_tensor(out=acc[:, s0:s1 - 1], in0=x_[:, s0 + 1:s1], scalar=cw[:, g, 2:3], in1=acc[:, s0:s1 - 1],
                                                                     op0=ALU.mult, op1=ALU.add), r=[x_, cw, acc], w=[acc])
                    P.op("act", lambda e: e.activation(out=xbcT[:, g, :], in_=acc.ap, func=AF.Silu, bias=cb[:, g:g + 1], scale=1.0), r=[acc, cb], w=[xbcT])
                P.barrier()
            ydir = [self.sb(st, [128, self.NT, 512]) for _ in range(2)]
            self.drive([self.ssd_chain(st, l, d, xbcT, dtb, aneg, dsk, ydir[d]) for d in range(2)])
            yd = self.sb(st, [128, 512]); zt = self.sb(st, [128, 512]); junk = self.sb(st, [128, 512], BF16); ss = self.sb(st, [128, 4])
            for i in range(self.NT):
                ts = slice(i * 128, (i + 1) * 128)
                P.dma("sp", zt.ap, self.UT[ts, O_SZ:O_SZ + 512], w=[zt])
                P.op("dve", lambda e: e.tensor_tensor(out=yd.ap, in0=ydir[0][:, i, :], in1=ydir[1][:, i, :], op=ALU.add), r=[ydir[0], ydir[1]], w=[yd])
                P.op("act", lambda e: e.activation(out=zt.ap, in_=zt.ap, func=AF.Silu), r=[zt], w=[zt])
                P.op("dve", lambda e: e.tensor_tensor(out=yd.ap, in0=yd.ap, in1=zt.ap, op=ALU.mult), r=[yd, zt], w=[yd])
                self.rstd_of(yd.ap, [yd], 512, junk.ap, ss)
                P.op("dve", lambda e: e.scalar_tensor_tensor(out=yd.ap, in0=yd.ap, scalar=ss[:, 3:4], in1=ng.ap, op0=ALU.mult, op1=ALU.mult), r=[yd, ss, ng], w=[yd])
                P.dma("sp", self.BR[3, ts, :], yd.ap, r=[yd])

    def ssd_chain(self, st, l, d, xbcT, dtb, aneg, dsk, yo):
        P = self.P
        pS, pO = self.ps[4 * d], self.ps[4 * d + 1]
        pool = {"banks": [4 * d + 2, 4 * d + 3], "rr": 0}
        Hs = self.sb(st, [128, 2, 256]); Hb = self.sb(st, [128, 2, 256], BF16)
        xtok = self.sb(st, [128, 512]); Btok = self.sb(st, [128, 256], BF16)
        xdt = self.sb(st, [128, 8, 64], BF16); xdtw = self.sb(st, [128, 8, 64], BF16)
        gt = self.sb(st, [128, 8]); dt = self.sb(st, [128, 8]); da = self.sb(st, [128, 8])
        sc = self.sb(st, [128, 48])
        lfB = self.sb(st, [128, 128]); Dm = self.sb(st, [128, 128]); PT = self.sb(st, [128, 128], BF16)
        tmpI = self.sb(st, [128, 512]); tmpD = self.sb(st, [128, 512])
        P.op("dve", lambda e: e.memset(Hs.ap, 0.0), w=[Hs])
        P.op("pool", lambda e: e.memset(Hb.ap, 0.0), w=[Hb])
        yield
        for i in self.order(d):
            ts = slice(i * 128, (i + 1) * 128)
            pb = self.bank(pool)
            for g in range(4):
                P.op("pe", lambda e: e.matmul(pb[:, g * 128:(g + 1) * 128], lhsT=xbcT[:, g, ts], rhs=self.idb.ap, start=True, stop=True), r=[xbcT, self.idb], w=[pb])
            P.op("act", lambda e: e.copy(out=xtok.ap, in_=pb[:, :]), r=[pb], w=[xtok])
            yield
            pb = self.bank(pool)
            for g in range(2):
                P.op("pe", lambda e: e.matmul(pb[:, g * 128:(g + 1) * 128], lhsT=xbcT[:, 4 + g, ts], rhs=self.idb.ap, start=True, stop=True), r=[xbcT, self.idb], w=[pb])
            P.op("act", lambda e: e.copy(out=Btok.ap, in_=pb[:, 0:256]), r=[pb], w=[Btok])
            yield
            P.dma("sp", gt.ap, self.UT[ts, O_DT + d * 8:O_DT + d * 8 + 8], w=[gt])
            P.op("dve", lambda e: e.tensor_tensor(out=dt.ap, in0=gt.ap, in1=dtb[:, d * 8:d * 8 + 8], op=ALU.add), r=[gt, dtb], w=[dt])
            yield
            P.op("act", lambda e: e.activation(out=dt.ap, in_=dt.ap, func=AF.Exp), r=[dt], w=[dt])
            yield
            P.op("act", lambda e: e.activation(out=dt.ap, in_=dt.ap, func=AF.Ln, bias=1.0), r=[dt], w=[dt])
            yield
            P.op("dve", lambda e: e.tensor_tensor(out=da.ap, in0=dt.ap, in1=aneg[:, d * 8:d * 8 + 8], op=ALU.mult), r=[dt, aneg], w=[da])
            yield
            self.decay_scalars(sc, da.ap, [da], 8, d, pool)
            yield
            P.op("dve", lambda e: e.tensor_tensor(out=xdt.ap, in0=xtok.ap.rearrange("p (h c) -> p h c", h=8), in1=dt.ap.unsqueeze(2).broadcast_to([128, 8, 64]), op=ALU.mult),
                 r=[xtok, dt], w=[xdt])
            yield
            P.op("pool", lambda e: e.tensor_tensor(out=xdtw.ap, in0=xdt.ap, in1=sc[:, 24:32].unsqueeze(2).broadcast_to([128, 8, 64]), op=ALU.mult), r=[xdt, sc], w=[xdtw])
            pI = self.bank(pool)
            for g in range(2):
                P.op("pe", lambda e: e.matmul(pS[:, g * 128:(g + 1) * 128], lhsT=xbcT[:, 4 + g, ts], rhs=xbcT[:, 6 + g, ts], start=True, stop=True), r=[xbcT], w=[pS])
                P.op("pe", lambda e: e.matmul(pI[:, g * 256:(g + 1) * 256], lhsT=xbcT[:, 6 + g, ts], rhs=Hb[:, g, :], start=True, stop=True), r=[xbcT, Hb], w=[pI])
            yield
            P.op("dve", lambda e: e.tensor_tensor(out=tmpI.ap.rearrange("p (h c) -> p h c", h=8), in0=pI[:, :].rearrange("p (h c) -> p h c", h=8),
                                                  in1=sc[:, 32:40].unsqueeze(2).broadcast_to([128, 8, 64]), op=ALU.mult), r=[pI, sc], w=[tmpI])
            yield
            if d == 0:
                P.op("dve", lambda e: e.tensor_tensor(out=tmpD.ap.rearrange("p (h c) -> p h c", h=8), in0=xtok.ap.rearrange("p (h c) -> p h c", h=8),
                                                      in1=dsk.ap.unsqueeze(2).broadcast_to([128, 8, 64]), op=ALU.mult), r=[xtok, dsk], w=[tmpD])
                yield
                P.op("dve", lambda e: e.tensor_tensor(out=tmpI.ap, in0=tmpI.ap, in1=tmpD.ap, op=ALU.add), r=[tmpI, tmpD], w=[tmpI])
                yield
            for j in range(8):
                g = j // 4
                self.decay_matrix(da[:, j:j + 1], [da], sc[:, 16 + j:17 + j], [sc], d, lfB, Dm, pool)
                yield
                P.op("dve", lambda e: e.tensor_tensor(out=PT.ap, in0=pS[:, g * 128:(g + 1) * 128], in1=Dm.ap, op=ALU.mult), r=[pS, Dm], w=[PT])
                yield
                P.op("pe", lambda e: e.matmul(pO[:, j * 64:(j + 1) * 64], lhsT=PT.ap, rhs=xdt[:, j, :], start=True, stop=True), r=[PT, xdt], w=[pO])
                yield
            P.op("dve", lambda e: e.tensor_tensor(out=yo[:, i, :], in0=pO[:, :], in1=tmpI.ap, op=ALU.add), r=[pO, tmpI], w=[yo])
            yield
            pU = self.bank(pool)
            for g in range(2):
                P.op("pe", lambda e: e.matmul(pU[:, g * 256:(g + 1) * 256], lhsT=Btok[:, g * 128:(g + 1) * 128], rhs=xdtw[:, g * 4:(g + 1) * 4, :].rearrange("p h c -> p (h c)"),
                                              start=True, stop=True), r=[Btok, xdtw], w=[pU])
            P.op("dve", lambda e: e.tensor_tensor(out=Hs.ap.rearrange("p g (h c) -> p (g h) c", h=4), in0=Hs.ap.rearrange("p g (h c) -> p (g h) c", h=4),
                                                  in1=sc[:, 40:48].unsqueeze(2).broadcast_to([128, 8, 64]), op=ALU.mult), r=[Hs, sc], w=[Hs])
            yield
            P.op("dve", lambda e: e.tensor_tensor(out=Hs.ap.rearrange("p g c -> p (g c)"), in0=Hs.ap.rearrange("p g c -> p (g c)"), in1=pU[:, :], op=ALU.add), r=[Hs, pU], w=[Hs])
            yield
            P.op("act", lambda e: e.copy(out=Hb.ap, in_=Hs.ap), r=[Hs], w=[Hb])
            yield

    def phase_hg(self, l):
        pass

    def hg_chain(self, st, l, d, omT, omB, ydst, banks):
        P = self.P
        pool = {"banks": list(banks), "rr": 0}
        yo = self.sb(st, [128, 512])
        Ss = self.sb(st, [128, 4, 128]); Sb = self.sb(st, [128, 4, 128], BF16)
        qT = self.sb(st, [128, 4, 128]); fT = self.sb(st, [128, 4, 128]); kTf = self.sb(st, [128, 4, 128])
        ft = self.sb(st, [128, 512]); lf = self.sb(st, [128, 512]); vt = self.sb(st, [128, 512]); vb = self.sb(st, [128, 512], BF16)
        E = self.sb(st, [128, 128]); Ei = self.sb(st, [128, 128]); ec = self.sb(st, [128, 4])
        qtl = self.sb(st, [128, 128], BF16); ktl = self.sb(st, [128, 128], BF16); ktok = self.sb(st, [128, 128], BF16)
        Af = self.sb(st, [128, 128]); AT = self.sb(st, [128, 128], BF16)
        lc = C_LCF if d == 0 else C_LCB
        tri = C_TRF if d == 0 else C_TRB
        hc0 = 0 if d == 0 else 3
        P.op("dve", lambda e: e.memset(Ss.ap, 0.0), w=[Ss])
        yield
        for i in self.order(d):
            ts = slice(i * 128, (i + 1) * 128)
            P.dma("sp", qT.ap, self.UF[O_HGQ:O_HGQ + 512, ts].rearrange("(h p) t -> p h t", p=128), w=[qT])
            P.dma("sp", fT.ap, self.UF[O_HGF + d * 512:O_HGF + (d + 1) * 512, ts].rearrange("(h p) t -> p h t", p=128), w=[fT])
            P.dma("sp", ft.ap, self.UT[ts, O_HGF + d * 512:O_HGF + (d + 1) * 512], w=[ft])
            P.dma("sp", vt.ap, self.UT[ts, O_HGI:O_HGI + 512], w=[vt])
            yield
            P.op("act", lambda e: e.activation(out=qT.ap, in_=qT.ap, func=AF.Silu), r=[qT], w=[qT])
            yield
            P.op("act", lambda e: e.activation(out=kTf.ap, in_=fT.ap, func=AF.Sigmoid, scale=-1.0), r=[fT], w=[kTf])
            yield
            for h in range(4):
                P.op("pool", lambda e: e.tensor_scalar(out=kTf[:, h, :], in0=kTf[:, h, :], scalar1=omT[:, h:h + 1], scalar2=None, op0=ALU.mult), r=[kTf, omT], w=[kTf])
            yield
            P.op("act", lambda e: e.activation(out=ft.ap, in_=ft.ap, func=AF.Sigmoid), r=[ft], w=[ft])
            yield
            P.op("dve", lambda e: e.tensor_tensor(out=ft.ap, in0=ft.ap, in1=omB.ap, op=ALU.mult), r=[ft, omB], w=[ft])
            yield
            P.op("dve", lambda e: e.tensor_tensor(out=ft.ap, in0=ft.ap, in1=self.lbB[:, l, :], op=ALU.add), r=[ft, self.lbB], w=[ft])
            yield
            P.op("act", lambda e: e.activation(out=lf.ap, in_=ft.ap, func=AF.Ln), r=[ft], w=[lf])
            P.op("pool", lambda e: e.tensor_copy(out=vb.ap, in_=vt.ap), r=[vt], w=[vb])
            yield
            for h in range(4):
                hs = slice(h * 128, (h + 1) * 128)
                pG = self.bank(pool)
                P.op("pe", lambda e: e.matmul(pG[:, 0:128], lhsT=lf[:, hs], rhs=self.cst[:, lc, :], start=True, stop=True), r=[lf, self.cst], w=[pG])
                P.op("pe", lambda e: e.matmul(pG[:, 128:132], lhsT=lf[:, hs], rhs=self.cst[:, C_HC, hc0:hc0 + 4], start=True, stop=True), r=[lf, self.cst], w=[pG])
                yield
                P.op("act", lambda e: e.activation(out=E.ap, in_=pG[:, 0:128], func=AF.Exp), r=[pG], w=[E])
                yield
                P.op("act", lambda e: e.activation(out=Ei.ap, in_=pG[:, 0:128], func=AF.Exp, scale=-1.0), r=[pG], w=[Ei])
                yield
                P.op("act", lambda e: e.activation(out=ec.ap, in_=pG[:, 128:132], func=AF.Exp), r=[pG], w=[ec])
                yield
                P.op("dve", lambda e: e.tensor_tensor(out=qtl.ap, in0=qT[:, h, :], in1=E.ap, op=ALU.mult), r=[qT, E], w=[qtl])
                yield
                P.op("dve", lambda e: e.tensor_tensor(out=ktl.ap, in0=kTf[:, h, :], in1=Ei.ap, op=ALU.mult), r=[kTf, Ei], w=[ktl])
                yield
                P.op("dve", lambda e: e.tensor_scalar(out=Sb[:, h, :], in0=Ss[:, h, :], scalar1=ec[:, 0:1], scalar2=None, op0=ALU.mult), r=[Ss, ec], w=[Sb])
                yield
                pA = self.bank(pool)
                P.op("pe", lambda e: e.matmul(pA[:, 0:128], lhsT=ktl.ap, rhs=qtl.ap, start=True, stop=True), r=[ktl, qtl], w=[pA])
                pK = self.bank(pool)
                P.op("pe", lambda e: e.matmul(pK[:, 0:128], lhsT=ktl.ap, rhs=self.idb.ap, start=True, stop=True), r=[ktl, self.idb], w=[pK])
                yield
                P.op("dve", lambda e: e.tensor_scalar(out=Af.ap, in0=pA[:, 0:128], scalar1=-1e30, scalar2=1e30, op0=ALU.max, op1=ALU.min), r=[pA], w=[Af])
                P.op("act", lambda e: e.copy(out=ktok.ap, in_=pK[:, 0:128]), r=[pK], w=[ktok])
                yield
                P.op("dve", lambda e: e.tensor_tensor(out=AT.ap, in0=Af.ap, in1=self.cst[:, tri, :], op=ALU.mult), r=[Af, self.cst], w=[AT])
                yield
                pO = self.bank(pool)
                P.op("pe", lambda e: e.matmul(pO[:, 0:128], lhsT=AT.ap, rhs=vb[:, hs], start=True, stop=False), r=[AT, vb], w=[pO])
                P.op("pe", lambda e: e.matmul(pO[:, 0:128], lhsT=qtl.ap, rhs=Sb[:, h, :], start=False, stop=True), r=[qtl, Sb], w=[pO])
                yield
                P.op("act", lambda e: e.copy(out=yo[:, hs], in_=pO[:, 0:128]), r=[pO], w=[yo])
                yield
                pU = self.bank(pool)
                P.op("pe", lambda e: e.matmul(pU[:, 0:128], lhsT=ktok.ap, rhs=vb[:, hs], start=True, stop=True), r=[ktok, vb], w=[pU])
                yield
                P.op("dve", lambda e: e.tensor_scalar(out=Ss[:, h, :], in0=Ss[:, h, :], scalar1=ec[:, 1:2], scalar2=None, op0=ALU.mult), r=[Ss, ec], w=[Ss])
                yield
                P.op("dve", lambda e: e.scalar_tensor_tensor(out=Ss[:, h, :], in0=pU[:, 0:128], scalar=ec[:, 2:3], in1=Ss[:, h, :], op0=ALU.mult, op1=ALU.add), r=[pU, ec, Ss], w=[Ss])
                yield
            P.dma("sp", self.YD[ydst[0], ydst[1], ts, :], yo.ap, r=[yo])
            yield

    def phase_mla(self, l):
        P = self.P
        T, NT, NCX = self.T, self.NT, self.n_ctx
        with ExitStack() as st:
            qnT = self.sb(st, [128, 4, T], BF16); qrT = self.sb(st, [128, 4, T], BF16)
            knT = self.sb(st, [128, 4, T], BF16); krT = self.sb(st, [128, T], BF16)
            vaug = self.sb(st, [128, NT, 4, 132], BF16)
            P.op("pool", lambda e: e.memset(vaug.ap, 1.0), w=[vaug])
            with ExitStack() as s2:
                gq = self.sb(s2, [128, 512]); gkv = self.sb(s2, [128, 256])
                P.dma("sp", gq.ap, self.mla_g_q[l:l + 1, :].broadcast_to([128, 512]), w=[gq])
                P.dma("sp", gkv.ap, self.mla_g_kv[l:l + 1, :].broadcast_to([128, 256]), w=[gkv])
                cqT = self.sb(s2, [128, 4, T], BF16); ckvT = self.sb(s2, [128, 2, T], BF16)
                cosT = self.sb(s2, [128, T]); sinT = self.sb(s2, [128, T])
                P.op("dve", lambda e: e.memset(cosT.ap, 0.0), w=[cosT])
                P.op("dve", lambda e: e.memset(sinT.ap, 0.0), w=[sinT])
                P.dma("sp", cosT[0:64, :], self.rope_in[0:64, :], w=[cosT])
                P.dma("sp", sinT[0:64, :], self.rope_in[64:128, :], w=[sinT])
                permb = self.sb(s2, [128, 128], BF16)
                P.op("dve", lambda e: e.tensor_copy(out=permb.ap, in_=self.cst[:, C_PERM, :]), r=[self.cst], w=[permb])
                wst = self.sb(s2, [128, 4, 768]); Wuq = self.sb(s2, [128, 4, 768], BF16)
                wst2 = self.sb(s2, [128, 2, 1024]); Wukv = self.sb(s2, [128, 2, 1024], BF16)
                Wr = self.sb(s2, [128, 4, 4, 128], BF16)
                P.dma("sp", wst.ap, self.mla_w_uq[l].rearrange("(k p) n -> p k n", p=128), w=[wst])
                P.dma("sp", wst2.ap, self.mla_w_ukv[l].rearrange("(k p) n -> p k n", p=128), w=[wst2])
                P.op("dve", lambda e: e.tensor_copy(out=Wuq.ap, in_=wst.ap), r=[wst], w=[Wuq])
                P.op("dve", lambda e: e.tensor_copy(out=Wukv.ap, in_=wst2.ap), r=[wst2], w=[Wukv])
                P.op("dve", lambda e: e.memset(Wr.ap, 0.0), w=[Wr])
                for h in range(4):
                    for kc in range(4):
                        P.op("dve", lambda e: e.tensor_copy(out=Wr[:, kc, h, 0:64], in_=wst[:, kc, h * 192 + 128:(h + 1) * 192]), r=[wst], w=[Wr])
                ct = self.sb(s2, [128, 768]); cb_ = self.sb(s2, [128, 768], BF16); junk = self.sb(s2, [128, 512], BF16)
                ss = self.sb(s2, [128, 4]); ss2 = self.sb(s2, [128, 4])
                for i in range(NT):
                    ts = slice(i * 128, (i + 1) * 128)
                    P.dma("sp", ct.ap, self.UT[ts, O_CQ:O_CQ + 768], w=[ct])
                    self.rstd_of(ct[:, 0:512], [ct], 512, junk.ap, ss)
                    self.rstd_of(ct[:, 512:768], [ct], 256, junk[:, 0:256], ss2)
                    P.op("dve", lambda e: e.scalar_tensor_tensor(out=cb_[:, 0:512], in0=ct[:, 0:512], scalar=ss[:, 3:4], in1=gq.ap, op0=ALU.mult, op1=ALU.mult), r=[ct, ss, gq], w=[cb_])
                    P.op("dve", lambda e: e.scalar_tensor_tensor(out=cb_[:, 512:768], in0=ct[:, 512:768], scalar=ss2[:, 3:4], in1=gkv.ap, op0=ALU.mult, op1=ALU.mult), r=[ct, ss2, gkv], w=[cb_])

                    def dst(q, nb, pb, i=i):
                        for j in range(nb):
                            blk = q + j
                            tgt = cqT[:, blk, i * 128:(i + 1) * 128] if blk < 4 else ckvT[:, blk - 4, i * 128:(i + 1) * 128]
                            P.op("act", lambda e: e.copy(out=tgt, in_=pb[:, j * 128:(j + 1) * 128]), r=[pb], w=[cqT if blk < 4 else ckvT])
                    self.transpose_to(cb_, 768, dst)
                if os.environ.get("MLA_STOP") == "A":
                    return
                xb = self.sb(s2, [128, 512], BF16); t1 = self.sb(s2, [128, 512]); t2 = self.sb(s2, [128, 512])
                xk = self.sb(s2, [128, T])

                def rope(src_ap, src_dep, out_ap, out_tl, t0, n):
                    P.op("act", lambda e: e.copy(out=xb[:, 0:n], in_=src_ap), r=src_dep, w=[xb])
                    pP = self.bank()
                    P.op("pe", lambda e: e.matmul(pP[:, 0:n], lhsT=permb.ap, rhs=xb[:, 0:n], start=True, stop=True), r=[permb, xb], w=[pP])
                    P.op("dve", lambda e: e.tensor_tensor(out=t1[:, 0:n], in0=src_ap, in1=cosT[:, t0:t0 + n], op=ALU.mult), r=src_dep + [cosT], w=[t1])
                    P.op("dve", lambda e: e.tensor_tensor(out=t2[:, 0:n], in0=pP[:, 0:n], in1=sinT[:, t0:t0 + n], op=ALU.mult), r=[pP, sinT], w=[t2])
                    P.op("dve", lambda e: e.tensor_tensor(out=out_ap, in0=t1[:, 0:n], in1=t2[:, 0:n], op=ALU.add), r=[t1, t2], w=[out_tl])

                P.op("dve", lambda e: e.memset(xk.ap, 0.0), w=[xk])
                P.dma("sp", xk[0:64, :], self.UF[O_KR:O_KR + 64, :], w=[xk])
                for (t0, n) in self.tok_blocks(0, T):
                    rope(xk[:, t0:t0 + n], [xk], krT[:, t0:t0 + n], krT, t0, n)
                    if os.environ.get("MLA_STOP") == "B1":
                        return
                    for h in range(4):
                        pq = self.bank()
                        for kc in range(4):
                            P.op("pe", lambda e: e.matmul(pq[:, 0:n], lhsT=Wuq[:, kc, h * 192:h * 192 + 128], rhs=cqT[:, kc, t0:t0 + n], start=(kc == 0), stop=(kc == 3)), r=[Wuq, cqT], w=[pq])
                        P.op("act", lambda e: e.copy(out=qnT[:, h, t0:t0 + n], in_=pq[:, 0:n]), r=[pq], w=[qnT])
                        if os.environ.get("MLA_STOP") == "B2a":
                            return
                        pr = self.bank()
                        for kc in range(4):
                            P.op("pe", lambda e: e.matmul(pr[:, 0:n], lhsT=Wr[:, kc, h, :], rhs=cqT[:, kc, t0:t0 + n], start=(kc == 0), stop=(kc == 3)), r=[Wr, cqT], w=[pr])
                        rope(pr[:, 0:n], [pr], qrT[:, h, t0:t0 + n], qrT, t0, n)
                        if os.environ.get("MLA_STOP") == "B2b":
                            return
                        pk = self.bank()
                        for kc in range(2):
                            P.op("pe", lambda e: e.matmul(pk[:, 0:n], lhsT=Wukv[:, kc, h * 256:h * 256 + 128], rhs=ckvT[:, kc, t0:t0 + n], start=(kc == 0), stop=(kc == 1)), r=[Wukv, ckvT], w=[pk])
                        P.op("act", lambda e: e.copy(out=knT[:, h, t0:t0 + n], in_=pk[:, 0:n]), r=[pk], w=[knT])
                if os.environ.get("MLA_STOP") == "B2":
                    return
                for i in range(NT):
                    pv = self.bank()
                    for h in range(4):
                        for kc in range(2):
                            P.op("pe", lambda e: e.matmul(pv[:, h * 128:(h + 1) * 128], lhsT=ckvT[:, kc, i * 128:(i + 1) * 128], rhs=Wukv[:, kc, h * 256 + 128:(h + 1) * 256],
                                                          start=(kc == 0), stop=(kc == 1)), r=[ckvT, Wukv], w=[pv])
                    P.op("act", lambda e: e.copy(out=vaug[:, i, :, 0:128], in_=pv[:, :].rearrange("p (h c) -> p h c", h=4)), r=[pv], w=[vaug])
                P.barrier()
            if os.environ.get("MLA_STOP") == "B":
                return
            (acc,) = self.take_banks(1)
            PTe = [self.sb(st, [128, 128], BF16) for _ in range(2)]
            ob = [self.sb(st, [128, 512]) for _ in range(2)]
            rc = self.sb(st, [128, 4])
            scale = float((128 + 64) ** -0.5)
            qtiles = [(i, list(range(self.NCT))) for i in range(self.NCT)] + [(i, list(range(NT))) for i in range(self.NCT, NT)]
            pi = 0
            for oi, (qi, ktl) in enumerate(qtiles):
                qsl = slice(qi * 128, (qi + 1) * 128)
                o_ = ob[oi % 2]
                for h in range(4):
                    for kt in ktl:
                        ks = slice(kt * 128, (kt + 1) * 128)
                        pST = self.bank()
                        P.op("pe", lambda e: e.matmul(pST[:, 0:128], lhsT=knT[:, h, ks], rhs=qnT[:, h, qsl], start=True, stop=False), r=[knT, qnT], w=[pST])
                        P.op("pe", lambda e: e.matmul(pST[:, 0:128], lhsT=krT[:, ks], rhs=qrT[:, h, qsl], start=False, stop=True), r=[krT, qrT], w=[pST])
                        pt = PTe[pi % 2]
                        pi += 1
                        P.op("act", lambda e: e.activation(out=pt.ap, in_=pST[:, 0:128], func=AF.Exp, scale=scale), r=[pST], w=[pt])
                        P.op("pe", lambda e: e.matmul(acc[:, 0:129], lhsT=pt.ap, rhs=vaug[:, kt, h, 0:129], start=(kt == ktl[0]), stop=(kt == ktl[-1])), r=[pt, vaug], w=[acc])
                    P.op("dve", lambda e: e.reciprocal(out=rc[:, h:h + 1], in_=acc[:, 128:129]), r=[acc], w=[rc])
                    P.op("dve", lambda e: e.tensor_scalar(out=o_[:, h * 128:(h + 1) * 128], in0=acc[:, 0:128], scalar1=rc[:, h:h + 1], scalar2=None, op0=ALU.mult), r=[acc, rc], w=[o_])
                P.dma("sp", self.BR[2, qsl, :], o_.ap, r=[o_])
            self.release_banks()

    def phase_merge(self, l):
        P = self.P
        T, NT = self.T, self.NT
        with ExitStack() as st:
            self.load_hT(st)
            brT = [self.sb(st, [128, 4, T], BF16) for _ in range(4)]
            with ExitStack() as s2:
                bt = self.sb(s2, [128, 512]); bb = self.sb(s2, [128, 512], BF16)
                for k in range(4):
                    for i in range(NT):
                        P.dma("sp", bt.ap, self.BR[k, i * 128:(i + 1) * 128, :], w=[bt])
                        P.op("dve", lambda e: e.tensor_copy(out=bb.ap, in_=bt.ap), r=[bt], w=[bb])

                        def dst(q, nb, pb, i=i, k=k):
                            P.op("act", lambda e: e.copy(out=brT[k][:, q:q + nb, i * 128:(i + 1) * 128],
                                                         in_=pb[:, 0:nb * 128].rearrange("p (a t) -> p a t", a=nb)), r=[pb], w=[brT[k]])
                        self.transpose_to(bb, 512, dst)
                P.barrier()
            wg = self.wpipe(st, KC, 128)
            wb_ = self.wpipe(st, 4, 128)
            acc = self.sb(st, [128, T]); accb = self.sb(st, [128, T], BF16)
            gs = self.sb(st, [128, 512]); tmp = self.sb(st, [128, 512])
            for n in range(16):
                ns = slice(n * 128, (n + 1) * 128)
                for k in range(4):
                    Wg = self.wload(wg, self.w_gate[l, k, :, ns], KC, 128)
                    Wb = self.wload(wb_, self.w_br[l, k, :, ns], 4, 128)
                    for (t0, nt) in self.tok_blocks(self.tile_lo(l) * 128, T):
                        pg = self.bank()
                        for kc in range(KC):
                            P.op("pe", lambda e: e.matmul(pg[:, 0:nt], lhsT=Wg[:, kc, :], rhs=self.hT[:, kc, t0:t0 + nt], start=(kc == 0), stop=(kc == KC - 1)), r=[Wg, self.hT], w=[pg])
                        P.op("act", lambda e: e.activation(out=gs[:, 0:nt], in_=pg[:, 0:nt], func=AF.Sigmoid), r=[pg], w=[gs])
                        pbk = self.bank()
                        for kc in range(4):
                            P.op("pe", lambda e: e.matmul(pbk[:, 0:nt], lhsT=Wb[:, kc, :], rhs=brT[k][:, kc, t0:t0 + nt], start=(kc == 0), stop=(kc == 3)), r=[Wb, brT[k]], w=[pbk])
                        if k == 0:
                            P.op("dve", lambda e: e.tensor_tensor(out=acc[:, t0:t0 + nt], in0=pbk[:, 0:nt], in1=gs[:, 0:nt], op=ALU.mult), r=[pbk, gs], w=[acc])
                        else:
                            P.op("dve", lambda e: e.tensor_tensor(out=tmp[:, 0:nt], in0=pbk[:, 0:nt], in1=gs[:, 0:nt], op=ALU.mult), r=[pbk, gs], w=[tmp])
                            P.op("dve", lambda e: e.tensor_tensor(out=acc[:, t0:t0 + nt], in0=acc[:, t0:t0 + nt], in1=tmp[:, 0:nt], op=ALU.add), r=[acc, tmp], w=[acc])
                tl0 = self.tile_lo(l) * 128
                P.op("act", lambda e: e.copy(out=accb[:, tl0:T], in_=acc[:, tl0:T]), r=[acc], w=[accb])
                P.dma("sp", self.ACC[ns, tl0:T], accb[:, tl0:T], r=[accb])

    def phase_wout(self, l):
        P = self.P
        T, NT = self.T, self.NT
        with ExitStack() as st:
            accT = self.sb(st, [128, KC, T], BF16)
            tl0 = self.tile_lo(l) * 128
            P.dma("sp", accT[:, :, tl0:T], self.ACC[:, tl0:T].rearrange("(k p) t -> p k t", p=128), w=[accT])
            wp = self.wpipe(st, KC, 256)
            ev = [self.sb(st, [128, 256]) for _ in range(2)]
            ei = 0
            for c in range(8):
                Wo = self.wload(wp, self.w_out[l, :, c * 256:(c + 1) * 256], KC, 256)
                for i in range(self.tile_lo(l), NT):
                    pb = self.bank()
                    for kc in range(KC):
                        P.op("pe", lambda e: e.matmul(pb[:, 0:256], lhsT=accT[:, kc, i * 128:(i + 1) * 128], rhs=Wo[:, kc, :], start=(kc == 0), stop=(kc == KC - 1)), r=[accT, Wo], w=[pb])
                    e_ = ev[ei % 2]
                    ei += 1
                    P.op("act", lambda e: e.copy(out=e_.ap, in_=pb[:, 0:256]), r=[pb], w=[e_])
                    P.dma("sp", self.Y[i * 128:(i + 1) * 128, c * 256:(c + 1) * 256], e_.ap, r=[e_])
            P.barrier()
        self.residual(l, "G2")
        P.barrier()
        self.norm_mod_to_hT(l, "A2", "B2", self.tile_lo(l))

    def residual(self, l, kind):
        P = self.P
        with ExitStack() as st:
            G = self.load_modvec(st, l, kind)
            yt = [self.sb(st, [128, D]) for _ in range(2)]
            xt = [self.sb(st, [128, D]) for _ in range(2)]
            junk = self.sb(st, [128, D], BF16)
            ss = [self.sb(st, [128, 4]) for _ in range(2)]
            for i in range(self.tile_lo(l), self.NT):
                j = 1 if i < self.NCT else 0
                y_, x_, s_ = yt[i % 2], xt[i % 2], ss[i % 2]
                P.dma("sp", y_.ap, self.Y[i * 128:(i + 1) * 128, :], w=[y_])
                P.dma("sp", x_.ap, self.xres[i * 128:(i + 1) * 128, :], w=[x_])
                self.rstd_of(y_.ap, [y_], D, junk.ap, s_)
                P.op("dve", lambda e: e.scalar_tensor_tensor(out=y_.ap, in0=y_.ap, scalar=s_[:, 3:4], in1=G[:, j, :], op0=ALU.mult, op1=ALU.mult), r=[y_, s_, G], w=[y_])
                P.op("dve", lambda e: e.tensor_tensor(out=x_.ap, in0=x_.ap, in1=y_.ap, op=ALU.add), r=[x_, y_], w=[x_])
                P.dma("sp", self.xres[i * 128:(i + 1) * 128, :], x_.ap, r=[x_])

    def phase_moe(self, l):
        P = self.P
        T, NT = self.T, self.NT
        NE = 1 if self.tiny_moe else 32
        TL = self.tile_lo(l)
        ntl = NT - TL
        npass = -(-ntl // 9)
        PTK = -(-ntl // npass)
        with ExitStack() as st:
            comb = self.sb(st, [128, NT, 32])
            with ExitStack() as s2:
                self.load_hT(s2)
                ws = self.sb(s2, [128, KC, 36]); wr = self.sb(s2, [128, KC, 36], BF16)
                P.dma("sp", ws[:, :, 0:4], self.w_grp[l].rearrange("(k p) n -> p k n", p=128), w=[ws])
                P.dma("sp", ws[:, :, 4:36], self.w_exp[l].rearrange("(k p) n -> p k n", p=128), w=[ws])
                P.op("dve", lambda e: e.tensor_copy(out=wr.ap, in_=ws.ap), r=[ws], w=[wr])
                lg = self.sb(s2, [128, 36]); sc = self.sb(s2, [128, 16]); gm = self.sb(s2, [128, 4]); ge = self.sb(s2, [128, 4])
                t48 = self.sb(s2, [128, 4, 8]); sel = self.sb(s2, [128, 8]); sel2 = self.sb(s2, [128, 8]); e1 = self.sb(s2, [128, 8]); e2 = self.sb(s2, [128, 8]); c8 = self.sb(s2, [128, 8])
                for i in range(TL, NT):
                    pb = self.bank()
                    for kc in range(KC):
                        P.op("pe", lambda e: e.matmul(pb[:, 0:36], lhsT=self.hT[:, kc, i * 128:(i + 1) * 128], rhs=wr[:, kc, :], start=(kc == 0), stop=(kc == KC - 1)), r=[self.hT, wr], w=[pb])
                    P.op("dve", lambda e: e.tensor_copy(out=lg.ap, in_=pb[:, 0:36]), r=[pb], w=[lg])
                    P.op("dve", lambda e: e.tensor_reduce(out=sc[:, 0:1], in_=lg[:, 0:4], axis=AX.X, op=ALU.max), r=[lg], w=[sc])
                    P.op("dve", lambda e: e.tensor_scalar(out=sc[:, 1:2], in0=sc[:, 0:1], scalar1=-1.0, scalar2=None, op0=ALU.mult), r=[sc], w=[sc])
                    P.op("act", lambda e: e.activation(out=ge.ap, in_=lg[:, 0:4], func=AF.Exp, bias=sc[:, 1:2], scale=1.0), r=[lg, sc], w=[ge])
                    P.op("dve", lambda e: e.tensor_reduce(out=sc[:, 2:3], in_=ge.ap, axis=AX.X, op=ALU.add), r=[ge], w=[sc])
                    P.op("dve", lambda e: e.reciprocal(out=sc[:, 3:4], in_=sc[:, 2:3]), r=[sc], w=[sc])
                    P.op("dve", lambda e: e.tensor_scalar(out=gm.ap, in0=lg[:, 0:4], scalar1=sc[:, 0:1], scalar2=None, op0=ALU.is_equal), r=[lg, sc], w=[gm])
                    P.op("dve", lambda e: e.tensor_tensor(out=t48.ap, in0=lg[:, 4:36].rearrange("p (g e) -> p g e", g=4), in1=gm.ap.unsqueeze(2).broadcast_to([128, 4, 8]), op=ALU.mult), r=[lg, gm], w=[t48])
                    P.op("dve", lambda e: e.tensor_reduce(out=sel.ap, in_=t48.ap.rearrange("p g e -> p e g"), axis=AX.X, op=ALU.add), r=[t48], w=[sel])
                    P.op("dve", lambda e: e.tensor_reduce(out=sc[:, 4:5], in_=sel.ap, axis=AX.X, op=ALU.max), r=[sel], w=[sc])
                    P.op("dve", lambda e: e.tensor_scalar(out=e1.ap, in0=sel.ap, scalar1=sc[:, 4:5], scalar2=None, op0=ALU.is_equal), r=[sel, sc], w=[e1])
                    P.op("dve", lambda e: e.scalar_tensor_tensor(out=sel2.ap, in0=e1.ap, scalar=-1e30, in1=sel.ap, op0=ALU.mult, op1=ALU.add), r=[e1, sel], w=[sel2])
                    P.op("dve", lambda e: e.tensor_reduce(out=sc[:, 5:6], in_=sel2.ap, axis=AX.X, op=ALU.max), r=[sel2], w=[sc])
                    P.op("dve", lambda e: e.tensor_scalar(out=e2.ap, in0=sel2.ap, scalar1=sc[:, 5:6], scalar2=None, op0=ALU.is_equal), r=[sel2, sc], w=[e2])
                    P.op("dve", lambda e: e.tensor_tensor(out=sc[:, 6:7], in0=sc[:, 5:6], in1=sc[:, 4:5], op=ALU.subtract), r=[sc], w=[sc])
                    P.op("act", lambda e: e.activation(out=sc[:, 6:7], in_=sc[:, 6:7], func=AF.Exp), r=[sc], w=[sc])
                    P.op("dve", lambda e: e.tensor_scalar(out=sc[:, 6:7], in0=sc[:, 6:7], scalar1=1.0, scalar2=None, op0=ALU.add), r=[sc], w=[sc])
                    P.op("dve", lambda e: e.reciprocal(out=sc[:, 7:8], in_=sc[:, 6:7]), r=[sc], w=[sc])
                    P.op("dve", lambda e: e.tensor_tensor(out=sc[:, 8:9], in0=sc[:, 7:8], in1=sc[:, 3:4], op=ALU.mult), r=[sc], w=[sc])
                    P.op("dve", lambda e: e.tensor_tensor(out=sc[:, 9:10], in0=sc[:, 3:4], in1=sc[:, 8:9], op=ALU.subtract), r=[sc], w=[sc])
                    P.op("dve", lambda e: e.tensor_scalar(out=c8.ap, in0=e1.ap, scalar1=sc[:, 8:9], scalar2=None, op0=ALU.mult), r=[e1, sc], w=[c8])
                    P.op("dve", lambda e: e.scalar_tensor_tensor(out=c8.ap, in0=e2.ap, scalar=sc[:, 9:10], in1=c8.ap, op0=ALU.mult, op1=ALU.add), r=[e2, sc, c8], w=[c8])
                    for g in range(4):
                        P.op("dve", lambda e: e.tensor_scalar(out=comb[:, i, g * 8:(g + 1) * 8], in0=c8.ap, scalar1=gm[:, g:g + 1], scalar2=None, op0=ALU.mult), r=[c8, gm], w=[comb])
                P.barrier()
            w13 = self.wpipe(st, KC, 128, nbuf=4)
            w2p = self.wpipe(st, 4, 512, nbuf=2)
            hTp = self.sb(st, [128, KC, PTK * 128], BF16)
            aT = self.sb(st, [128, 4, PTK * 128], BF16)
            acc = self.sb(st, [128, PTK, D])
            s1 = [self.sb(st, [128, 512]) for _ in range(2)]
            si = 0
            for p0 in range(TL, NT, PTK):
                np_ = min(PTK, NT - p0)
                ntok = np_ * 128
                P.dma("sp", hTp[:, :, 0:ntok], self.HTd[:, :, p0 * 128:p0 * 128 + ntok], w=[hTp])
                for ex in range(NE):
                    for c in range(4):
                        W1 = self.wload(w13, self.w1[l, ex, :, c * 128:(c + 1) * 128], KC, 128)
                        W3 = self.wload(w13, self.w3[l, ex, :, c * 128:(c + 1) * 128], KC, 128)
                        for (t0, nt) in self.tok_blocks(0, ntok):
                            p1 = self.bank()
                            for kc in range(KC):
                                P.op("pe", lambda e: e.matmul(p1[:, 0:nt], lhsT=W1[:, kc, :], rhs=hTp[:, kc, t0:t0 + nt], start=(kc == 0), stop=(kc == KC - 1)), r=[W1, hTp], w=[p1])
                            p3 = self.bank()
                            for kc in range(KC):
                                P.op("pe", lambda e: e.matmul(p3[:, 0:nt], lhsT=W3[:, kc, :], rhs=hTp[:, kc, t0:t0 + nt], start=(kc == 0), stop=(kc == KC - 1)), r=[W3, hTp], w=[p3])
                            s_ = s1[si % 2]
                            si += 1
                            P.op("act", lambda e: e.activation(out=s_[:, 0:nt], in_=p1[:, 0:nt], func=AF.Silu), r=[p1], w=[s_])
                            P.op("dve", lambda e: e.tensor_tensor(out=aT[:, c, t0:t0 + nt], in0=p3[:, 0:nt], in1=s_[:, 0:nt], op=ALU.mult), r=[p3, s_], w=[aT])
                    for cc in range(4):
                        W2 = self.wload(w2p, self.w2[l, ex, :, cc * 512:(cc + 1) * 512], 4, 512)
                        for t in range(np_):
                            py = self.bank()
                            for kc in range(4):
                                P.op("pe", lambda e: e.matmul(py[:, :], lhsT=aT[:, kc, t * 128:(t + 1) * 128], rhs=W2[:, kc, :], start=(kc == 0), stop=(kc == 3)), r=[aT, W2], w=[py])
                            cw_ = comb[:, p0 + t, ex:ex + 1]
                            if ex == 0:
                                P.op("dve", lambda e: e.tensor_scalar(out=acc[:, t, cc * 512:(cc + 1) * 512], in0=py[:, :], scalar1=cw_, scalar2=None, op0=ALU.mult), r=[py, comb], w=[acc])
                            else:
                                P.op("dve", lambda e: e.scalar_tensor_tensor(out=acc[:, t, cc * 512:(cc + 1) * 512], in0=py[:, :], scalar=cw_, in1=acc[:, t, cc * 512:(cc + 1) * 512],
                                                                             op0=ALU.mult, op1=ALU.add), r=[py, comb, acc], w=[acc])
                for t in range(np_):
                    P.dma("sp", self.Y[(p0 + t) * 128:(p0 + t + 1) * 128, :], acc[:, t, :], r=[acc])

    def phase_fin(self, l):
        self.residual(l, "G4")


def core_inputs(inputs, b, n_ctx, n_lat, consts, rope, tiny_moe=False):
    m = {}
    f = lambda a: np.ascontiguousarray(a, dtype=np.float32)
    m["x"] = f(inputs["x"][b, :n_lat])
    m["ctx"] = f(inputs["ctx"][b, :n_ctx])
    m["c"] = f(f(inputs["c"][b]).reshape(16, 128).T)
    m["c_ctx"] = f(f(inputs["c_ctx"]).reshape(16, 128).T)
    for k in ["w_ada", "b_ada", "norm_g", "w_in", "hg_lb_logits", "hg_norm_g", "mla_g_q", "mla_g_kv", "mla_w_uq",
              "mla_w_ukv", "ssd_conv_w", "ssd_conv_b", "ssd_norm_g", "w_gate", "w_br", "w_out", "moe_w_grp",
              "moe_w_exp", "moe_w1", "moe_w3", "moe_w2", "ssd_d"]:
        m[k] = f(inputs[k])
    L = inputs["w_in"].shape[0]
    m["ssd_conv_w"] = f(f(inputs["ssd_conv_w"]).reshape(L, 3, 8, 128).transpose(0, 3, 2, 1))
    m["ssd_conv_b"] = f(f(inputs["ssd_conv_b"]).reshape(L, 8, 128).transpose(0, 2, 1))
    m["hg_lbT"] = f(f(inputs["hg_lb_logits"]).reshape(L + 1, 4, 128).transpose(2, 0, 1))
    m["ml_f_bias"] = f(inputs["ml_f_bias"]).reshape(L, 8)
    m["ssd_a_log"] = f(inputs["ssd_a_log"]).reshape(L, 16)
    m["ssd_dt_bias"] = f(inputs["ssd_dt_bias"]).reshape(L, 16)
    if tiny_moe:
        for k in ["moe_w1", "moe_w3", "moe_w2"]:
            m[k] = np.ascontiguousarray(m[k][:, 0:1])
    m["consts"] = consts
    m["rope"] = rope
    return m


def kernel(**inputs):
    B, n_lat, _ = inputs["x"].shape
    n_ctx = inputs["ctx"].shape[1]
    mk = MK(n_ctx, n_lat)
    nc = mk.build()
    consts, rope = make_consts(n_ctx, n_lat)
    in_maps = [core_inputs(inputs, b, n_ctx, n_lat, consts, rope) for b in range(B)]
    res = run_bass_kernel_spmd(nc, in_maps, core_ids=list(range(B)))
    return np.stack([np.asarray(r["out"]) for r in res.results], axis=0).astype(np.float32)
```

```python
import os
import numpy as np
from contextlib import ExitStack
import concourse.bass as bass
import concourse.mybir as mybir
from concourse.bass_utils import run_bass_kernel_spmd

F32 = mybir.dt.float32
BF16 = mybir.dt.bfloat16
AF = mybir.ActivationFunctionType
ALU = mybir.AluOpType
AX = mybir.AxisListType

D = 2048
KC = 16
DIN = 7008
EPS = 1e-6
NEG = -30000.0
EPOCH = 20000
N_DMA_SEMS = 24

O_MLQ, O_MLK, O_MLV, O_MLO, O_MLG = 0, 512, 1024, 1536, 2048
O_HGQ, O_HGI, O_HGG, O_HGF = 2064, 2576, 3088, 3600
O_CQ, O_CKV, O_KR = 4624, 5136, 5392
O_SZ, O_XBC, O_DT = 5456, 5968, 6992


class Buf:
    __slots__ = ("w", "r", "excl")

    def __init__(self):
        self.w = None
        self.r = {}
        self.excl = False


class Tl:
    def __init__(self, t):
        self.t = t
        self.b = Buf()

    def __getitem__(self, idx):
        return self.t.ap()[idx]

    @property
    def ap(self):
        return self.t.ap()


class Prog:
    def __init__(self, nc):
        self.nc = nc
        self.engs = {"pe": nc.tensor, "act": nc.scalar, "dve": nc.vector,
                     "pool": nc.gpsimd, "sp": nc.sync}
        self.sem = {}
        self.cnt = {}
        self.nsem = 0
        self.last = {}
        for e in self.engs:
            self._new_epoch(e)
        self.waited = {e: {} for e in self.engs}
        self.dma_sems = []
        for i in range(N_DMA_SEMS):
            s = nc.alloc_semaphore(f"dsem{i}")
            self.dma_sems.append([s, 0, ("d", i)])
        self.dma_rr = 0
        self.ninst = 0

    def _new_epoch(self, e):
        self.nsem += 1
        s = self.nc.alloc_semaphore(f"s_{e}_{self.nsem}")
        self.sem[e] = (s, ("e", e, self.nsem))
        self.cnt[e] = 0

    def _wait(self, eng, evs):
        best = {}
        for ev in evs:
            if ev is None:
                continue
            sem, val, key, src = ev
            if eng == "pe" and src == "pe":
                continue
            if self.waited[eng].get(key, 0) >= val:
                continue
            if key not in best or best[key][1] < val:
                best[key] = ev
        for key, (sem, val, _, _) in best.items():
            self.engs[eng].wait_ge(sem, val)
            self.waited[eng][key] = val

    def _tick(self, eng, inst):
        if self.cnt[eng] >= EPOCH:
            self._new_epoch(eng)
        sem, key = self.sem[eng]
        self.cnt[eng] += 1
        inst.then_inc(sem, 1)
        self.ninst += 1
        ev = (sem, self.cnt[eng], key, eng)
        self.last[key] = ev
        return ev

    @staticmethod
    def _deps(r, w):
        evs = []
        for b in r:
            evs.append(b.w)
            if b.excl:
                evs.extend(b.r.values())
        for b in w:
            evs.append(b.w)
            evs.extend(b.r.values())
        return evs

    @staticmethod
    def _commit(ev, r, w):
        for b in r:
            old = b.r.get(ev[2])
            if old is None or old[1] < ev[1]:
                b.r[ev[2]] = ev
        for b in w:
            b.w = ev
            b.r = {}

    def op(self, eng, fn, r=(), w=()):
        r = [x.b if isinstance(x, Tl) else x for x in r]
        w = [x.b if isinstance(x, Tl) else x for x in w]
        self._wait(eng, self._deps(r, w))
        inst = fn(self.engs[eng])
        ev = self._tick(eng, inst)
        self._commit(ev, r, w)
        return ev

    def dma(self, q, out, in_, r=(), w=(), **kw):
        r = [x.b if isinstance(x, Tl) else x for x in r]
        w = [x.b if isinstance(x, Tl) else x for x in w]
        ent = self.dma_sems[self.dma_rr]
        self.dma_rr = (self.dma_rr + 1) % len(self.dma_sems)
        sem, val, key = ent
        evs = self._deps(r, w)
        if val > 0:
            evs.append((sem, val, key, "dma"))
        self._wait(q, evs)
        inst = self.engs[q].dma_start(out=out, in_=in_, **kw)
        ent[1] = val + 16
        inst.then_inc(sem, 16)
        ev = (sem, val + 16, key, "dma")
        self._commit(ev, r, w)
        self.last[key] = ev
        self.ninst += 1
        return ev

    def barrier(self):
        evs = list(self.last.values())
        for e in self.engs:
            self._wait(e, evs)


C_ID, C_TRF, C_TRB, C_MNF, C_MNB, C_ONE, C_LCF, C_LCB, C_HC, C_PERM = range(10)
NCST = 10


def make_consts(n_ctx, n_lat):
    s = np.arange(128)[:, None]
    t = np.arange(128)[None, :]
    c = np.zeros((NCST, 128, 128), np.float32)
    c[C_ID] = np.eye(128)
    c[C_TRF] = (s <= t)
    c[C_TRB] = (s >= t)
    c[C_MNF] = np.where(s <= t, 0.0, NEG)
    c[C_MNB] = np.where(s >= t, 0.0, NEG)
    c[C_ONE] = 1.0
    c[C_LCF] = (s <= t).astype(np.float32) - (s <= 63)
    c[C_LCB] = (s >= t).astype(np.float32) - (s >= 64)
    sv = np.arange(128)
    c[C_HC][:, 0] = (sv <= 63)
    c[C_HC][:, 1] = 1.0
    c[C_HC][:, 2] = 1.0 - (sv <= 63)
    c[C_HC][:, 3] = (sv >= 64)
    c[C_HC][:, 4] = 1.0
    c[C_HC][:, 5] = 1.0 - (sv >= 64)
    Pm = np.zeros((64, 64), np.float32)
    for base in (0, 32):
        for i in range(16):
            Pm[base + i, base + 16 + i] = -1.0
            Pm[base + 16 + i, base + i] = 1.0
    c[C_PERM][:64, :64] = Pm.T
    consts = np.ascontiguousarray(c.transpose(1, 0, 2).reshape(128, NCST * 128))
    T = n_ctx + n_lat
    tok = np.arange(n_lat)
    pr, pc = tok // 64, tok % 64
    inv = (10000.0 ** (-np.arange(16, dtype=np.float32) / 16)).astype(np.float32)
    cos = np.ones((64, T), np.float32)
    sin = np.zeros((64, T), np.float32)
    for base, pos in ((0, pr), (32, pc)):
        ang = pos.astype(np.float32)[None, :] * inv[:, None]
        cos[base:base + 16, n_ctx:] = np.cos(ang)
        cos[base + 16:base + 32, n_ctx:] = np.cos(ang)
        sin[base:base + 16, n_ctx:] = np.sin(ang)
        sin[base + 16:base + 32, n_ctx:] = np.sin(ang)
    return consts, np.concatenate([cos, sin], axis=0)


class MK:
    def __init__(self, n_ctx, n_lat, depth=2, dbg=False, upto=None, tiny_moe=False):
        self.n_ctx, self.n_lat, self.depth, self.dbg, self.upto = n_ctx, n_lat, depth, dbg, upto
        self.tiny_moe = tiny_moe
        self.T = n_ctx + n_lat
        self.NT = self.T // 128
        self.NCT = n_ctx // 128
        nc = self.nc = bass.Bass("TRN2", target_bir_lowering=False)
        self.P = Prog(nc)
        self.ins = {}
        self.uid = 0
        self.dumped = set()

    def din(self, name, shape):
        a = self.nc.dram_tensor(name, list(shape), F32, kind="ExternalInput").ap()
        self.ins[name] = a
        return a

    def dscr(self, name, shape, dt=F32):
        kind = "ExternalOutput" if self.dbg else "Internal"
        return self.nc.dram_tensor(name, list(shape), dt, kind=kind).ap()

    def sb(self, st, shape, dt=F32, name=None):
        self.uid += 1
        return Tl(st.enter_context(self.nc.sbuf_tensor(f"{name or 't'}{self.uid}", list(shape), dt)))

    def dump(self, name, tl, ap=None, dt=F32):
        if not self.dbg or name in self.dumped:
            return
        self.dumped.add(name)
        ap = tl.ap if ap is None else ap
        dst = self.nc.dram_tensor("dbg_" + name, list(ap.shape), dt, kind="ExternalOutput").ap()
        self.P.dma("sp", dst, ap, r=[tl])

    def take_banks(self, k):
        self.free_banks = list(range(8 - k))
        self.bank_rr = 0
        return [self.ps[8 - k + i] for i in range(k)]

    def release_banks(self):
        self.free_banks = list(range(8))
        self.bank_rr = 0

    def bank(self, pool=None):
        if pool is not None:
            pool["rr"] = (pool["rr"] + 1) % len(pool["banks"])
            return self.ps[pool["banks"][pool["rr"]]]
        self.bank_rr = (self.bank_rr + 1) % len(self.free_banks)
        return self.ps[self.free_banks[self.bank_rr]]

    @staticmethod
    def drive(gens):
        gens = list(gens)
        while gens:
            for g in list(gens):
                try:
                    next(g)
                except StopIteration:
                    gens.remove(g)

    def tok_blocks(self, t0, t1, bs=512):
        out = []
        while t0 < t1:
            n = min(bs, t1 - t0)
            out.append((t0, n))
            t0 += n
        return out

    def build(self):
        nc, P = self.nc, self.P
        T, NT = self.T, self.NT
        L = self.depth
        I = self.din
        self.x_in = I("x", [self.n_lat, D])
        self.ctx_in = I("ctx", [self.n_ctx, D])
        self.c_in = I("c", [128, KC])
        self.cctx_in = I("c_ctx", [128, KC])
        self.w_ada = I("w_ada", [L, D, 6 * D])
        self.b_ada = I("b_ada", [L, 6 * D])
        self.norm_g = I("norm_g", [L, 4, D])
        self.w_in = I("w_in", [L, D, DIN])
        self.ml_f_bias = I("ml_f_bias", [L, 8])
        self.hg_lb = I("hg_lb_logits", [L + 1, 512])
        self.hg_norm_g = I("hg_norm_g", [L, 512])
        self.hg_lbT = I("hg_lbT", [128, L + 1, 4])
        self.mla_g_q = I("mla_g_q", [L, 512])
        self.mla_g_kv = I("mla_g_kv", [L, 256])
        self.mla_w_uq = I("mla_w_uq", [L, 512, 768])
        self.mla_w_ukv = I("mla_w_ukv", [L, 256, 1024])
        self.ssd_conv_w = I("ssd_conv_w", [L, 128, 8, 3])
        self.ssd_conv_b = I("ssd_conv_b", [L, 128, 8])
        self.ssd_a_log = I("ssd_a_log", [L, 16])
        self.ssd_dt_bias = I("ssd_dt_bias", [L, 16])
        self.ssd_d = I("ssd_d", [L, 8])
        self.ssd_norm_g = I("ssd_norm_g", [L, 512])
        self.w_gate = I("w_gate", [L, 4, D, D])
        self.w_br = I("w_br", [L, 4, 512, D])
        self.w_out = I("w_out", [L, D, D])
        self.w_grp = I("moe_w_grp", [L, D, 4])
        self.w_exp = I("moe_w_exp", [L, D, 32])
        ne = 1 if self.tiny_moe else 32
        self.w1 = I("moe_w1", [L, ne, D, 512])
        self.w3 = I("moe_w3", [L, ne, D, 512])
        self.w2 = I("moe_w2", [L, ne, 512, D])
        self.consts_in = I("consts", [128, NCST * 128])
        self.rope_in = I("rope", [128, T])
        self.out = nc.dram_tensor("out", [self.n_lat, D], F32, kind="ExternalOutput").ap()

        S = self.dscr
        self.xres = S("xres", [T, D])
        self.modv = S("modv", [L, 2, 6, D])
        self.lbd = S("lbd", [L, 512])
        self.UT = S("UT", [T, DIN])
        self.UF = S("UF", [DIN, T])
        self.BR = S("BR", [4, T, 512])
        self.ACC = S("ACC", [D, T], BF16)
        self.Y = S("Y", [T, D])
        self.HTd = S("HTd", [128, KC, T], BF16)

        with ExitStack() as top:
            self.ps = [Tl(top.enter_context(nc.psum_tensor(f"ps{i}", [128, 512], F32))) for i in range(8)]
            for p_ in self.ps:
                p_.b.excl = True
            self.free_banks = list(range(8))
            self.bank_rr = 0
            self.cst = self.sb(top, [128, NCST, 128], F32, "cst")
            self.idb = self.sb(top, [128, 128], BF16, "idb")
            self.lbB = self.sb(top, [128, L, 512], F32, "lbB")
            P.dma("sp", self.cst.ap, self.consts_in.rearrange("p (c n) -> p c n", c=NCST), w=[self.cst])
            P.op("dve", lambda e: e.tensor_copy(out=self.idb.ap, in_=self.cst[:, C_ID, :]), r=[self.cst], w=[self.idb])
            P.dma("sp", self.xres[0:self.n_ctx, :], self.ctx_in)
            P.dma("sp", self.xres[self.n_ctx:T, :], self.x_in)
            steps = [("lb", lambda: self.setup_lb())]
            for l in range(L):
                steps.append((f"mod{l}", lambda l=l: self.phase_mod(l)))
            stages = ["norm1", "win", "ml", "hg", "mla", "ssd", "merge", "wout", "moe", "fin"]
            for l in range(L):
                for st_ in stages:
                    steps.append(((l, st_), lambda l=l, st_=st_: getattr(self, "phase_" + st_)(l)))
            P.barrier()
            if self.upto != "init":
                for name, fn in steps:
                    fn()
                    P.barrier()
                    if self.upto == name:
                        break
            P.dma("sp", self.out, self.xres[self.n_ctx:T, :])
            P.barrier()
        return nc

    def setup_lb(self):
        P, L = self.P, self.depth
        with ExitStack() as st:
            lg = self.sb(st, [128, L + 1, 512])
            ex = self.sb(st, [128, L + 1, 512])
            sm = self.sb(st, [128, 512])
            rc = self.sb(st, [128, 512])
            cum = self.sb(st, [128, 512])
            for j in range(L + 1):
                P.dma("sp", lg[:, j, :], self.hg_lb[j:j + 1, :].broadcast_to([128, 512]), w=[lg])
            P.op("act", lambda e: e.activation(out=ex.ap, in_=lg.ap, func=AF.Exp), r=[lg], w=[ex])
            P.op("dve", lambda e: e.tensor_tensor(out=sm.ap, in0=ex[:, 0, :], in1=ex[:, 1, :], op=ALU.add), r=[ex], w=[sm])
            for j in range(2, L + 1):
                P.op("dve", lambda e: e.tensor_tensor(out=sm.ap, in0=sm.ap, in1=ex[:, j, :], op=ALU.add), r=[ex, sm], w=[sm])
            P.op("dve", lambda e: e.reciprocal(out=rc.ap, in_=sm.ap), r=[sm], w=[rc])
            for l in range(L):
                if l == 0:
                    P.op("dve", lambda e: e.tensor_copy(out=cum.ap, in_=ex[:, 0, :]), r=[ex], w=[cum])
                else:
                    P.op("dve", lambda e: e.tensor_tensor(out=cum.ap, in0=cum.ap, in1=ex[:, l, :], op=ALU.add), r=[ex, cum], w=[cum])
                P.op("dve", lambda e: e.tensor_tensor(out=self.lbB[:, l, :], in0=cum.ap, in1=rc.ap, op=ALU.mult), r=[cum, rc], w=[self.lbB])
                P.dma("sp", self.lbd[l:l + 1, :], self.lbB[0:1, l, :], r=[self.lbB])
            P.barrier()

    def phase_mod(self, l):
        P, nc = self.P, self.nc
        with ExitStack() as st:
            cv = self.sb(st, [128, 2, KC])
            cs = self.sb(st, [128, 2, KC])
            SB = self.sb(st, [128, 2, KC, 128])
            bb = [self.sb(st, [1, 512]) for _ in range(2)]
            mo = [self.sb(st, [1, 2, 512]) for _ in range(2)]
            wst = [self.sb(st, [128, KC, 512]) for _ in range(2)]
            P.dma("sp", cv[:, 0, :], self.c_in, w=[cv])
            P.dma("sp", cv[:, 1, :], self.cctx_in, w=[cv])
            P.op("act", lambda e: e.activation(out=cs.ap, in_=cv.ap, func=AF.Silu), r=[cv], w=[cs])
            for j in range(2):
                for kc in range(KC):
                    P.op("dve", lambda e: e.tensor_copy(out=SB[:, j, kc, :], in_=cs[:, j, kc:kc + 1].broadcast_to([128, 128])), r=[cs], w=[SB])
            def ld(ch):
                P.dma("sp", wst[ch % 2].ap, self.w_ada[l, :, ch * 512:(ch + 1) * 512].rearrange("(k p) n -> p k n", p=128), w=[wst[ch % 2]])
                P.dma("sp", bb[ch % 2].ap, self.b_ada[l:l + 1, ch * 512:(ch + 1) * 512], w=[bb[ch % 2]])
            ld(0)
            for ch in range(24):
                w_, b_, m_ = wst[ch % 2], bb[ch % 2], mo[ch % 2]
                if ch + 1 < 24:
                    ld(ch + 1)
                s_, c0 = divmod(ch * 512, D)
                for j in range(2):
                    pb = self.bank()
                    for kc in range(KC):
                        P.op("pe", lambda e: e.matmul(pb[:, :], lhsT=SB[:, j, kc, :], rhs=w_[:, kc, :], start=(kc == 0), stop=(kc == KC - 1)),
                             r=[SB, w_], w=[pb])
                    P.op("dve", lambda e: e.tensor_tensor(out=m_[:, j, :], in0=pb[0:1, :], in1=b_.ap, op=ALU.add), r=[pb, b_], w=[m_])
                P.dma("sp", self.modv[l:l + 1, :, s_, c0:c0 + 512], m_.ap, r=[m_])
            P.barrier()

    def load_modvec(self, st, l, kind):
        P = self.P
        mi, gi, add1 = {"A1": (1, 0, True), "B1": (0, None, False), "G2": (2, 1, False),
                        "A2": (4, 2, True), "B2": (3, None, False), "G4": (5, 3, False)}[kind]
        t = self.sb(st, [128, 2, D])
        for j in range(2):
            P.dma("sp", t[:, j, :], self.modv[l, j:j + 1, mi, :].broadcast_to([128, D]), w=[t])
        if gi is not None:
            with ExitStack() as s2:
                g = self.sb(s2, [128, D])
                P.dma("sp", g.ap, self.norm_g[l, gi:gi + 1, :].broadcast_to([128, D]), w=[g])
                for j in range(2):
                    if add1:
                        P.op("dve", lambda e: e.scalar_tensor_tensor(out=t[:, j, :], in0=t[:, j, :], scalar=1.0, in1=g.ap,
                                                                     op0=ALU.add, op1=ALU.mult), r=[t, g], w=[t])
                    else:
                        P.op("dve", lambda e: e.tensor_tensor(out=t[:, j, :], in0=t[:, j, :], in1=g.ap, op=ALU.mult), r=[t, g], w=[t])
                P.barrier()
        return t

    def rstd_of(self, src_ap, src_dep, n, junk, ss):
        P = self.P
        P.op("act", lambda e: e.activation(out=junk, in_=src_ap, func=AF.Square, accum_out=ss[:, 0:1]), r=src_dep, w=[ss])
        P.op("dve", lambda e: e.tensor_scalar(out=ss[:, 1:2], in0=ss[:, 0:1], scalar1=1.0 / n, scalar2=EPS, op0=ALU.mult, op1=ALU.add), r=[ss], w=[ss])
        P.op("act", lambda e: e.activation(out=ss[:, 2:3], in_=ss[:, 1:2], func=AF.Sqrt), r=[ss], w=[ss])
        P.op("dve", lambda e: e.reciprocal(out=ss[:, 3:4], in_=ss[:, 2:3]), r=[ss], w=[ss])

    def transpose_to(self, src, ncol, dst_fn, r_extra=()):
        P = self.P
        nblk = ncol // 128
        for q in range(0, nblk, 4):
            nb = min(4, nblk - q)
            pb = self.bank()
            for j in range(nb):
                P.op("pe", lambda e: e.matmul(pb[:, j * 128:(j + 1) * 128], lhsT=src[:, (q + j) * 128:(q + j + 1) * 128], rhs=self.idb.ap,
                                              start=True, stop=True), r=[src, self.idb], w=[pb])
            dst_fn(q, nb, pb)

    def norm_mod_to_hT(self, l, ia, ib):
        P = self.P
        with ExitStack() as st:
            A = self.load_modvec(st, l, ia)
            B = self.load_modvec(st, l, ib)
            xt = [self.sb(st, [128, D]) for _ in range(2)]
            tmp = self.sb(st, [128, D])
            hb = [self.sb(st, [128, D], BF16) for _ in range(2)]
            junk = self.sb(st, [128, D], BF16)
            ss = [self.sb(st, [128, 4]) for _ in range(2)]
            self.hT = self.sb(st, [128, KC, self.T], BF16, "hT")
            for i in range(self.NT):
                j = 1 if i < self.NCT else 0
                x_, h_, s_ = xt[i % 2], hb[i % 2], ss[i % 2]
                P.dma("sp", x_.ap, self.xres[i * 128:(i + 1) * 128, :], w=[x_])
                self.rstd_of(x_.ap, [x_], D, junk.ap, s_)
                P.op("dve", lambda e: e.scalar_tensor_tensor(out=tmp.ap, in0=x_.ap, scalar=s_[:, 3:4], in1=A[:, j, :], op0=ALU.mult, op1=ALU.mult),
                     r=[x_, s_, A], w=[tmp])
                P.op("pool", lambda e: e.tensor_tensor(out=h_.ap, in0=tmp.ap, in1=B[:, j, :], op=ALU.add), r=[tmp, B], w=[h_])

                def dst(q, nb, pb, i=i):
                    P.op("act", lambda e: e.copy(out=self.hT[:, q:q + nb, i * 128:(i + 1) * 128],
                                                 in_=pb[:, 0:nb * 128].rearrange("p (a t) -> p a t", a=nb)), r=[pb], w=[self.hT])
                self.transpose_to(h_, D, dst)
            P.dma("sp", self.HTd, self.hT.ap, r=[self.hT])
            P.barrier()

    def load_hT(self, st):
        self.hT = self.sb(st, [128, KC, self.T], BF16, "hT")
        self.P.dma("sp", self.hT.ap, self.HTd, w=[self.hT])

    def phase_norm1(self, l):
        self.norm_mod_to_hT(l, "A1", "B1")

    def wpipe(self, st, kc, ncol, nbuf=2):
        return {"stg": [self.sb(st, [128, kc, ncol]) for _ in range(nbuf)],
                "wb": [self.sb(st, [128, kc, ncol], BF16) for _ in range(nbuf)], "i": 0, "n": nbuf}

    def wload(self, wp, dram_ap, kc, ncol):
        P = self.P
        i = wp["i"] % wp["n"]
        wp["i"] += 1
        s_, b_ = wp["stg"][i], wp["wb"][i]
        P.dma("sp", s_[:, 0:kc, 0:ncol], dram_ap.rearrange("(k p) n -> p k n", p=128), w=[s_])
        P.op("pool", lambda e: e.tensor_copy(out=b_[:, 0:kc, 0:ncol], in_=s_[:, 0:kc, 0:ncol]), r=[s_], w=[b_])
        return b_

    def phase_win(self, l):
        P = self.P
        T = self.T
        groups = [(O_MLQ, 512, "F"), (O_MLK, 512, "FT"), (O_MLV, 512, "T"), (O_MLO, 512, "T"), (O_MLG, 16, "T"),
                  (O_HGQ, 512, "F"), (O_HGI, 512, "T"), (O_HGG, 512, "T"), (O_HGF, 1024, "FT"),
                  (O_CQ, 512, "T"), (O_CKV, 256, "T"), (O_KR, 64, "F"),
                  (O_SZ, 512, "T"), (O_XBC, 1024, "F"), (O_DT, 16, "T")]
        with ExitStack() as st:
            self.load_hT(st)
            wp = self.wpipe(st, KC, 256)
            ev = [self.sb(st, [128, 512]) for _ in range(4)]
            ei = 0
            chunks = []
            for (c0, n, mode) in groups:
                for cc in range(c0, c0 + n, 256):
                    chunks.append((cc, min(256, c0 + n - cc), mode))
            nxt = self.wload(wp, self.w_in[l, :, chunks[0][0]:chunks[0][0] + chunks[0][1]], KC, chunks[0][1])
            for ci, (cc, nn, mode) in enumerate(chunks):
                    wb = nxt
                    if ci + 1 < len(chunks):
                        c2, n2, _ = chunks[ci + 1]
                        nxt = self.wload(wp, self.w_in[l, :, c2:c2 + n2], KC, n2)
                    if "F" in mode:
                        for m0 in range(0, nn, 128):
                            mm = min(128, nn - m0)
                            for (t0, nt) in self.tok_blocks(0, T):
                                pb = self.bank()
                                for kc in range(KC):
                                    P.op("pe", lambda e: e.matmul(pb[0:mm, 0:nt], lhsT=wb[:, kc, m0:m0 + mm], rhs=self.hT[:, kc, t0:t0 + nt],
                                                                  start=(kc == 0), stop=(kc == KC - 1)), r=[wb, self.hT], w=[pb])
                                e_ = ev[ei % 4]
                                eng = "act" if ei % 2 == 0 else "dve"
                                ei += 1
                                if eng == "act":
                                    P.op("act", lambda e: e.copy(out=e_[0:mm, 0:nt], in_=pb[0:mm, 0:nt]), r=[pb], w=[e_])
                                else:
                                    P.op("dve", lambda e: e.tensor_copy(out=e_[0:mm, 0:nt], in_=pb[0:mm, 0:nt]), r=[pb], w=[e_])
                                P.dma("sp", self.UF[cc + m0:cc + m0 + mm, t0:t0 + nt], e_[0:mm, 0:nt], r=[e_])
                    if "T" in mode:
                        for i in range(self.NT):
                            pb = self.bank()
                            for kc in range(KC):
                                P.op("pe", lambda e: e.matmul(pb[:, 0:nn], lhsT=self.hT[:, kc, i * 128:(i + 1) * 128], rhs=wb[:, kc, 0:nn],
                                                              start=(kc == 0), stop=(kc == KC - 1)), r=[wb, self.hT], w=[pb])
                            e_ = ev[ei % 4]
                            eng = "act" if ei % 2 == 0 else "dve"
                            ei += 1
                            if eng == "act":
                                P.op("act", lambda e: e.copy(out=e_[:, 0:nn], in_=pb[:, 0:nn]), r=[pb], w=[e_])
                            else:
                                P.op("dve", lambda e: e.tensor_copy(out=e_[:, 0:nn], in_=pb[:, 0:nn]), r=[pb], w=[e_])
                            P.dma("sp", self.UT[i * 128:(i + 1) * 128, cc:cc + nn], e_[:, 0:nn], r=[e_])

    def order(self, d):
        c = list(range(self.NCT))
        la = list(range(self.NCT, self.NT))
        return c + la if d == 0 else c[::-1] + la[::-1]

    def decay_scalars(self, sc, lf_ap, lf_dep, n, d, pool=None):
        P = self.P
        tri = C_TRF if d == 0 else C_TRB
        pb = self.bank(pool)
        P.op("pe", lambda e: e.matmul(pb[:, 0:n], lhsT=self.cst[:, tri, :], rhs=lf_ap, start=True, stop=True), r=[self.cst] + lf_dep, w=[pb])
        P.op("pe", lambda e: e.matmul(pb[:, n:2 * n], lhsT=self.cst[:, C_ONE, :], rhs=lf_ap, start=True, stop=True), r=[self.cst] + lf_dep, w=[pb])
        P.op("dve", lambda e: e.tensor_copy(out=sc[:, 0:2 * n], in_=pb[:, 0:2 * n]), r=[pb], w=[sc])
        P.op("dve", lambda e: e.tensor_scalar(out=sc[:, 2 * n:3 * n], in0=sc[:, 0:n], scalar1=-1.0, scalar2=None, op0=ALU.mult), r=[sc], w=[sc])
        P.op("dve", lambda e: e.tensor_tensor(out=sc[:, 3 * n:4 * n], in0=sc[:, n:2 * n], in1=sc[:, 0:n], op=ALU.subtract), r=[sc], w=[sc])
        P.op("act", lambda e: e.activation(out=sc[:, 3 * n:4 * n], in_=sc[:, 3 * n:4 * n], func=AF.Exp), r=[sc], w=[sc])
        P.op("act", lambda e: e.activation(out=sc[:, 4 * n:6 * n], in_=sc[:, 0:2 * n], func=AF.Exp), r=[sc], w=[sc])

    def decay_matrix(self, lf_col, lf_dep, bias_col, bias_dep, d, lfB, Dm, pool=None):
        P = self.P
        tri = C_TRF if d == 0 else C_TRB
        mn = C_MNF if d == 0 else C_MNB
        P.op("dve", lambda e: e.tensor_copy(out=lfB.ap, in_=lf_col.broadcast_to([128, 128])), r=lf_dep, w=[lfB])
        pb = self.bank(pool)
        P.op("pe", lambda e: e.matmul(pb[:, 0:128], lhsT=lfB.ap, rhs=self.cst[:, tri, :], start=True, stop=False), r=[lfB, self.cst], w=[pb])
        P.op("pe", lambda e: e.matmul(pb[:, 0:128], lhsT=self.cst[:, C_ID, :], rhs=self.cst[:, mn, :], start=False, stop=True), r=[self.cst], w=[pb])
        P.op("act", lambda e: e.activation(out=Dm.ap, in_=pb[:, 0:128], func=AF.Exp, bias=bias_col, scale=1.0), r=[pb] + bias_dep, w=[Dm])

    def phase_ml(self, l):
        P = self.P
        with ExitStack() as st:
            bias = self.sb(st, [128, 8])
            P.dma("sp", bias.ap, self.ml_f_bias[l:l + 1, :].broadcast_to([128, 8]), w=[bias])
            ydir = [self.sb(st, [128, self.NT, 512]) for _ in range(2)]
            self.drive([self.ml_chain(st, l, d, bias, ydir[d]) for d in range(2)])
            ot = self.sb(st, [128, 512]); sg = self.sb(st, [128, 512])
            for i in range(self.NT):
                ts = slice(i * 128, (i + 1) * 128)
                P.dma("sp", ot.ap, self.UT[ts, O_MLO:O_MLO + 512], w=[ot])
                P.op("act", lambda e: e.activation(out=sg.ap, in_=ot.ap, func=AF.Sigmoid), r=[ot], w=[sg])
                P.op("dve", lambda e: e.tensor_tensor(out=ot.ap, in0=ydir[0][:, i, :], in1=ydir[1][:, i, :], op=ALU.add), r=[ydir[0], ydir[1]], w=[ot])
                P.op("dve", lambda e: e.tensor_tensor(out=sg.ap, in0=sg.ap, in1=ot.ap, op=ALU.mult), r=[sg, ot], w=[sg])
                P.dma("sp", self.BR[0, ts, :], sg.ap, r=[sg])

    def ml_chain(self, st, l, d, bias, yo):
        P = self.P
        pool = {"banks": list(range(4 * d, 4 * d + 4)), "rr": 0}
        Cs = self.sb(st, [128, 4, 132]); Cb = self.sb(st, [128, 4, 132], BF16)
        qT = self.sb(st, [128, 4, 128]); kT = self.sb(st, [128, 4, 128])
        qTb = self.sb(st, [128, 4, 128], BF16); kTb = self.sb(st, [128, 4, 128], BF16)
        kt = self.sb(st, [128, 512]); vt = self.sb(st, [128, 512])
        vaug = self.sb(st, [128, 4, 132], BF16)
        gt = self.sb(st, [128, 16]); xg = self.sb(st, [128, 4]); lf = self.sb(st, [128, 4]); ab = self.sb(st, [128, 8])
        sc = self.sb(st, [128, 24])
        lfB = self.sb(st, [128, 128]); Dm = self.sb(st, [128, 128]); PT = self.sb(st, [128, 128], BF16)
        tmpI = self.sb(st, [128, 132]); tot = self.sb(st, [128, 132]); dn = self.sb(st, [128, 2])
        kw = self.sb(st, [128, 128], BF16)
        P.op("pool", lambda e: e.memset(vaug.ap, 1.0), w=[vaug])
        P.op("dve", lambda e: e.memset(Cs.ap, 0.0), w=[Cs])
        P.op("pool", lambda e: e.memset(Cb.ap, 0.0), w=[Cb])
        yield
        for i in self.order(d):
            ts = slice(i * 128, (i + 1) * 128)
            P.dma("sp", qT.ap, self.UF[O_MLQ:O_MLQ + 512, ts].rearrange("(h p) t -> p h t", p=128), w=[qT])
            P.dma("sp", kT.ap, self.UF[O_MLK:O_MLK + 512, ts].rearrange("(h p) t -> p h t", p=128), w=[kT])
            P.dma("sp", kt.ap, self.UT[ts, O_MLK:O_MLK + 512], w=[kt])
            P.dma("sp", vt.ap, self.UT[ts, O_MLV:O_MLV + 512], w=[vt])
            P.dma("sp", gt.ap, self.UT[ts, O_MLG:O_MLG + 16], w=[gt])
            yield
            P.op("act", lambda e: e.mul(out=qTb.ap, in_=qT.ap, mul=128.0 ** -0.5), r=[qT], w=[qTb])
            P.op("pool", lambda e: e.tensor_copy(out=kTb.ap, in_=kT.ap), r=[kT], w=[kTb])
            P.op("pool", lambda e: e.tensor_copy(out=vaug[:, :, 0:128], in_=vt.ap.rearrange("p (h d) -> p h d", h=4)), r=[vt], w=[vaug])
            yield
            P.op("dve", lambda e: e.tensor_tensor(out=xg.ap, in0=gt[:, d * 8 + 4:d * 8 + 8], in1=bias[:, d * 4:d * 4 + 4], op=ALU.add), r=[gt, bias], w=[xg])
            yield
            P.op("act", lambda e: e.activation(out=xg.ap, in_=xg.ap, func=AF.Exp, scale=-1.0), r=[xg], w=[xg])
            yield
            P.op("act", lambda e: e.activation(out=xg.ap, in_=xg.ap, func=AF.Ln, bias=1.0), r=[xg], w=[xg])
            yield
            P.op("dve", lambda e: e.tensor_scalar(out=lf.ap, in0=xg.ap, scalar1=-1.0, scalar2=None, op0=ALU.mult), r=[xg], w=[lf])
            yield
            self.decay_scalars(sc, lf.ap, [lf], 4, d, pool)
            yield
            P.op("dve", lambda e: e.tensor_tensor(out=ab[:, 0:4], in0=gt[:, d * 8:d * 8 + 4], in1=sc[:, 0:4], op=ALU.subtract), r=[gt, sc], w=[ab])
            P.op("dve", lambda e: e.tensor_tensor(out=ab[:, 4:8], in0=ab[:, 0:4], in1=sc[:, 4:8], op=ALU.add), r=[ab, sc], w=[ab])
            yield
            P.op("act", lambda e: e.activation(out=ab[:, 4:8], in_=ab[:, 4:8], func=AF.Exp), r=[ab], w=[ab])
            yield
            for h in range(4):
                hs = slice(h * 128, (h + 1) * 128)
                pS = self.bank(pool)
                P.op("pe", lambda e: e.matmul(pS[:, 0:128], lhsT=kTb[:, h, :], rhs=qTb[:, h, :], start=True, stop=True), r=[kTb, qTb], w=[pS])
                yield
                self.decay_matrix(lf[:, h:h + 1], [lf], ab[:, h:h + 1], [ab], d, lfB, Dm, pool)
                yield
                P.op("dve", lambda e: e.tensor_tensor(out=PT.ap, in0=pS[:, 0:128], in1=Dm.ap, op=ALU.mult), r=[pS, Dm], w=[PT])
                yield
                pO = self.bank(pool)
                P.op("pe", lambda e: e.matmul(pO[:, 0:129], lhsT=PT.ap, rhs=vaug[:, h, 0:129], start=True, stop=True), r=[PT, vaug], w=[pO])
                pI = self.bank(pool)
                P.op("pe", lambda e: e.matmul(pI[:, 0:129], lhsT=qTb[:, h, :], rhs=Cb[:, h, 0:129], start=True, stop=True), r=[qTb, Cb], w=[pI])
                yield
                P.op("dve", lambda e: e.tensor_scalar(out=tmpI[:, 0:129], in0=pI[:, 0:129], scalar1=sc[:, 16 + h:17 + h], scalar2=None, op0=ALU.mult), r=[pI, sc], w=[tmpI])
                yield
                P.op("dve", lambda e: e.tensor_tensor(out=tot[:, 0:129], in0=pO[:, 0:129], in1=tmpI[:, 0:129], op=ALU.add), r=[pO, tmpI], w=[tot])
                yield
                P.op("act", lambda e: e.activation(out=dn[:, 0:1], in_=tot[:, 128:129], func=AF.Abs), r=[tot], w=[dn])
                yield
                P.op("dve", lambda e: e.tensor_scalar(out=dn[:, 0:1], in0=dn[:, 0:1], scalar1=1.0, scalar2=None, op0=ALU.max), r=[dn], w=[dn])
                yield
                P.op("dve", lambda e: e.reciprocal(out=dn[:, 1:2], in_=dn[:, 0:1]), r=[dn], w=[dn])
                yield
                P.op("dve", lambda e: e.tensor_scalar(out=yo[:, i, hs], in0=tot[:, 0:128], scalar1=dn[:, 1:2], scalar2=None, op0=ALU.mult), r=[tot, dn], w=[yo])
                yield
                P.op("pool", lambda e: e.tensor_scalar(out=kw.ap, in0=kt[:, hs], scalar1=ab[:, 4 + h:5 + h], scalar2=None, op0=ALU.mult), r=[kt, ab], w=[kw])
                yield
                pU = self.bank(pool)
                P.op("pe", lambda e: e.matmul(pU[:, 0:129], lhsT=kw.ap, rhs=vaug[:, h, 0:129], start=True, stop=True), r=[kw, vaug], w=[pU])
                yield
                P.op("dve", lambda e: e.scalar_tensor_tensor(out=Cs[:, h, 0:129], in0=Cs[:, h, 0:129], scalar=sc[:, 20 + h:21 + h], in1=pU[:, 0:129], op0=ALU.mult, op1=ALU.add),
                     r=[Cs, sc, pU], w=[Cs])
                yield
                P.op("act", lambda e: e.copy(out=Cb[:, h, 0:129], in_=Cs[:, h, 0:129]), r=[Cs], w=[Cb])
                yield

    def phase_ssd(self, l):
        P = self.P
        T, NCX = self.T, self.n_ctx
        with ExitStack() as st:
            xbcT = self.sb(st, [128, 8, T], BF16)
            cw = self.sb(st, [128, 8, 3]); cb = self.sb(st, [128, 8])
            dtb = self.sb(st, [128, 16]); aneg = self.sb(st, [128, 16]); dsk = self.sb(st, [128, 8]); ng = self.sb(st, [128, 512])
            P.dma("sp", cw.ap, self.ssd_conv_w[l], w=[cw])
            P.dma("sp", cb.ap, self.ssd_conv_b[l], w=[cb])
            P.dma("sp", dtb.ap, self.ssd_dt_bias[l:l + 1, :].broadcast_to([128, 16]), w=[dtb])
            P.dma("sp", aneg.ap, self.ssd_a_log[l:l + 1, :].broadcast_to([128, 16]), w=[aneg])
            P.dma("sp", dsk.ap, self.ssd_d[l:l + 1, :].broadcast_to([128, 8]), w=[dsk])
            P.dma("sp", ng.ap, self.ssd_norm_g[l:l + 1, :].broadcast_to([128, 512]), w=[ng])
            P.op("act", lambda e: e.activation(out=aneg.ap, in_=aneg.ap, func=AF.Exp), r=[aneg], w=[aneg])
            P.op("dve", lambda e: e.tensor_scalar(out=aneg.ap, in0=aneg.ap, scalar1=-1.0, scalar2=None, op0=ALU.mult), r=[aneg], w=[aneg])
            with ExitStack() as s2:
                xin = [self.sb(s2, [128, T]) for _ in range(2)]
                acc = self.sb(s2, [128, T])
                for g in range(8):
                    x_ = xin[g % 2]
                    P.dma("sp", x_.ap, self.UF[O_XBC + g * 128:O_XBC + (g + 1) * 128, :], w=[x_])
                    P.op("dve", lambda e: e.tensor_scalar(out=acc.ap, in0=x_.ap, scalar1=cw[:, g, 1:2], scalar2=None, op0=ALU.mult), r=[x_, cw], w=[acc])
                    for (s0, s1) in ((0, NCX), (NCX, T)):
                        P.op("dve", lambda e: e.scalar_tensor_tensor(out=acc[:, s0 + 1:s1], in0=x_[:, s0:s1 - 1], scalar=cw[:, g, 0:1], in1=acc[:, s0 + 1:s1],
                                                                     op0=ALU.mult, op1=ALU.add), r=[x_, cw, acc], w=[acc])
                        P.op("dve", lambda e: e.scalar_tensor_tensor(out=acc[:, s0:s1 - 1], in0=x_[:, s0 + 1:s1], scalar=cw[:, g, 2:3], in1=acc[:, s0:s1 - 1],
                                                                     op0=ALU.mult, op1=ALU.add), r=[x_, cw, acc], w=[acc])
                    P.op("act", lambda e: e.activation(out=xbcT[:, g, :], in_=acc.ap, func=AF.Silu, bias=cb[:, g:g + 1], scale=1.0), r=[acc, cb], w=[xbcT])
                P.barrier()
            ydir = [self.sb(st, [128, self.NT, 512]) for _ in range(2)]
            self.drive([self.ssd_chain(st, l, d, xbcT, dtb, aneg, dsk, ydir[d]) for d in range(2)])
            yd = self.sb(st, [128, 512]); zt = self.sb(st, [128, 512]); junk = self.sb(st, [128, 512], BF16); ss = self.sb(st, [128, 4])
            for i in range(self.NT):
                ts = slice(i * 128, (i + 1) * 128)
                P.dma("sp", zt.ap, self.UT[ts, O_SZ:O_SZ + 512], w=[zt])
                P.op("dve", lambda e: e.tensor_tensor(out=yd.ap, in0=ydir[0][:, i, :], in1=ydir[1][:, i, :], op=ALU.add), r=[ydir[0], ydir[1]], w=[yd])
                P.op("act", lambda e: e.activation(out=zt.ap, in_=zt.ap, func=AF.Silu), r=[zt], w=[zt])
                P.op("dve", lambda e: e.tensor_tensor(out=yd.ap, in0=yd.ap, in1=zt.ap, op=ALU.mult), r=[yd, zt], w=[yd])
                self.rstd_of(yd.ap, [yd], 512, junk.ap, ss)
                P.op("dve", lambda e: e.scalar_tensor_tensor(out=yd.ap, in0=yd.ap, scalar=ss[:, 3:4], in1=ng.ap, op0=ALU.mult, op1=ALU.mult), r=[yd, ss, ng], w=[yd])
                P.dma("sp", self.BR[3, ts, :], yd.ap, r=[yd])

    def ssd_chain(self, st, l, d, xbcT, dtb, aneg, dsk, yo):
        P = self.P
        pS, pO = self.ps[4 * d], self.ps[4 * d + 1]
        pool = {"banks": [4 * d + 2, 4 * d + 3], "rr": 0}
        Hs = self.sb(st, [128, 2, 256]); Hb = self.sb(st, [128, 2, 256], BF16)
        xtok = self.sb(st, [128, 512]); Btok = self.sb(st, [128, 256], BF16)
        xdt = self.sb(st, [128, 8, 64], BF16); xdtw = self.sb(st, [128, 8, 64], BF16)
        gt = self.sb(st, [128, 8]); dt = self.sb(st, [128, 8]); da = self.sb(st, [128, 8])
        sc = self.sb(st, [128, 48])
        lfB = self.sb(st, [128, 128]); Dm = self.sb(st, [128, 128]); PT = self.sb(st, [128, 128], BF16)
        tmpI = self.sb(st, [128, 512]); tmpD = self.sb(st, [128, 512])
        P.op("dve", lambda e: e.memset(Hs.ap, 0.0), w=[Hs])
        P.op("pool", lambda e: e.memset(Hb.ap, 0.0), w=[Hb])
        yield
        for i in self.order(d):
            ts = slice(i * 128, (i + 1) * 128)
            pb = self.bank(pool)
            for g in range(4):
                P.op("pe", lambda e: e.matmul(pb[:, g * 128:(g + 1) * 128], lhsT=xbcT[:, g, ts], rhs=self.idb.ap, start=True, stop=True), r=[xbcT, self.idb], w=[pb])
            P.op("act", lambda e: e.copy(out=xtok.ap, in_=pb[:, :]), r=[pb], w=[xtok])
            yield
            pb = self.bank(pool)
            for g in range(2):
                P.op("pe", lambda e: e.matmul(pb[:, g * 128:(g + 1) * 128], lhsT=xbcT[:, 4 + g, ts], rhs=self.idb.ap, start=True, stop=True), r=[xbcT, self.idb], w=[pb])
            P.op("act", lambda e: e.copy(out=Btok.ap, in_=pb[:, 0:256]), r=[pb], w=[Btok])
            yield
            P.dma("sp", gt.ap, self.UT[ts, O_DT + d * 8:O_DT + d * 8 + 8], w=[gt])
            P.op("dve", lambda e: e.tensor_tensor(out=dt.ap, in0=gt.ap, in1=dtb[:, d * 8:d * 8 + 8], op=ALU.add), r=[gt, dtb], w=[dt])
            yield
            P.op("act", lambda e: e.activation(out=dt.ap, in_=dt.ap, func=AF.Exp), r=[dt], w=[dt])
            yield
            P.op("act", lambda e: e.activation(out=dt.ap, in_=dt.ap, func=AF.Ln, bias=1.0), r=[dt], w=[dt])
            yield
            P.op("dve", lambda e: e.tensor_tensor(out=da.ap, in0=dt.ap, in1=aneg[:, d * 8:d * 8 + 8], op=ALU.mult), r=[dt, aneg], w=[da])
            yield
            self.decay_scalars(sc, da.ap, [da], 8, d, pool)
            yield
            P.op("dve", lambda e: e.tensor_tensor(out=xdt.ap, in0=xtok.ap.rearrange("p (h c) -> p h c", h=8), in1=dt.ap.unsqueeze(2).broadcast_to([128, 8, 64]), op=ALU.mult),
                 r=[xtok, dt], w=[xdt])
            yield
            P.op("pool", lambda e: e.tensor_tensor(out=xdtw.ap, in0=xdt.ap, in1=sc[:, 24:32].unsqueeze(2).broadcast_to([128, 8, 64]), op=ALU.mult), r=[xdt, sc], w=[xdtw])
            pI = self.bank(pool)
            for g in range(2):
                P.op("pe", lambda e: e.matmul(pS[:, g * 128:(g + 1) * 128], lhsT=xbcT[:, 4 + g, ts], rhs=xbcT[:, 6 + g, ts], start=True, stop=True), r=[xbcT], w=[pS])
                P.op("pe", lambda e: e.matmul(pI[:, g * 256:(g + 1) * 256], lhsT=xbcT[:, 6 + g, ts], rhs=Hb[:, g, :], start=True, stop=True), r=[xbcT, Hb], w=[pI])
            yield
            P.op("dve", lambda e: e.tensor_tensor(out=tmpI.ap.rearrange("p (h c) -> p h c", h=8), in0=pI[:, :].rearrange("p (h c) -> p h c", h=8),
                                                  in1=sc[:, 32:40].unsqueeze(2).broadcast_to([128, 8, 64]), op=ALU.mult), r=[pI, sc], w=[tmpI])
            yield
            if d == 0:
                P.op("dve", lambda e: e.tensor_tensor(out=tmpD.ap.rearrange("p (h c) -> p h c", h=8), in0=xtok.ap.rearrange("p (h c) -> p h c", h=8),
                                                      in1=dsk.ap.unsqueeze(2).broadcast_to([128, 8, 64]), op=ALU.mult), r=[xtok, dsk], w=[tmpD])
                yield
                P.op("dve", lambda e: e.tensor_tensor(out=tmpI.ap, in0=tmpI.ap, in1=tmpD.ap, op=ALU.add), r=[tmpI, tmpD], w=[tmpI])
                yield
            for j in range(8):
                g = j // 4
                self.decay_matrix(da[:, j:j + 1], [da], sc[:, 16 + j:17 + j], [sc], d, lfB, Dm, pool)
                yield
                P.op("dve", lambda e: e.tensor_tensor(out=PT.ap, in0=pS[:, g * 128:(g + 1) * 128], in1=Dm.ap, op=ALU.mult), r=[pS, Dm], w=[PT])
                yield
                P.op("pe", lambda e: e.matmul(pO[:, j * 64:(j + 1) * 64], lhsT=PT.ap, rhs=xdt[:, j, :], start=True, stop=True), r=[PT, xdt], w=[pO])
                yield
            P.op("dve", lambda e: e.tensor_tensor(out=yo[:, i, :], in0=pO[:, :], in1=tmpI.ap, op=ALU.add), r=[pO, tmpI], w=[yo])
            yield
            pU = self.bank(pool)
            for g in range(2):
                P.op("pe", lambda e: e.matmul(pU[:, g * 256:(g + 1) * 256], lhsT=Btok[:, g * 128:(g + 1) * 128], rhs=xdtw[:, g * 4:(g + 1) * 4, :].rearrange("p h c -> p (h c)"),
                                              start=True, stop=True), r=[Btok, xdtw], w=[pU])
            P.op("dve", lambda e: e.tensor_tensor(out=Hs.ap.rearrange("p g (h c) -> p (g h) c", h=4), in0=Hs.ap.rearrange("p g (h c) -> p (g h) c", h=4),
                                                  in1=sc[:, 40:48].unsqueeze(2).broadcast_to([128, 8, 64]), op=ALU.mult), r=[Hs, sc], w=[Hs])
            yield
            P.op("dve", lambda e: e.tensor_tensor(out=Hs.ap.rearrange("p g c -> p (g c)"), in0=Hs.ap.rearrange("p g c -> p (g c)"), in1=pU[:, :], op=ALU.add), r=[Hs, pU], w=[Hs])
            yield
            P.op("act", lambda e: e.copy(out=Hb.ap, in_=Hs.ap), r=[Hs], w=[Hb])
            yield

    def phase_hg(self, l):
        P = self.P
        L = self.depth
        with ExitStack() as st:
            lg = self.sb(st, [128, L + 1, 4]); lbT = self.sb(st, [128, 4]); omT = self.sb(st, [128, 4]); smT = self.sb(st, [128, 4])
            omB = self.sb(st, [128, 512]); ngB = self.sb(st, [128, 512])
            P.dma("sp", lg.ap, self.hg_lbT, w=[lg])
            P.dma("sp", ngB.ap, self.hg_norm_g[l:l + 1, :].broadcast_to([128, 512]), w=[ngB])
            P.op("act", lambda e: e.activation(out=lg.ap, in_=lg.ap, func=AF.Exp), r=[lg], w=[lg])
            P.op("dve", lambda e: e.tensor_tensor(out=smT.ap, in0=lg[:, 0, :], in1=lg[:, 1, :], op=ALU.add), r=[lg], w=[smT])
            for j in range(2, L + 1):
                P.op("dve", lambda e: e.tensor_tensor(out=smT.ap, in0=smT.ap, in1=lg[:, j, :], op=ALU.add), r=[lg, smT], w=[smT])
            P.op("dve", lambda e: e.reciprocal(out=smT.ap, in_=smT.ap), r=[smT], w=[smT])
            P.op("dve", lambda e: e.tensor_copy(out=lbT.ap, in_=lg[:, 0, :]), r=[lg], w=[lbT])
            for j in range(1, l + 1):
                P.op("dve", lambda e: e.tensor_tensor(out=lbT.ap, in0=lbT.ap, in1=lg[:, j, :], op=ALU.add), r=[lg, lbT], w=[lbT])
            P.op("dve", lambda e: e.tensor_tensor(out=lbT.ap, in0=lbT.ap, in1=smT.ap, op=ALU.mult), r=[lbT, smT], w=[lbT])
            P.op("dve", lambda e: e.tensor_scalar(out=omT.ap, in0=lbT.ap, scalar1=-1.0, scalar2=1.0, op0=ALU.mult, op1=ALU.add), r=[lbT], w=[omT])
            P.op("dve", lambda e: e.tensor_scalar(out=omB.ap, in0=self.lbB[:, l, :], scalar1=-1.0, scalar2=1.0, op0=ALU.mult, op1=ALU.add), r=[self.lbB], w=[omB])
            ydir = [self.sb(st, [128, self.NT, 512]) for _ in range(2)]
            self.drive([self.hg_chain(st, l, d, omT, omB, ydir[d]) for d in range(2)])
            ys = self.sb(st, [128, 512]); sq = self.sb(st, [128, 512]); rs = self.sb(st, [128, 8]); gtk = self.sb(st, [128, 512])
            for i in range(self.NT):
                ts = slice(i * 128, (i + 1) * 128)
                P.dma("sp", gtk.ap, self.UT[ts, O_HGG:O_HGG + 512], w=[gtk])
                P.op("dve", lambda e: e.tensor_tensor(out=ys.ap, in0=ydir[0][:, i, :], in1=ydir[1][:, i, :], op=ALU.add), r=[ydir[0], ydir[1]], w=[ys])
                P.op("dve", lambda e: e.tensor_tensor(out=sq.ap, in0=ys.ap, in1=ys.ap, op=ALU.mult), r=[ys], w=[sq])
                P.op("dve", lambda e: e.tensor_reduce(out=rs[:, 0:4], in_=sq.ap.rearrange("p (h c) -> p h c", h=4), axis=AX.X, op=ALU.add), r=[sq], w=[rs])
                P.op("dve", lambda e: e.tensor_scalar(out=rs[:, 0:4], in0=rs[:, 0:4], scalar1=1.0 / 128, scalar2=EPS, op0=ALU.mult, op1=ALU.add), r=[rs], w=[rs])
                P.op("act", lambda e: e.activation(out=rs[:, 0:4], in_=rs[:, 0:4], func=AF.Sqrt), r=[rs], w=[rs])
                P.op("dve", lambda e: e.reciprocal(out=rs[:, 4:8], in_=rs[:, 0:4]), r=[rs], w=[rs])
                P.op("dve", lambda e: e.tensor_tensor(out=ys.ap.rearrange("p (h c) -> p h c", h=4), in0=ys.ap.rearrange("p (h c) -> p h c", h=4),
                                                      in1=rs[:, 4:8].unsqueeze(2).broadcast_to([128, 4, 128]), op=ALU.mult), r=[ys, rs], w=[ys])
                P.op("dve", lambda e: e.tensor_tensor(out=ys.ap, in0=ys.ap, in1=ngB.ap, op=ALU.mult), r=[ys, ngB], w=[ys])
                P.op("act", lambda e: e.activation(out=gtk.ap, in_=gtk.ap, func=AF.Sigmoid), r=[gtk], w=[gtk])
                P.op("dve", lambda e: e.tensor_tensor(out=ys.ap, in0=ys.ap, in1=gtk.ap, op=ALU.mult), r=[ys, gtk], w=[ys])
                P.dma("sp", self.BR[1, ts, :], ys.ap, r=[ys])

    def hg_chain(self, st, l, d, omT, omB, yo):
        P = self.P
        pool = {"banks": list(range(4 * d, 4 * d + 4)), "rr": 0}
        Ss = self.sb(st, [128, 4, 128]); Sb = self.sb(st, [128, 4, 128], BF16)
        qT = self.sb(st, [128, 4, 128]); fT = self.sb(st, [128, 4, 128]); kTf = self.sb(st, [128, 4, 128])
        ft = self.sb(st, [128, 512]); lf = self.sb(st, [128, 512]); vt = self.sb(st, [128, 512]); vb = self.sb(st, [128, 512], BF16)
        E = self.sb(st, [128, 128]); Ei = self.sb(st, [128, 128]); ec = self.sb(st, [128, 4])
        qtl = self.sb(st, [128, 128], BF16); ktl = self.sb(st, [128, 128], BF16); ktok = self.sb(st, [128, 128], BF16)
        Af = self.sb(st, [128, 128]); AT = self.sb(st, [128, 128], BF16)
        lc = C_LCF if d == 0 else C_LCB
        tri = C_TRF if d == 0 else C_TRB
        hc0 = 0 if d == 0 else 3
        P.op("dve", lambda e: e.memset(Ss.ap, 0.0), w=[Ss])
        yield
        for i in self.order(d):
            ts = slice(i * 128, (i + 1) * 128)
            P.dma("sp", qT.ap, self.UF[O_HGQ:O_HGQ + 512, ts].rearrange("(h p) t -> p h t", p=128), w=[qT])
            P.dma("sp", fT.ap, self.UF[O_HGF + d * 512:O_HGF + (d + 1) * 512, ts].rearrange("(h p) t -> p h t", p=128), w=[fT])
            P.dma("sp", ft.ap, self.UT[ts, O_HGF + d * 512:O_HGF + (d + 1) * 512], w=[ft])
            P.dma("sp", vt.ap, self.UT[ts, O_HGI:O_HGI + 512], w=[vt])
            yield
            P.op("act", lambda e: e.activation(out=qT.ap, in_=qT.ap, func=AF.Silu), r=[qT], w=[qT])
            yield
            P.op("act", lambda e: e.activation(out=kTf.ap, in_=fT.ap, func=AF.Sigmoid, scale=-1.0), r=[fT], w=[kTf])
            yield
            for h in range(4):
                P.op("pool", lambda e: e.tensor_scalar(out=kTf[:, h, :], in0=kTf[:, h, :], scalar1=omT[:, h:h + 1], scalar2=None, op0=ALU.mult), r=[kTf, omT], w=[kTf])
            yield
            P.op("act", lambda e: e.activation(out=ft.ap, in_=ft.ap, func=AF.Sigmoid), r=[ft], w=[ft])
            yield
            P.op("dve", lambda e: e.tensor_tensor(out=ft.ap, in0=ft.ap, in1=omB.ap, op=ALU.mult), r=[ft, omB], w=[ft])
            yield
            P.op("dve", lambda e: e.tensor_tensor(out=ft.ap, in0=ft.ap, in1=self.lbB[:, l, :], op=ALU.add), r=[ft, self.lbB], w=[ft])
            yield
            P.op("act", lambda e: e.activation(out=lf.ap, in_=ft.ap, func=AF.Ln), r=[ft], w=[lf])
            P.op("pool", lambda e: e.tensor_copy(out=vb.ap, in_=vt.ap), r=[vt], w=[vb])
            yield
            for h in range(4):
                hs = slice(h * 128, (h + 1) * 128)
                pG = self.bank(pool)
                P.op("pe", lambda e: e.matmul(pG[:, 0:128], lhsT=lf[:, hs], rhs=self.cst[:, lc, :], start=True, stop=True), r=[lf, self.cst], w=[pG])
                P.op("pe", lambda e: e.matmul(pG[:, 128:132], lhsT=lf[:, hs], rhs=self.cst[:, C_HC, hc0:hc0 + 4], start=True, stop=True), r=[lf, self.cst], w=[pG])
                yield
                P.op("act", lambda e: e.activation(out=E.ap, in_=pG[:, 0:128], func=AF.Exp), r=[pG], w=[E])
                yield
                P.op("act", lambda e: e.activation(out=Ei.ap, in_=pG[:, 0:128], func=AF.Exp, scale=-1.0), r=[pG], w=[Ei])
                yield
                P.op("act", lambda e: e.activation(out=ec.ap, in_=pG[:, 128:132], func=AF.Exp), r=[pG], w=[ec])
                yield
                P.op("dve", lambda e: e.tensor_tensor(out=qtl.ap, in0=qT[:, h, :], in1=E.ap, op=ALU.mult), r=[qT, E], w=[qtl])
                yield
                P.op("dve", lambda e: e.tensor_tensor(out=ktl.ap, in0=kTf[:, h, :], in1=Ei.ap, op=ALU.mult), r=[kTf, Ei], w=[ktl])
                yield
                P.op("dve", lambda e: e.tensor_scalar(out=Sb[:, h, :], in0=Ss[:, h, :], scalar1=ec[:, 0:1], scalar2=None, op0=ALU.mult), r=[Ss, ec], w=[Sb])
                yield
                pA = self.bank(pool)
                P.op("pe", lambda e: e.matmul(pA[:, 0:128], lhsT=ktl.ap, rhs=qtl.ap, start=True, stop=True), r=[ktl, qtl], w=[pA])
                pK = self.bank(pool)
                P.op("pe", lambda e: e.matmul(pK[:, 0:128], lhsT=ktl.ap, rhs=self.idb.ap, start=True, stop=True), r=[ktl, self.idb], w=[pK])
                yield
                P.op("dve", lambda e: e.tensor_scalar(out=Af.ap, in0=pA[:, 0:128], scalar1=-1e30, scalar2=1e30, op0=ALU.max, op1=ALU.min), r=[pA], w=[Af])
                P.op("act", lambda e: e.copy(out=ktok.ap, in_=pK[:, 0:128]), r=[pK], w=[ktok])
                yield
                P.op("dve", lambda e: e.tensor_tensor(out=AT.ap, in0=Af.ap, in1=self.cst[:, tri, :], op=ALU.mult), r=[Af, self.cst], w=[AT])
                yield
                pO = self.bank(pool)
                P.op("pe", lambda e: e.matmul(pO[:, 0:128], lhsT=AT.ap, rhs=vb[:, hs], start=True, stop=False), r=[AT, vb], w=[pO])
                P.op("pe", lambda e: e.matmul(pO[:, 0:128], lhsT=qtl.ap, rhs=Sb[:, h, :], start=False, stop=True), r=[qtl, Sb], w=[pO])
                yield
                P.op("act", lambda e: e.copy(out=yo[:, i, hs], in_=pO[:, 0:128]), r=[pO], w=[yo])
                yield
                pU = self.bank(pool)
                P.op("pe", lambda e: e.matmul(pU[:, 0:128], lhsT=ktok.ap, rhs=vb[:, hs], start=True, stop=True), r=[ktok, vb], w=[pU])
                yield
                P.op("dve", lambda e: e.tensor_scalar(out=Ss[:, h, :], in0=Ss[:, h, :], scalar1=ec[:, 1:2], scalar2=None, op0=ALU.mult), r=[Ss, ec], w=[Ss])
                yield
                P.op("dve", lambda e: e.scalar_tensor_tensor(out=Ss[:, h, :], in0=pU[:, 0:128], scalar=ec[:, 2:3], in1=Ss[:, h, :], op0=ALU.mult, op1=ALU.add), r=[pU, ec, Ss], w=[Ss])
                yield

    def phase_mla(self, l):
        P = self.P
        T, NT, NCX = self.T, self.NT, self.n_ctx
        with ExitStack() as st:
            qnT = self.sb(st, [128, 4, T], BF16); qrT = self.sb(st, [128, 4, T], BF16)
            knT = self.sb(st, [128, 4, T], BF16); krT = self.sb(st, [128, T], BF16)
            vaug = self.sb(st, [128, NT, 4, 132], BF16)
            P.op("pool", lambda e: e.memset(vaug.ap, 1.0), w=[vaug])
            with ExitStack() as s2:
                gq = self.sb(s2, [128, 512]); gkv = self.sb(s2, [128, 256])
                P.dma("sp", gq.ap, self.mla_g_q[l:l + 1, :].broadcast_to([128, 512]), w=[gq])
                P.dma("sp", gkv.ap, self.mla_g_kv[l:l + 1, :].broadcast_to([128, 256]), w=[gkv])
                cqT = self.sb(s2, [128, 4, T], BF16); ckvT = self.sb(s2, [128, 2, T], BF16)
                cosT = self.sb(s2, [128, T]); sinT = self.sb(s2, [128, T])
                P.op("dve", lambda e: e.memset(cosT.ap, 0.0), w=[cosT])
                P.op("dve", lambda e: e.memset(sinT.ap, 0.0), w=[sinT])
                P.dma("sp", cosT[0:64, :], self.rope_in[0:64, :], w=[cosT])
                P.dma("sp", sinT[0:64, :], self.rope_in[64:128, :], w=[sinT])
                permb = self.sb(s2, [128, 128], BF16)
                P.op("dve", lambda e: e.tensor_copy(out=permb.ap, in_=self.cst[:, C_PERM, :]), r=[self.cst], w=[permb])
                wst = self.sb(s2, [128, 4, 768]); Wuq = self.sb(s2, [128, 4, 768], BF16)
                wst2 = self.sb(s2, [128, 2, 1024]); Wukv = self.sb(s2, [128, 2, 1024], BF16)
                Wr = self.sb(s2, [128, 4, 4, 128], BF16)
                P.dma("sp", wst.ap, self.mla_w_uq[l].rearrange("(k p) n -> p k n", p=128), w=[wst])
                P.dma("sp", wst2.ap, self.mla_w_ukv[l].rearrange("(k p) n -> p k n", p=128), w=[wst2])
                P.op("dve", lambda e: e.tensor_copy(out=Wuq.ap, in_=wst.ap), r=[wst], w=[Wuq])
                P.op("dve", lambda e: e.tensor_copy(out=Wukv.ap, in_=wst2.ap), r=[wst2], w=[Wukv])
                P.op("dve", lambda e: e.memset(Wr.ap, 0.0), w=[Wr])
                for h in range(4):
                    for kc in range(4):
                        P.op("dve", lambda e: e.tensor_copy(out=Wr[:, kc, h, 0:64], in_=wst[:, kc, h * 192 + 128:(h + 1) * 192]), r=[wst], w=[Wr])
                ct = self.sb(s2, [128, 768]); cb_ = self.sb(s2, [128, 768], BF16); junk = self.sb(s2, [128, 512], BF16)
                ss = self.sb(s2, [128, 4]); ss2 = self.sb(s2, [128, 4])
                for i in range(NT):
                    ts = slice(i * 128, (i + 1) * 128)
                    P.dma("sp", ct.ap, self.UT[ts, O_CQ:O_CQ + 768], w=[ct])
                    self.rstd_of(ct[:, 0:512], [ct], 512, junk.ap, ss)
                    self.rstd_of(ct[:, 512:768], [ct], 256, junk[:, 0:256], ss2)
                    P.op("dve", lambda e: e.scalar_tensor_tensor(out=cb_[:, 0:512], in0=ct[:, 0:512], scalar=ss[:, 3:4], in1=gq.ap, op0=ALU.mult, op1=ALU.mult), r=[ct, ss, gq], w=[cb_])
                    P.op("dve", lambda e: e.scalar_tensor_tensor(out=cb_[:, 512:768], in0=ct[:, 512:768], scalar=ss2[:, 3:4], in1=gkv.ap, op0=ALU.mult, op1=ALU.mult), r=[ct, ss2, gkv], w=[cb_])

                    def dst(q, nb, pb, i=i):
                        for j in range(nb):
                            blk = q + j
                            tgt = cqT[:, blk, i * 128:(i + 1) * 128] if blk < 4 else ckvT[:, blk - 4, i * 128:(i + 1) * 128]
                            P.op("act", lambda e: e.copy(out=tgt, in_=pb[:, j * 128:(j + 1) * 128]), r=[pb], w=[cqT if blk < 4 else ckvT])
                    self.transpose_to(cb_, 768, dst)
                if os.environ.get("MLA_STOP") == "A":
                    return
                xb = self.sb(s2, [128, 512], BF16); t1 = self.sb(s2, [128, 512]); t2 = self.sb(s2, [128, 512])
                xk = self.sb(s2, [128, T])

                def rope(src_ap, src_dep, out_ap, out_tl, t0, n):
                    P.op("act", lambda e: e.copy(out=xb[:, 0:n], in_=src_ap), r=src_dep, w=[xb])
                    pP = self.bank()
                    P.op("pe", lambda e: e.matmul(pP[:, 0:n], lhsT=permb.ap, rhs=xb[:, 0:n], start=True, stop=True), r=[permb, xb], w=[pP])
                    P.op("dve", lambda e: e.tensor_tensor(out=t1[:, 0:n], in0=src_ap, in1=cosT[:, t0:t0 + n], op=ALU.mult), r=src_dep + [cosT], w=[t1])
                    P.op("dve", lambda e: e.tensor_tensor(out=t2[:, 0:n], in0=pP[:, 0:n], in1=sinT[:, t0:t0 + n], op=ALU.mult), r=[pP, sinT], w=[t2])
                    P.op("dve", lambda e: e.tensor_tensor(out=out_ap, in0=t1[:, 0:n], in1=t2[:, 0:n], op=ALU.add), r=[t1, t2], w=[out_tl])

                P.op("dve", lambda e: e.memset(xk.ap, 0.0), w=[xk])
                P.dma("sp", xk[0:64, :], self.UF[O_KR:O_KR + 64, :], w=[xk])
                for (t0, n) in self.tok_blocks(0, T):
                    rope(xk[:, t0:t0 + n], [xk], krT[:, t0:t0 + n], krT, t0, n)
                    if os.environ.get("MLA_STOP") == "B1":
                        return
                    for h in range(4):
                        pq = self.bank()
                        for kc in range(4):
                            P.op("pe", lambda e: e.matmul(pq[:, 0:n], lhsT=Wuq[:, kc, h * 192:h * 192 + 128], rhs=cqT[:, kc, t0:t0 + n], start=(kc == 0), stop=(kc == 3)), r=[Wuq, cqT], w=[pq])
                        P.op("act", lambda e: e.copy(out=qnT[:, h, t0:t0 + n], in_=pq[:, 0:n]), r=[pq], w=[qnT])
                        if os.environ.get("MLA_STOP") == "B2a":
                            return
                        pr = self.bank()
                        for kc in range(4):
                            P.op("pe", lambda e: e.matmul(pr[:, 0:n], lhsT=Wr[:, kc, h, :], rhs=cqT[:, kc, t0:t0 + n], start=(kc == 0), stop=(kc == 3)), r=[Wr, cqT], w=[pr])
                        rope(pr[:, 0:n], [pr], qrT[:, h, t0:t0 + n], qrT, t0, n)
                        if os.environ.get("MLA_STOP") == "B2b":
                            return
                        pk = self.bank()
                        for kc in range(2):
                            P.op("pe", lambda e: e.matmul(pk[:, 0:n], lhsT=Wukv[:, kc, h * 256:h * 256 + 128], rhs=ckvT[:, kc, t0:t0 + n], start=(kc == 0), stop=(kc == 1)), r=[Wukv, ckvT], w=[pk])
                        P.op("act", lambda e: e.copy(out=knT[:, h, t0:t0 + n], in_=pk[:, 0:n]), r=[pk], w=[knT])
                if os.environ.get("MLA_STOP") == "B2":
                    return
                for i in range(NT):
                    pv = self.bank()
                    for h in range(4):
                        for kc in range(2):
                            P.op("pe", lambda e: e.matmul(pv[:, h * 128:(h + 1) * 128], lhsT=ckvT[:, kc, i * 128:(i + 1) * 128], rhs=Wukv[:, kc, h * 256 + 128:(h + 1) * 256],
                                                          start=(kc == 0), stop=(kc == 1)), r=[ckvT, Wukv], w=[pv])
                    P.op("act", lambda e: e.copy(out=vaug[:, i, :, 0:128], in_=pv[:, :].rearrange("p (h c) -> p h c", h=4)), r=[pv], w=[vaug])
                P.barrier()
            if os.environ.get("MLA_STOP") == "B":
                return
            (acc,) = self.take_banks(1)
            PTe = [self.sb(st, [128, 128], BF16) for _ in range(2)]
            ob = [self.sb(st, [128, 512]) for _ in range(2)]
            rc = self.sb(st, [128, 4])
            scale = float((128 + 64) ** -0.5)
            qtiles = [(i, list(range(self.NCT))) for i in range(self.NCT)] + [(i, list(range(NT))) for i in range(self.NCT, NT)]
            pi = 0
            for oi, (qi, ktl) in enumerate(qtiles):
                qsl = slice(qi * 128, (qi + 1) * 128)
                o_ = ob[oi % 2]
                for h in range(4):
                    for kt in ktl:
                        ks = slice(kt * 128, (kt + 1) * 128)
                        pST = self.bank()
                        P.op("pe", lambda e: e.matmul(pST[:, 0:128], lhsT=knT[:, h, ks], rhs=qnT[:, h, qsl], start=True, stop=False), r=[knT, qnT], w=[pST])
                        P.op("pe", lambda e: e.matmul(pST[:, 0:128], lhsT=krT[:, ks], rhs=qrT[:, h, qsl], start=False, stop=True), r=[krT, qrT], w=[pST])
                        pt = PTe[pi % 2]
                        pi += 1
                        P.op("act", lambda e: e.activation(out=pt.ap, in_=pST[:, 0:128], func=AF.Exp, scale=scale), r=[pST], w=[pt])
                        P.op("pe", lambda e: e.matmul(acc[:, 0:129], lhsT=pt.ap, rhs=vaug[:, kt, h, 0:129], start=(kt == ktl[0]), stop=(kt == ktl[-1])), r=[pt, vaug], w=[acc])
                    P.op("dve", lambda e: e.reciprocal(out=rc[:, h:h + 1], in_=acc[:, 128:129]), r=[acc], w=[rc])
                    P.op("dve", lambda e: e.tensor_scalar(out=o_[:, h * 128:(h + 1) * 128], in0=acc[:, 0:128], scalar1=rc[:, h:h + 1], scalar2=None, op0=ALU.mult), r=[acc, rc], w=[o_])
                P.dma("sp", self.BR[2, qsl, :], o_.ap, r=[o_])
            self.release_banks()

    def phase_merge(self, l):
        P = self.P
        T, NT = self.T, self.NT
        with ExitStack() as st:
            self.load_hT(st)
            brT = [self.sb(st, [128, 4, T], BF16) for _ in range(4)]
            with ExitStack() as s2:
                bt = self.sb(s2, [128, 512]); bb = self.sb(s2, [128, 512], BF16)
                for k in range(4):
                    for i in range(NT):
                        P.dma("sp", bt.ap, self.BR[k, i * 128:(i + 1) * 128, :], w=[bt])
                        P.op("dve", lambda e: e.tensor_copy(out=bb.ap, in_=bt.ap), r=[bt], w=[bb])

                        def dst(q, nb, pb, i=i, k=k):
                            P.op("act", lambda e: e.copy(out=brT[k][:, q:q + nb, i * 128:(i + 1) * 128],
                                                         in_=pb[:, 0:nb * 128].rearrange("p (a t) -> p a t", a=nb)), r=[pb], w=[brT[k]])
                        self.transpose_to(bb, 512, dst)
                P.barrier()
            wg = self.wpipe(st, KC, 128)
            wb_ = self.wpipe(st, 4, 128)
            acc = self.sb(st, [128, T]); accb = self.sb(st, [128, T], BF16)
            gs = self.sb(st, [128, 512]); tmp = self.sb(st, [128, 512])
            for n in range(16):
                ns = slice(n * 128, (n + 1) * 128)
                for k in range(4):
                    Wg = self.wload(wg, self.w_gate[l, k, :, ns], KC, 128)
                    Wb = self.wload(wb_, self.w_br[l, k, :, ns], 4, 128)
                    for (t0, nt) in self.tok_blocks(0, T):
                        pg = self.bank()
                        for kc in range(KC):
                            P.op("pe", lambda e: e.matmul(pg[:, 0:nt], lhsT=Wg[:, kc, :], rhs=self.hT[:, kc, t0:t0 + nt], start=(kc == 0), stop=(kc == KC - 1)), r=[Wg, self.hT], w=[pg])
                        P.op("act", lambda e: e.activation(out=gs[:, 0:nt], in_=pg[:, 0:nt], func=AF.Sigmoid), r=[pg], w=[gs])
                        pbk = self.bank()
                        for kc in range(4):
                            P.op("pe", lambda e: e.matmul(pbk[:, 0:nt], lhsT=Wb[:, kc, :], rhs=brT[k][:, kc, t0:t0 + nt], start=(kc == 0), stop=(kc == 3)), r=[Wb, brT[k]], w=[pbk])
                        if k == 0:
                            P.op("dve", lambda e: e.tensor_tensor(out=acc[:, t0:t0 + nt], in0=pbk[:, 0:nt], in1=gs[:, 0:nt], op=ALU.mult), r=[pbk, gs], w=[acc])
                        else:
                            P.op("dve", lambda e: e.tensor_tensor(out=tmp[:, 0:nt], in0=pbk[:, 0:nt], in1=gs[:, 0:nt], op=ALU.mult), r=[pbk, gs], w=[tmp])
                            P.op("dve", lambda e: e.tensor_tensor(out=acc[:, t0:t0 + nt], in0=acc[:, t0:t0 + nt], in1=tmp[:, 0:nt], op=ALU.add), r=[acc, tmp], w=[acc])
                P.op("act", lambda e: e.copy(out=accb.ap, in_=acc.ap), r=[acc], w=[accb])
                P.dma("sp", self.ACC[ns, :], accb.ap, r=[accb])

    def phase_wout(self, l):
        P = self.P
        T, NT = self.T, self.NT
        with ExitStack() as st:
            accT = self.sb(st, [128, KC, T], BF16)
            P.dma("sp", accT.ap, self.ACC.rearrange("(k p) t -> p k t", p=128), w=[accT])
            wp = self.wpipe(st, KC, 256)
            ev = [self.sb(st, [128, 256]) for _ in range(2)]
            ei = 0
            nxt = self.wload(wp, self.w_out[l, :, 0:256], KC, 256)
            for c in range(8):
                Wo = nxt
                if c + 1 < 8:
                    nxt = self.wload(wp, self.w_out[l, :, (c + 1) * 256:(c + 2) * 256], KC, 256)
                for i in range(NT):
                    pb = self.bank()
                    for kc in range(KC):
                        P.op("pe", lambda e: e.matmul(pb[:, 0:256], lhsT=accT[:, kc, i * 128:(i + 1) * 128], rhs=Wo[:, kc, :], start=(kc == 0), stop=(kc == KC - 1)), r=[accT, Wo], w=[pb])
                    e_ = ev[ei % 2]
                    ei += 1
                    P.op("act", lambda e: e.copy(out=e_.ap, in_=pb[:, 0:256]), r=[pb], w=[e_])
                    P.dma("sp", self.Y[i * 128:(i + 1) * 128, c * 256:(c + 1) * 256], e_.ap, r=[e_])
            P.barrier()
        self.residual(l, "G2")
        P.barrier()
        self.norm_mod_to_hT(l, "A2", "B2")

    def residual(self, l, kind):
        P = self.P
        with ExitStack() as st:
            G = self.load_modvec(st, l, kind)
            yt = [self.sb(st, [128, D]) for _ in range(2)]
            xt = [self.sb(st, [128, D]) for _ in range(2)]
            junk = self.sb(st, [128, D], BF16)
            ss = [self.sb(st, [128, 4]) for _ in range(2)]
            for i in range(self.NT):
                j = 1 if i < self.NCT else 0
                y_, x_, s_ = yt[i % 2], xt[i % 2], ss[i % 2]
                P.dma("sp", y_.ap, self.Y[i * 128:(i + 1) * 128, :], w=[y_])
                P.dma("sp", x_.ap, self.xres[i * 128:(i + 1) * 128, :], w=[x_])
                self.rstd_of(y_.ap, [y_], D, junk.ap, s_)
                P.op("dve", lambda e: e.scalar_tensor_tensor(out=y_.ap, in0=y_.ap, scalar=s_[:, 3:4], in1=G[:, j, :], op0=ALU.mult, op1=ALU.mult), r=[y_, s_, G], w=[y_])
                P.op("dve", lambda e: e.tensor_tensor(out=x_.ap, in0=x_.ap, in1=y_.ap, op=ALU.add), r=[x_, y_], w=[x_])
                P.dma("sp", self.xres[i * 128:(i + 1) * 128, :], x_.ap, r=[x_])

    def phase_moe(self, l):
        P = self.P
        T, NT = self.T, self.NT
        NE = 1 if self.tiny_moe else 32
        PTK = min(NT, 9)
        with ExitStack() as st:
            comb = self.sb(st, [128, NT, 32])
            with ExitStack() as s2:
                self.load_hT(s2)
                ws = self.sb(s2, [128, KC, 36]); wr = self.sb(s2, [128, KC, 36], BF16)
                P.dma("sp", ws[:, :, 0:4], self.w_grp[l].rearrange("(k p) n -> p k n", p=128), w=[ws])
                P.dma("sp", ws[:, :, 4:36], self.w_exp[l].rearrange("(k p) n -> p k n", p=128), w=[ws])
                P.op("dve", lambda e: e.tensor_copy(out=wr.ap, in_=ws.ap), r=[ws], w=[wr])
                lg = self.sb(s2, [128, 36]); sc = self.sb(s2, [128, 16]); gm = self.sb(s2, [128, 4]); ge = self.sb(s2, [128, 4])
                t48 = self.sb(s2, [128, 4, 8]); sel = self.sb(s2, [128, 8]); sel2 = self.sb(s2, [128, 8]); e1 = self.sb(s2, [128, 8]); e2 = self.sb(s2, [128, 8]); c8 = self.sb(s2, [128, 8])
                for i in range(NT):
                    pb = self.bank()
                    for kc in range(KC):
                        P.op("pe", lambda e: e.matmul(pb[:, 0:36], lhsT=self.hT[:, kc, i * 128:(i + 1) * 128], rhs=wr[:, kc, :], start=(kc == 0), stop=(kc == KC - 1)), r=[self.hT, wr], w=[pb])
                    P.op("dve", lambda e: e.tensor_copy(out=lg.ap, in_=pb[:, 0:36]), r=[pb], w=[lg])
                    P.op("dve", lambda e: e.tensor_reduce(out=sc[:, 0:1], in_=lg[:, 0:4], axis=AX.X, op=ALU.max), r=[lg], w=[sc])
                    P.op("dve", lambda e: e.tensor_scalar(out=sc[:, 1:2], in0=sc[:, 0:1], scalar1=-1.0, scalar2=None, op0=ALU.mult), r=[sc], w=[sc])
                    P.op("act", lambda e: e.activation(out=ge.ap, in_=lg[:, 0:4], func=AF.Exp, bias=sc[:, 1:2], scale=1.0), r=[lg, sc], w=[ge])
                    P.op("dve", lambda e: e.tensor_reduce(out=sc[:, 2:3], in_=ge.ap, axis=AX.X, op=ALU.add), r=[ge], w=[sc])
                    P.op("dve", lambda e: e.reciprocal(out=sc[:, 3:4], in_=sc[:, 2:3]), r=[sc], w=[sc])
                    P.op("dve", lambda e: e.tensor_scalar(out=gm.ap, in0=lg[:, 0:4], scalar1=sc[:, 0:1], scalar2=None, op0=ALU.is_equal), r=[lg, sc], w=[gm])
                    P.op("dve", lambda e: e.tensor_tensor(out=t48.ap, in0=lg[:, 4:36].rearrange("p (g e) -> p g e", g=4), in1=gm.ap.unsqueeze(2).broadcast_to([128, 4, 8]), op=ALU.mult), r=[lg, gm], w=[t48])
                    P.op("dve", lambda e: e.tensor_reduce(out=sel.ap, in_=t48.ap.rearrange("p g e -> p e g"), axis=AX.X, op=ALU.add), r=[t48], w=[sel])
                    P.op("dve", lambda e: e.tensor_reduce(out=sc[:, 4:5], in_=sel.ap, axis=AX.X, op=ALU.max), r=[sel], w=[sc])
                    P.op("dve", lambda e: e.tensor_scalar(out=e1.ap, in0=sel.ap, scalar1=sc[:, 4:5], scalar2=None, op0=ALU.is_equal), r=[sel, sc], w=[e1])
                    P.op("dve", lambda e: e.scalar_tensor_tensor(out=sel2.ap, in0=e1.ap, scalar=-1e30, in1=sel.ap, op0=ALU.mult, op1=ALU.add), r=[e1, sel], w=[sel2])
                    P.op("dve", lambda e: e.tensor_reduce(out=sc[:, 5:6], in_=sel2.ap, axis=AX.X, op=ALU.max), r=[sel2], w=[sc])
                    P.op("dve", lambda e: e.tensor_scalar(out=e2.ap, in0=sel2.ap, scalar1=sc[:, 5:6], scalar2=None, op0=ALU.is_equal), r=[sel2, sc], w=[e2])
                    P.op("dve", lambda e: e.tensor_tensor(out=sc[:, 6:7], in0=sc[:, 5:6], in1=sc[:, 4:5], op=ALU.subtract), r=[sc], w=[sc])
                    P.op("act", lambda e: e.activation(out=sc[:, 6:7], in_=sc[:, 6:7], func=AF.Exp), r=[sc], w=[sc])
                    P.op("dve", lambda e: e.tensor_scalar(out=sc[:, 6:7], in0=sc[:, 6:7], scalar1=1.0, scalar2=None, op0=ALU.add), r=[sc], w=[sc])
                    P.op("dve", lambda e: e.reciprocal(out=sc[:, 7:8], in_=sc[:, 6:7]), r=[sc], w=[sc])
                    P.op("dve", lambda e: e.tensor_tensor(out=sc[:, 8:9], in0=sc[:, 7:8], in1=sc[:, 3:4], op=ALU.mult), r=[sc], w=[sc])
                    P.op("dve", lambda e: e.tensor_tensor(out=sc[:, 9:10], in0=sc[:, 3:4], in1=sc[:, 8:9], op=ALU.subtract), r=[sc], w=[sc])
                    P.op("dve", lambda e: e.tensor_scalar(out=c8.ap, in0=e1.ap, scalar1=sc[:, 8:9], scalar2=None, op0=ALU.mult), r=[e1, sc], w=[c8])
                    P.op("dve", lambda e: e.scalar_tensor_tensor(out=c8.ap, in0=e2.ap, scalar=sc[:, 9:10], in1=c8.ap, op0=ALU.mult, op1=ALU.add), r=[e2, sc, c8], w=[c8])
                    for g in range(4):
                        P.op("dve", lambda e: e.tensor_scalar(out=comb[:, i, g * 8:(g + 1) * 8], in0=c8.ap, scalar1=gm[:, g:g + 1], scalar2=None, op0=ALU.mult), r=[c8, gm], w=[comb])
                P.barrier()
            w13 = self.wpipe(st, KC, 128, nbuf=4)
            w2p = self.wpipe(st, 4, 512, nbuf=2)
            hTp = self.sb(st, [128, KC, PTK * 128], BF16)
            aT = self.sb(st, [128, 4, PTK * 128], BF16)
            acc = self.sb(st, [128, PTK, D])
            s1 = [self.sb(st, [128, 512]) for _ in range(2)]
            si = 0
            for p0 in range(0, NT, PTK):
                np_ = min(PTK, NT - p0)
                ntok = np_ * 128
                P.dma("sp", hTp[:, :, 0:ntok], self.HTd[:, :, p0 * 128:p0 * 128 + ntok], w=[hTp])
                for ex in range(NE):
                    for c in range(4):
                        W1 = self.wload(w13, self.w1[l, ex, :, c * 128:(c + 1) * 128], KC, 128)
                        W3 = self.wload(w13, self.w3[l, ex, :, c * 128:(c + 1) * 128], KC, 128)
                        for (t0, nt) in self.tok_blocks(0, ntok):
                            p1 = self.bank()
                            for kc in range(KC):
                                P.op("pe", lambda e: e.matmul(p1[:, 0:nt], lhsT=W1[:, kc, :], rhs=hTp[:, kc, t0:t0 + nt], start=(kc == 0), stop=(kc == KC - 1)), r=[W1, hTp], w=[p1])
                            p3 = self.bank()
                            for kc in range(KC):
                                P.op("pe", lambda e: e.matmul(p3[:, 0:nt], lhsT=W3[:, kc, :], rhs=hTp[:, kc, t0:t0 + nt], start=(kc == 0), stop=(kc == KC - 1)), r=[W3, hTp], w=[p3])
                            s_ = s1[si % 2]
                            si += 1
                            P.op("act", lambda e: e.activation(out=s_[:, 0:nt], in_=p1[:, 0:nt], func=AF.Silu), r=[p1], w=[s_])
                            P.op("dve", lambda e: e.tensor_tensor(out=aT[:, c, t0:t0 + nt], in0=p3[:, 0:nt], in1=s_[:, 0:nt], op=ALU.mult), r=[p3, s_], w=[aT])
                    for cc in range(4):
                        W2 = self.wload(w2p, self.w2[l, ex, :, cc * 512:(cc + 1) * 512], 4, 512)
                        for t in range(np_):
                            py = self.bank()
                            for kc in range(4):
                                P.op("pe", lambda e: e.matmul(py[:, :], lhsT=aT[:, kc, t * 128:(t + 1) * 128], rhs=W2[:, kc, :], start=(kc == 0), stop=(kc == 3)), r=[aT, W2], w=[py])
                            cw_ = comb[:, p0 + t, ex:ex + 1]
                            if ex == 0:
                                P.op("dve", lambda e: e.tensor_scalar(out=acc[:, t, cc * 512:(cc + 1) * 512], in0=py[:, :], scalar1=cw_, scalar2=None, op0=ALU.mult), r=[py, comb], w=[acc])
                            else:
                                P.op("dve", lambda e: e.scalar_tensor_tensor(out=acc[:, t, cc * 512:(cc + 1) * 512], in0=py[:, :], scalar=cw_, in1=acc[:, t, cc * 512:(cc + 1) * 512],
                                                                             op0=ALU.mult, op1=ALU.add), r=[py, comb, acc], w=[acc])
                for t in range(np_):
                    P.dma("sp", self.Y[(p0 + t) * 128:(p0 + t + 1) * 128, :], acc[:, t, :], r=[acc])

    def phase_fin(self, l):
        self.residual(l, "G4")


def core_inputs(inputs, b, n_ctx, n_lat, consts, rope, tiny_moe=False):
    m = {}
    f = lambda a: np.ascontiguousarray(a, dtype=np.float32)
    m["x"] = f(inputs["x"][b, :n_lat])
    m["ctx"] = f(inputs["ctx"][b, :n_ctx])
    m["c"] = f(f(inputs["c"][b]).reshape(16, 128).T)
    m["c_ctx"] = f(f(inputs["c_ctx"]).reshape(16, 128).T)
    for k in ["w_ada", "b_ada", "norm_g", "w_in", "hg_lb_logits", "hg_norm_g", "mla_g_q", "mla_g_kv", "mla_w_uq",
              "mla_w_ukv", "ssd_conv_w", "ssd_conv_b", "ssd_norm_g", "w_gate", "w_br", "w_out", "moe_w_grp",
              "moe_w_exp", "moe_w1", "moe_w3", "moe_w2", "ssd_d"]:
        m[k] = f(inputs[k])
    L = inputs["w_in"].shape[0]
    m["ssd_conv_w"] = f(f(inputs["ssd_conv_w"]).reshape(L, 3, 8, 128).transpose(0, 3, 2, 1))
    m["ssd_conv_b"] = f(f(inputs["ssd_conv_b"]).reshape(L, 8, 128).transpose(0, 2, 1))
    m["hg_lbT"] = f(f(inputs["hg_lb_logits"]).reshape(L + 1, 4, 128).transpose(2, 0, 1))
    m["ml_f_bias"] = f(inputs["ml_f_bias"]).reshape(L, 8)
    m["ssd_a_log"] = f(inputs["ssd_a_log"]).reshape(L, 16)
    m["ssd_dt_bias"] = f(inputs["ssd_dt_bias"]).reshape(L, 16)
    if tiny_moe:
        for k in ["moe_w1", "moe_w3", "moe_w2"]:
            m[k] = np.ascontiguousarray(m[k][:, 0:1])
    m["consts"] = consts
    m["rope"] = rope
    return m


def kernel(**inputs):
    B, n_lat, _ = inputs["x"].shape
    n_ctx = inputs["ctx"].shape[1]
    mk = MK(n_ctx, n_lat)
    nc = mk.build()
    consts, rope = make_consts(n_ctx, n_lat)
    in_maps = [core_inputs(inputs, b, n_ctx, n_lat, consts, rope) for b in range(B)]
    res = run_bass_kernel_spmd(nc, in_maps, core_ids=list(range(B)))
    return np.stack([np.asarray(r["out"]) for r in res.results], axis=0).astype(np.float32)
```
